# Optimizing a Trainium2 kernel written in Bass

```python
import jax, jax.numpy as jnp
from jax import lax
import numpy as np

D_MODEL = 1024
BATCH = 16
SEQ = 4096
DEPTH = 4

CHUNK = 64
N_MEM = 256
EPS = 1e-6
D_A = D_MODEL // 2
D_B = D_MODEL - D_A
CONV_A = 3
CONV_B = 31
M_HEADS = 8
M_HEAD_DIM = D_MODEL // M_HEADS
X_HEADS = 4
X_HEAD_DIM = D_MODEL // X_HEADS
D_FF = ((8 * D_MODEL + 3 * 256 - 1) // (3 * 256)) * 256
N_EVEN = (DEPTH + 1) // 2
N_ODD = DEPTH // 2
A_IN = 3 * D_A + 2 * D_B
M_IN = 4 * D_MODEL + 2 * M_HEADS

kernel_name = "hybrid_conv_mlstm_memxattn_trunk"


def rmsnorm(x, g):
    xf = x.astype(jnp.float32)
    y = xf * lax.rsqrt(jnp.mean(xf * xf, axis=-1, keepdims=True) + EPS)
    return (y * g.astype(jnp.float32)).astype(x.dtype)


def layernorm(x, g, b):
    xf = x.astype(jnp.float32)
    mu = jnp.mean(xf, axis=-1, keepdims=True)
    var = jnp.mean(jnp.square(xf - mu), axis=-1, keepdims=True)
    y = (xf - mu) * lax.rsqrt(var + EPS)
    return (y * g.astype(jnp.float32) + b.astype(jnp.float32)).astype(x.dtype)


def causal_dwconv(x, w):
    k = w.shape[0]
    xp = jnp.pad(x, ((0, 0), (k - 1, 0), (0, 0)))
    return lax.conv_general_dilated(
        xp, w[:, None, :].astype(x.dtype), window_strides=(1,), padding="VALID",
        dimension_numbers=("NWC", "WIO", "NWC"), feature_group_count=x.shape[-1])


def conv_mixer(h, w_in, conv_a, conv_b, conv_b_bias, ln_g, ln_b, w_out):
    u = h @ w_in
    gb, gc, xa, ua, ub = jnp.split(u, [D_A, 2 * D_A, 3 * D_A, 3 * D_A + D_B], axis=-1)
    y_a = gb * causal_dwconv(gc * xa, conv_a)
    glu = ua * jax.nn.sigmoid(ub)
    z = causal_dwconv(glu, conv_b) + conv_b_bias
    y_b = jax.nn.silu(layernorm(z, ln_g, ln_b))
    return jnp.concatenate([y_a, y_b], axis=-1) @ w_out


def mlstm_mixer(h, w_in, i_bias, f_bias, mh_norm_g, w_out):
    bsz, s, _ = h.shape
    f32 = jnp.float32
    nc = s // CHUNK
    u = h @ w_in
    q, k, v, o, i_pre, f_pre = jnp.split(
        u, [D_MODEL, 2 * D_MODEL, 3 * D_MODEL, 4 * D_MODEL, 4 * D_MODEL + M_HEADS], axis=-1)
    i_log = (i_pre + i_bias).astype(f32)
    f_log = jax.nn.log_sigmoid((f_pre + f_bias).astype(f32))

    def heads_to_chunks(t):
        return t.astype(f32).reshape(bsz, nc, CHUNK, M_HEADS, M_HEAD_DIM).transpose(1, 0, 3, 2, 4)

    def gates_to_chunks(t):
        return t.reshape(bsz, nc, CHUNK, M_HEADS).transpose(1, 0, 3, 2)

    qc = heads_to_chunks(q)
    kc = heads_to_chunks(k) * (M_HEAD_DIM ** -0.5)
    vc = heads_to_chunks(v)
    ic = gates_to_chunks(i_log)
    fc = gates_to_chunks(f_log)
    causal = jnp.tril(jnp.ones((CHUNK, CHUNK), dtype=bool))

    def step(carry, xs):
        c_st, n_st, m_st = carry
        qq, kk, vv, ig, lf = xs
        b = jnp.cumsum(lf, axis=-1)
        g = b[..., -1]
        dmat = jnp.where(causal, b[..., :, None] - b[..., None, :] + ig[..., None, :], -jnp.inf)
        m_inter = b + m_st[..., None]
        m_t = jnp.maximum(m_inter, jnp.max(dmat, axis=-1))
        w = jnp.exp(dmat - m_t[..., None]) * jnp.einsum("bhld,bhsd->bhls", qq, kk)
        inter = jnp.exp(m_inter - m_t)
        num = inter[..., None] * jnp.einsum("bhld,bhdv->bhlv", qq, c_st) \
            + jnp.einsum("bhls,bhsv->bhlv", w, vv)
        den = inter * jnp.einsum("bhld,bhd->bhl", qq, n_st) + jnp.sum(w, axis=-1)
        out = num / jnp.maximum(jnp.abs(den), jnp.exp(-m_t))[..., None]
        a = g[..., None] - b + ig
        m_new = jnp.maximum(g + m_st, jnp.max(a, axis=-1))
        wa = jnp.exp(a - m_new[..., None])
        decay = jnp.exp(g + m_st - m_new)
        c_new = decay[..., None, None] * c_st + jnp.einsum("bhs,bhsk,bhsv->bhkv", wa, kk, vv)
        n_new = decay[..., None] * n_st + jnp.einsum("bhs,bhsk->bhk", wa, kk)
        return (c_new, n_new, m_new), out

    init = (jnp.zeros((bsz, M_HEADS, M_HEAD_DIM, M_HEAD_DIM), f32),
            jnp.zeros((bsz, M_HEADS, M_HEAD_DIM), f32),
            jnp.zeros((bsz, M_HEADS), f32))
    _, hc = lax.scan(step, init, (qc, kc, vc, ic, fc))
    hh = hc.transpose(1, 0, 3, 2, 4).reshape(bsz, s, M_HEADS, M_HEAD_DIM)
    mu = jnp.mean(hh, axis=-1, keepdims=True)
    var = jnp.mean(jnp.square(hh - mu), axis=-1, keepdims=True)
    hn = (hh - mu) * lax.rsqrt(var + EPS) * mh_norm_g.astype(f32).reshape(M_HEADS, M_HEAD_DIM)
    y = hn.reshape(bsz, s, D_MODEL).astype(h.dtype) * jax.nn.sigmoid(o)
    return y @ w_out


def mem_attention(h, memn, w_q, w_kv, w_o):
    bsz, s, _ = h.shape
    q = (h @ w_q).reshape(bsz, s, X_HEADS, X_HEAD_DIM)
    k, v = jnp.split(memn @ w_kv, 2, axis=-1)
    k = k.reshape(bsz, -1, X_HEADS, X_HEAD_DIM)
    v = v.reshape(bsz, -1, X_HEADS, X_HEAD_DIM)
    sc = jnp.einsum("bshd,bmhd->bhsm", q, k).astype(jnp.float32) * (X_HEAD_DIM ** -0.5)
    p = jax.nn.softmax(sc, axis=-1).astype(h.dtype)
    o = jnp.einsum("bhsm,bmhd->bshd", p, v).reshape(bsz, s, D_MODEL)
    return o @ w_o


def swiglu(h, w_gu, w_down):
    gt, up = jnp.split(h @ w_gu, 2, axis=-1)
    return (jax.nn.silu(gt) * up) @ w_down


def setup_inputs(seed: int = 0) -> dict:
    key = jax.random.key(seed)
    ks = jax.random.split(key, 24)
    nrm = jax.random.normal
    f32 = jnp.float32

    def lin(k, shape, fan_in):
        return nrm(k, shape, f32) * (fan_in ** -0.5)

    def gain(k, shape):
        return 1.0 + 0.02 * nrm(k, shape, f32)

    f_bias = jnp.linspace(3.0, 6.0, M_HEADS, dtype=f32)[None, :] + 0.1 * nrm(ks[14], (N_ODD, M_HEADS), f32)
    return {
        "x": nrm(ks[0], (BATCH, SEQ, D_MODEL), f32),
        "mem": nrm(ks[1], (BATCH, N_MEM, D_MODEL), f32),
        "norm_g": gain(ks[2], (DEPTH, 6, D_MODEL)),
        "mem_norm_g": gain(ks[3], (DEPTH, D_MODEL)),
        "a_w_in": lin(ks[4], (N_EVEN, D_MODEL, A_IN), D_MODEL),
        "a_conv_a": lin(ks[5], (N_EVEN, CONV_A, D_A), CONV_A),
        "a_conv_b": lin(ks[6], (N_EVEN, CONV_B, D_B), CONV_B),
        "a_conv_b_bias": 0.01 * nrm(ks[7], (N_EVEN, D_B), f32),
        "a_ln_g": gain(ks[8], (N_EVEN, D_B)),
        "a_ln_b": 0.02 * nrm(ks[9], (N_EVEN, D_B), f32),
        "a_w_out": lin(ks[10], (N_EVEN, D_MODEL, D_MODEL), D_MODEL),
        "m_w_in": lin(ks[11], (N_ODD, D_MODEL, M_IN), D_MODEL),
        "m_i_bias": 0.1 * nrm(ks[12], (N_ODD, M_HEADS), f32),
        "m_f_bias": f_bias,
        "m_norm_g": gain(ks[13], (N_ODD, D_MODEL)),
        "m_w_out": lin(ks[15], (N_ODD, D_MODEL, D_MODEL), D_MODEL),
        "x_w_q": lin(ks[16], (DEPTH, D_MODEL, D_MODEL), D_MODEL),
        "x_w_kv": lin(ks[17], (DEPTH, D_MODEL, 2 * D_MODEL), D_MODEL),
        "x_w_o": lin(ks[18], (DEPTH, D_MODEL, D_MODEL), D_MODEL),
        "f_w_gu": lin(ks[19], (DEPTH, D_MODEL, 2 * D_FF), D_MODEL),
        "f_w_down": lin(ks[20], (DEPTH, D_FF, D_MODEL), D_FF),
    }


def reference(x, mem, norm_g, mem_norm_g, a_w_in, a_conv_a, a_conv_b, a_conv_b_bias,
              a_ln_g, a_ln_b, a_w_out, m_w_in, m_i_bias, m_f_bias, m_norm_g, m_w_out,
              x_w_q, x_w_kv, x_w_o, f_w_gu, f_w_down):
    for layer in range(DEPTH):
        g = norm_g[layer]
        h = rmsnorm(x, g[0])
        if layer % 2 == 0:
            e = layer // 2
            y = conv_mixer(h, a_w_in[e], a_conv_a[e], a_conv_b[e], a_conv_b_bias[e],
                           a_ln_g[e], a_ln_b[e], a_w_out[e])
        else:
            o = layer // 2
            y = mlstm_mixer(h, m_w_in[o], m_i_bias[o], m_f_bias[o], m_norm_g[o], m_w_out[o])
        x = x + rmsnorm(y, g[1])
        memn = rmsnorm(mem, mem_norm_g[layer])
        x = x + rmsnorm(mem_attention(rmsnorm(x, g[2]), memn, x_w_q[layer], x_w_kv[layer],
                                      x_w_o[layer]), g[3])
        x = x + rmsnorm(swiglu(rmsnorm(x, g[4]), f_w_gu[layer], f_w_down[layer]), g[5])
    return x
```

```python
import contextlib
import numpy as np
import concourse.bass as bass
import concourse.mybir as mybir
from concourse.bass_utils import run_bass_kernel_spmd

F32 = mybir.dt.float32
BF16 = mybir.dt.bfloat16
AF = mybir.ActivationFunctionType
ALU = mybir.AluOpType
AX = mybir.AxisListType

D = 1024
KC = 8
T = 256
NMEM = 256
DFF = 2816
SEQ = 4096
NCORES = 8
EPS = 1e-6
NRING = 5
SKEW = 2
SLOT = 4096
KSCALE = 128.0 ** -0.5

def _cp_layout():
    off = {}
    n = 0
    for name, cnt in (("norm_g", 4 * 6 * 8), ("mem_g", 4 * 8), ("conv_a", 2 * 4 * 3), ("conv_b", 2 * 4 * 31),
                      ("cb_bias", 2 * 4), ("ln_g", 2 * 4), ("ln_b", 2 * 4), ("m_g", 2 * 8)):
        off[name] = n
        n += cnt
    return off, n


CPO, NCP = _cp_layout()


class Ctr:
    LIMIT = 16000

    def __init__(self, fw, name, owner=None):
        self.fw, self.name, self.owner, self.k, self.val = fw, name, owner, 0, 0
        self.sem = fw.new_sem(name + "_0")

    def bump(self, inc):
        if self.val + inc > self.LIMIT:
            self.k += 1
            self.sem = self.fw.new_sem("%s_%d" % (self.name, self.k))
            self.val = 0
        self.val += inc
        return (self.sem, self.val, self.owner)


class Eng:
    def __init__(self, fw, name, h, selfsync=True):
        self.h, self.name, self.selfsync = h, name, selfsync
        self.ctr = Ctr(fw, name, self)
        self.waited = {}


class Buf:
    def __init__(self, name, ap=None):
        self.name, self.ap = name, ap
        self.w = None
        self.r = {}
        self.dctr = None

    def deps(self):
        d = list(self.r.values())
        if self.w is not None:
            d.append(self.w)
        return d


class FW:
    def __init__(self, nc, es, dry):
        self.nc, self.es, self.dry = nc, es, dry
        self.nsem = 0
        self.nops = 0
        self.dctrs = {}
        if not dry:
            self.pe = Eng(self, "pe", nc.tensor, selfsync=False)
            self.act = Eng(self, "act", nc.scalar)
            self.dve = Eng(self, "dve", nc.vector)
            self.pool = Eng(self, "pool", nc.gpsimd)
            self.sp = Eng(self, "sp", nc.sync)
        else:
            self.pe = self.act = self.dve = self.pool = self.sp = None
        self.psbanks = None
        self.psptr = 0

    def new_sem(self, name):
        if self.dry:
            return None
        self.nsem += 1
        return self.es.enter_context(self.nc.semaphore(name))

    def _wait(self, eng, deps):
        for (sem, val, owner) in deps:
            if owner is eng and not eng.selfsync:
                continue
            k = id(sem)
            if eng.waited.get(k, 0) < val:
                eng.h.wait_ge(sem, val)
                eng.waited[k] = val

    def _deps(self, reads, writes):
        deps = []
        for b in reads:
            if b.w is not None:
                deps.append(b.w)
        for b in writes:
            deps.extend(b.deps())
        return deps

    def _commit(self, d, reads, writes):
        k = id(d[0])
        for b in reads:
            old = b.r.get(k)
            if old is None or old[1] < d[1]:
                b.r[k] = d
        for b in writes:
            b.w = d
            b.r = {}

    def op(self, eng, fn, reads=(), writes=()):
        if self.dry:
            return
        self._wait(eng, self._deps(reads, writes))
        ins = fn(eng.h)
        d = eng.ctr.bump(1)
        ins.then_inc(d[0], 1)
        self._commit(d, reads, writes)
        self.nops += 1

    def dma(self, eng, out, in_, reads=(), writes=()):
        if self.dry:
            return
        self._wait(eng, self._deps(reads, writes))
        ins = eng.h.dma_start(out=out, in_=in_)
        b = writes[0] if writes else reads[0]
        if b.dctr is None:
            if b.name not in self.dctrs:
                self.dctrs[b.name] = Ctr(self, "d_" + b.name, None)
            b.dctr = self.dctrs[b.name]
        d = b.dctr.bump(16)
        ins.then_inc(d[0], 16)
        self._commit(d, reads, writes)
        self.nops += 1

    def wait_all(self, eng, bufs):
        if self.dry:
            return
        deps = []
        for b in bufs:
            deps.extend(b.deps())
        self._wait(eng, deps)

    def psum(self, n=1):
        if self.psptr + n > 8:
            self.psptr = 0
        bs = self.psbanks[self.psptr:self.psptr + n]
        self.psptr = (self.psptr + n) % 8
        return bs


class Arena:
    def __init__(self, fw, tensor, nelem):
        self.fw, self.t, self.n = fw, tensor, nelem
        self.hist = []
        self.cur = []
        self.off = 0

    def reset(self):
        newh = list(self.cur)
        for (a, b, buf) in self.hist:
            if not any(a < cb and ca < b for (ca, cb, _) in self.cur):
                newh.append((a, b, buf))
        self.hist = newh
        self.cur = []
        self.off = 0

    def alloc(self, name, shape, dtype):
        n = 1
        for s in shape:
            n *= s
        nb = n * (2 if dtype == F32 else 1)
        nb = (nb + 15) // 16 * 16
        a, b = self.off, self.off + nb
        assert b <= self.n, "arena overflow %s %d > %d" % (name, b, self.n)
        self.off = b
        ap = self.t[:, a:a + n * (2 if dtype == F32 else 1)]
        if dtype == F32:
            ap = ap.bitcast(F32)
        if len(shape) == 2:
            ap = ap.rearrange("p (a b) -> p a b", b=shape[1])
        elif len(shape) == 3:
            ap = ap.rearrange("p (a b c) -> p a b c", b=shape[1], c=shape[2])
        buf = Buf(name, ap)
        for (ha, hb, hbuf) in self.hist:
            if ha < b and a < hb:
                for d in hbuf.deps():
                    k = id(d[0])
                    old = buf.r.get(k)
                    if old is None or old[1] < d[1]:
                        buf.r[k] = d
        self.cur.append((a, b, buf))
        return buf


class WQ:
    def __init__(self, fw, slots, wdram, plan):
        self.fw, self.slots, self.wdram = fw, slots, wdram
        self.plan = plan
        self.i = 0
        self.issued = 0

    def _view(self, slot, nkc, ncols):
        return slot.ap[:, 0:nkc * ncols].rearrange("p (k m) -> p k m", m=ncols)

    def _issue(self, j):
        (wname, li, row0, nkc, segs) = self.plan[j]
        slot = self.slots[j % NRING]
        ncols = sum(n for (_, n) in segs)
        v = self._view(slot, nkc, ncols)
        W = self.wdram[wname][li]
        off = 0
        for (c0, n) in segs:
            src = W[row0:row0 + nkc * 128, c0:c0 + n].rearrange("(k p) m -> p k m", p=128)
            self.fw.dma(self.fw.pool, v[:, :, off:off + n], src, writes=[slot])
            off += n

    def get(self, k, spec, bmin):
        (wname, li, row0, nkc, segs) = spec
        ncols = sum(n for (_, n) in segs)
        assert nkc * ncols <= SLOT
        if self.fw.dry:
            assert k == len(self.plan)
            self.plan.append(spec)
            return self.slots[0], self._view(self.slots[0], nkc, ncols)
        assert self.plan[k] == spec, (k, self.plan[k], spec)
        while self.issued < min(len(self.plan), bmin + NRING):
            self._issue(self.issued)
            self.issued += 1
        assert k < self.issued
        slot = self.slots[k % NRING]
        return slot, self._view(slot, nkc, ncols)


class Ctx:
    pass


class L:
    pass


def _emit(nc, es, dry, plan, NSEQ, NT, DEPTH, io):
    fw = FW(nc, es, dry)
    E = es.enter_context
    sfx = "d" if dry else "r"
    TC = T // 128

    def sb(name, shape, dt):
        return E(nc.sbuf_tensor(name + sfx, shape, dt))

    ring = [Buf("ring%d" % i, sb("ring%d" % i, [128, SLOT], BF16)[:]) for i in range(NRING)]
    CSTt = sb("CST", [128, 3, 128], F32)
    CSBt = sb("CSB", [128, 3, 128], BF16)
    CST = Buf("CST", CSTt[:])
    CSB = Buf("CSB", CSBt[:])
    CPt = sb("CP", [128, NCP], F32)
    CP = Buf("CP", CPt[:])
    RPt = sb("RP", [128, 2, 16], F32)
    RP = Buf("RP", RPt[:])
    NA = 28544
    ctxs = []
    for s in range(NSEQ):
        c = Ctx()
        c.s = s
        c.Xt = sb("X%d" % s, [128, KC, T], F32)
        c.X = Buf("X%d" % s, c.Xt[:])
        c.C32t = sb("C32_%d" % s, [128, 2, 8, 129], F32)
        c.C32 = [Buf("C32_%d_%d" % (s, o), c.C32t[:, o]) for o in range(2)]
        c.GLUt = sb("GLU%d" % s, [128, 2, 4, 30 + T], BF16)
        c.PBt = sb("PB%d" % s, [128, 2, 4, 2 + T], BF16)
        c.GLU = [Buf("GLU%d_%d" % (s, e), c.GLUt[:, e]) for e in range(2)]
        c.PB = [Buf("PB%d_%d" % (s, e), c.PBt[:, e]) for e in range(2)]
        c.ARt = sb("AR%d" % s, [128, NA], BF16)
        c.ar = Arena(fw, c.ARt, NA)
        c.KVD = [Buf("kvd%d_%d" % (s, l)) for l in range(4)]
        ctxs.append(c)
    PSt = E(nc.psum_tensor("PS" + sfx, [128, 8, 512], F32))
    fw.psbanks = [Buf("ps%d" % i, PSt[:, i, :]) for i in range(8)]
    kvd = io["kvd"]

    wq = WQ(fw, ring, io["w"], plan)
    pe, act, dve, pool, sp = fw.pe, fw.act, fw.dve, fw.pool, fw.sp

    IDENTB = CSBt[:, 0, :]
    ONESB = CSBt[:, 2, :]
    TRIF = CSTt[:, 1, :]
    ONESF = CSTt[:, 2, :]

    def cpcol(name, idx):
        cc = CPO[name] + idx
        return CPt[:, cc:cc + 1]

    fw.dma(sp, CSTt[:], io["consts"], writes=[CST])
    fw.dma(sp, CPt[:], io["cp"], writes=[CP])
    fw.dma(sp, RPt[:], io["rp"], writes=[RP])
    fw.op(dve, lambda h: h.tensor_copy(CSBt[:], CSTt[:]), reads=[CST], writes=[CSB])

    def rms_rstd(srcs, srcbufs, nk, ncols, RB, SQ, dn):
        (ps,) = fw.psum(1)
        for k in range(nk):
            sq = SQ[k % 2]
            fw.op(act, lambda h: h.activation(out=sq.ap[:, 0:ncols], in_=srcs[k], func=AF.Square),
                  reads=[srcbufs[k]], writes=[sq])
            fw.op(pe, lambda h: h.matmul(ps.ap[:, 0:ncols], ONESB, sq.ap[:, 0:ncols], start=(k == 0), stop=(k == nk - 1)),
                  reads=[sq, CSB], writes=[ps])
        fw.op(dve, lambda h: h.tensor_scalar(out=RB.ap[:, 0:ncols], in0=ps.ap[:, 0:ncols], scalar1=1.0 / dn,
                                             scalar2=EPS, op0=ALU.mult, op1=ALU.add), reads=[ps], writes=[RB])
        fw.op(act, lambda h: h.activation(out=RB.ap[:, 0:ncols], in_=RB.ap[:, 0:ncols], func=AF.Sqrt),
              reads=[RB], writes=[RB])
        fw.op(dve, lambda h: h.reciprocal(out=RB.ap[:, 0:ncols], in_=RB.ap[:, 0:ncols]), reads=[RB], writes=[RB])

    def common_alloc(c):
        l = L()
        ar = c.ar
        ar.reset()
        l.H = ar.alloc("H", [KC, T], BF16)
        l.SQ = [ar.alloc("SQ%d" % i, [NMEM], BF16) for i in range(2)]
        l.RB = ar.alloc("RB", [NMEM], F32)
        l.RB2 = ar.alloc("RB2", [T], F32)
        l.YA = ar.alloc("YA", [KC, T], F32)
        l.SQA = ar.alloc("SQA", [KC, T], BF16)
        l.MIX = l.H
        c.l = l
        return l

    def rstd_from_ps(ps, RB):
        fw.op(act, lambda h: h.activation(out=RB.ap[:, 0:T], in_=ps.ap[:, 0:T], func=AF.Ln, scale=1.0 / D, bias=EPS),
              reads=[ps], writes=[RB])
        fw.op(act, lambda h: h.activation(out=RB.ap[:, 0:T], in_=RB.ap[:, 0:T], func=AF.Exp, scale=-0.5),
              reads=[RB], writes=[RB])

    def prenorm(c, layer, j):
        l = c.l
        fw.op(act, lambda h: h.activation(out=l.SQA.ap, in_=c.Xt[:], func=AF.Square), reads=[c.X], writes=[l.SQA])
        (ps,) = fw.psum(1)
        for k in range(KC):
            fw.op(pe, lambda h: h.matmul(ps.ap[:, 0:T], ONESB, l.SQA.ap[:, k, :], start=(k == 0), stop=(k == KC - 1)),
                  reads=[l.SQA, CSB], writes=[ps])
        rstd_from_ps(ps, l.RB)
        for k in range(KC):
            g = cpcol("norm_g", (layer * 6 + j) * 8 + k)
            fw.op(dve, lambda h: h.scalar_tensor_tensor(out=l.H.ap[:, k, :], in0=c.Xt[:, k, :], scalar=g,
                                                        in1=l.RB.ap[:, 0:T], op0=ALU.mult, op1=ALU.mult),
                  reads=[c.X, l.RB, CP], writes=[l.H])

    def linear_fm(cs, getsrc, nk, wname, li, col0, ncols_total, evac, blk=512):
        j = 0
        for cc in range(0, ncols_total, blk):
            n = min(blk, ncols_total - cc)
            slot, v = yield (wname, li, 0, nk, ((col0 + cc, n),))
            for m in range(n // 128):
                for c in cs:
                    src = getsrc(c)
                    (ps,) = fw.psum(1)
                    for k in range(nk):
                        fw.op(pe, lambda h: h.matmul(ps.ap[:, 0:T], v[:, k, m * 128:(m + 1) * 128], src.ap[:, k, :],
                                                     start=(k == 0), stop=(k == nk - 1)),
                              reads=[slot, src], writes=[ps])
                    evac(c, j, ps)
                j += 1

    def evac_y(c, j, ps, layer, jg):
        l = c.l
        g = cpcol("norm_g", (layer * 6 + jg) * 8 + j)
        sq = l.SQ[j % 2]
        fw.op(act, lambda h: h.activation(out=l.YA.ap[:, j, :], in_=ps.ap[:, 0:T], func=AF.Copy, scale=g),
              reads=[ps, CP], writes=[l.YA])
        fw.op(act, lambda h: h.activation(out=sq.ap[:, 0:T], in_=ps.ap[:, 0:T], func=AF.Square), reads=[ps], writes=[sq])
        (pst,) = fw.psum(1)
        fw.op(pe, lambda h: h.matmul(pst.ap[:, 0:T], ONESB, sq.ap[:, 0:T], start=True, stop=True), reads=[sq, CSB], writes=[pst])
        if j == 0:
            fw.op(dve, lambda h: h.tensor_copy(l.RB2.ap, pst.ap[:, 0:T]), reads=[pst], writes=[l.RB2])
        else:
            fw.op(dve, lambda h: h.tensor_tensor(out=l.RB2.ap, in0=pst.ap[:, 0:T], in1=l.RB2.ap, op=ALU.add),
                  reads=[pst, l.RB2], writes=[l.RB2])

    def postnorm(c, layer, jg):
        l = c.l
        rstd_from_ps(l.RB2, l.RB2)
        fw.op(dve, lambda h: h.tensor_tensor(out=l.YA.ap, in0=l.YA.ap, in1=l.RB2.ap.unsqueeze(1).broadcast_to([128, KC, T]), op=ALU.mult),
              reads=[l.YA, l.RB2], writes=[l.YA])
        fw.op(dve, lambda h: h.tensor_tensor(out=c.Xt[:], in0=c.Xt[:], in1=l.YA.ap, op=ALU.add), reads=[c.X, l.YA], writes=[c.X])

    def outproj_postnorm(cs, layer, jg, wname, li):
        def ev(c, j, ps):
            evac_y(c, j, ps, layer, jg)
        yield from linear_fm(cs, lambda c: c.l.MIX, KC, wname, li, 0, D, ev)
        for c in cs:
            postnorm(c, layer, jg)

    def kv_prep(cs, layer):
        for c in cs:
            ar = c.ar
            ar.reset()
            l = L()
            c.l = l
            l.MT = ar.alloc("MT%d" % c.s, [KC, NMEM], F32)
            l.HM = ar.alloc("HM", [KC, NMEM], BF16)
            l.SQ = [ar.alloc("SQ%d" % i, [NMEM], BF16) for i in range(2)]
            l.RB = ar.alloc("RB", [NMEM], F32)
            l.KTb = ar.alloc("KTs%d" % c.s, [KC, NMEM], BF16)
            l.Vb = ar.alloc("Vs%d" % c.s, [2, D], BF16)
            fw.dma(sp, l.MT.ap, io["mem"][c.s].rearrange("(k p) m -> p k m", p=128), writes=[l.MT])
            rms_rstd([l.MT.ap[:, k, :] for k in range(KC)], [l.MT] * KC, KC, NMEM, l.RB, l.SQ, float(D))
            for k in range(KC):
                g = cpcol("mem_g", layer * 8 + k)
                fw.op(dve, lambda h: h.scalar_tensor_tensor(out=l.HM.ap[:, k, :], in0=l.MT.ap[:, k, :], scalar=g,
                                                            in1=l.RB.ap[:, 0:NMEM], op0=ALU.mult, op1=ALU.mult),
                      reads=[l.MT, l.RB, CP], writes=[l.HM])
        for cc in range(2):
            slot, v = yield ("x_w_kv", layer, 0, KC, ((cc * 512, 512),))
            for m in range(4):
                for c in cs:
                    l = c.l
                    (ps,) = fw.psum(1)
                    for k in range(KC):
                        fw.op(pe, lambda h: h.matmul(ps.ap[:, 0:NMEM], v[:, k, m * 128:(m + 1) * 128], l.HM.ap[:, k, :],
                                                     start=(k == 0), stop=(k == KC - 1)), reads=[slot, l.HM], writes=[ps])
                    fw.op(act, lambda h: h.activation(out=l.KTb.ap[:, cc * 4 + m, :], in_=ps.ap[:, 0:NMEM], func=AF.Copy),
                          reads=[ps], writes=[l.KTb])
        for cc in range(2):
            slot, v = yield ("x_w_kv", layer, 0, KC, ((D + cc * 512, 512),))
            for tc in range(2):
                for c in cs:
                    l = c.l
                    (ps,) = fw.psum(1)
                    for k in range(KC):
                        fw.op(pe, lambda h: h.matmul(ps.ap, l.HM.ap[:, k, tc * 128:(tc + 1) * 128], v[:, k, :],
                                                     start=(k == 0), stop=(k == KC - 1)), reads=[slot, l.HM], writes=[ps])
                    fw.op(act, lambda h: h.activation(out=l.Vb.ap[:, tc, cc * 512:(cc + 1) * 512], in_=ps.ap, func=AF.Copy),
                          reads=[ps], writes=[l.Vb])
        for c in cs:
            l = c.l
            fw.dma(sp, kvd[c.s, layer, :, 0:2048], l.KTb.ap.rearrange("p a b -> p (a b)"), reads=[l.KTb], writes=[c.KVD[layer]])
            fw.dma(sp, kvd[c.s, layer, :, 2048:4096], l.Vb.ap.rearrange("p a b -> p (a b)"), reads=[l.Vb], writes=[c.KVD[layer]])

    def conv_sublayer(cs, layer):
        e = layer // 2
        for c in cs:
            l = common_alloc(c)
            ar = c.ar
            l.GB = ar.alloc("GB", [4, T], BF16)
            l.GC = ar.alloc("GC", [4, T], F32)
            l.UA = ar.alloc("UA", [4, T], F32)
            l.Z = [ar.alloc("Z%d" % j, [T], F32) for j in range(4)]
            l.SG = [ar.alloc("SG%d" % i, [T], F32) for i in range(2)]
            l.DG = [ar.alloc("DG%d" % i, [34, 128], BF16) for i in range(2)]
            l.MU = ar.alloc("MU", [T], F32)
            l.VR = ar.alloc("VR", [T], F32)
            l.cnt = 0
            prenorm(c, layer, 0)

        def ev(c, j, ps):
            l = c.l
            grp, jj = j // 4, j % 4
            pv = ps.ap[:, 0:T]
            if grp == 0:
                fw.op(act, lambda h: h.activation(out=l.GB.ap[:, jj, :], in_=pv, func=AF.Copy), reads=[ps], writes=[l.GB])
            elif grp == 1:
                fw.op(act, lambda h: h.activation(out=l.GC.ap[:, jj, :], in_=pv, func=AF.Copy), reads=[ps], writes=[l.GC])
            elif grp == 2:
                fw.op(dve, lambda h: h.tensor_tensor(out=c.PBt[:, e, jj, 2:2 + T], in0=pv, in1=l.GC.ap[:, jj, :], op=ALU.mult),
                      reads=[ps, l.GC], writes=[c.PB[e]])
            elif grp == 3:
                fw.op(act, lambda h: h.activation(out=l.UA.ap[:, jj, :], in_=pv, func=AF.Copy), reads=[ps], writes=[l.UA])
            else:
                sg = l.SG[l.cnt % 2]
                l.cnt += 1
                fw.op(act, lambda h: h.activation(out=sg.ap, in_=pv, func=AF.Sigmoid), reads=[ps], writes=[sg])
                fw.op(dve, lambda h: h.tensor_tensor(out=c.GLUt[:, e, jj, 30:30 + T], in0=sg.ap, in1=l.UA.ap[:, jj, :], op=ALU.mult),
                      reads=[sg, l.UA], writes=[c.GLU[e]])
        yield from linear_fm(cs, lambda c: c.l.H, KC, "a_w_in", e, 0, 2560, ev)
        for c in cs:
            l = c.l
            MIX = l.MIX
            Z = l.Z
            def build_dg(jj):
                dg = l.DG[jj % 2]
                wa = CPt[:, CPO["conv_a"] + (e * 4 + jj) * 3: CPO["conv_a"] + (e * 4 + jj) * 3 + 3]
                wb = CPt[:, CPO["conv_b"] + (e * 4 + jj) * 31: CPO["conv_b"] + (e * 4 + jj) * 31 + 31]
                fw.op(dve, lambda h: h.tensor_tensor(out=dg.ap[:, 0:3, :], in0=CSTt[:, 0:1, :].broadcast_to([128, 3, 128]),
                                                     in1=wa.unsqueeze(2).broadcast_to([128, 3, 128]), op=ALU.mult),
                      reads=[CST, CP], writes=[dg])
                fw.op(dve, lambda h: h.tensor_tensor(out=dg.ap[:, 3:34, :], in0=CSTt[:, 0:1, :].broadcast_to([128, 31, 128]),
                                                     in1=wb.unsqueeze(2).broadcast_to([128, 31, 128]), op=ALU.mult),
                      reads=[CST, CP], writes=[dg])
            build_dg(0)
            for jj in range(4):
                dg = l.DG[jj % 2]
                (ps,) = fw.psum(1)
                for k in range(3):
                    fw.op(pe, lambda h: h.matmul(ps.ap[:, 0:T], dg.ap[:, k, :], c.PBt[:, e, jj, k:k + T], start=(k == 0), stop=(k == 2)),
                          reads=[dg, c.PB[e]], writes=[ps])
                (ps2,) = fw.psum(1)
                for k in range(31):
                    fw.op(pe, lambda h: h.matmul(ps2.ap[:, 0:T], dg.ap[:, 3 + k, :], c.GLUt[:, e, jj, k:k + T], start=(k == 0), stop=(k == 30)),
                          reads=[dg, c.GLU[e]], writes=[ps2])
                if jj < 3:
                    build_dg(jj + 1)
                fw.op(dve, lambda h: h.tensor_tensor(out=MIX.ap[:, jj, :], in0=ps.ap[:, 0:T], in1=l.GB.ap[:, jj, :], op=ALU.mult),
                      reads=[ps, l.GB], writes=[MIX])
                bcol = cpcol("cb_bias", e * 4 + jj)
                fw.op(dve, lambda h: h.tensor_scalar(out=Z[jj].ap, in0=ps2.ap[:, 0:T], scalar1=bcol, scalar2=None, op0=ALU.add),
                      reads=[ps2, CP], writes=[Z[jj]])
            fw.op(dve, lambda h: h.tensor_copy(c.PBt[:, e, :, 0:2], c.PBt[:, e, :, T:T + 2]), reads=[c.PB[e]], writes=[c.PB[e]])
            fw.op(dve, lambda h: h.tensor_copy(c.GLUt[:, e, :, 0:30], c.GLUt[:, e, :, T:T + 30]), reads=[c.GLU[e]], writes=[c.GLU[e]])
            (pm,) = fw.psum(1)
            (pq,) = fw.psum(1)
            MU, VR = l.MU, l.VR
            for jj in range(4):
                zb = l.SQ[0]
                fw.op(act, lambda h: h.activation(out=zb.ap[:, 0:T], in_=Z[jj].ap, func=AF.Copy), reads=[Z[jj]], writes=[zb])
                fw.op(pe, lambda h: h.matmul(pm.ap[:, 0:T], ONESB, zb.ap[:, 0:T], start=(jj == 0), stop=(jj == 3)), reads=[zb, CSB], writes=[pm])
                zq = l.SQ[1]
                fw.op(act, lambda h: h.activation(out=zq.ap[:, 0:T], in_=Z[jj].ap, func=AF.Square), reads=[Z[jj]], writes=[zq])
                fw.op(pe, lambda h: h.matmul(pq.ap[:, 0:T], ONESB, zq.ap[:, 0:T], start=(jj == 0), stop=(jj == 3)), reads=[zq, CSB], writes=[pq])
            fw.op(act, lambda h: h.activation(out=MU.ap, in_=pm.ap[:, 0:T], func=AF.Copy, scale=1.0 / 512), reads=[pm], writes=[MU])
            fw.op(dve, lambda h: h.tensor_tensor(out=VR.ap, in0=MU.ap, in1=MU.ap, op=ALU.mult), reads=[MU], writes=[VR])
            fw.op(dve, lambda h: h.scalar_tensor_tensor(out=VR.ap, in0=pq.ap[:, 0:T], scalar=1.0 / 512, in1=VR.ap, op0=ALU.mult,
                                                        op1=ALU.subtract), reads=[pq, VR], writes=[VR])
            fw.op(dve, lambda h: h.tensor_scalar(out=VR.ap, in0=VR.ap, scalar1=EPS, scalar2=None, op0=ALU.add), reads=[VR], writes=[VR])
            fw.op(act, lambda h: h.activation(out=VR.ap, in_=VR.ap, func=AF.Sqrt), reads=[VR], writes=[VR])
            fw.op(dve, lambda h: h.reciprocal(out=VR.ap, in_=VR.ap), reads=[VR], writes=[VR])
            for jj in range(4):
                fw.op(dve, lambda h: h.tensor_tensor(out=Z[jj].ap, in0=Z[jj].ap, in1=MU.ap, op=ALU.subtract), reads=[Z[jj], MU], writes=[Z[jj]])
                fw.op(dve, lambda h: h.tensor_tensor(out=Z[jj].ap, in0=Z[jj].ap, in1=VR.ap, op=ALU.mult), reads=[Z[jj], VR], writes=[Z[jj]])
                gcol = cpcol("ln_g", e * 4 + jj)
                bcol = cpcol("ln_b", e * 4 + jj)
                fw.op(act, lambda h: h.activation(out=MIX.ap[:, 4 + jj, :], in_=Z[jj].ap, func=AF.Silu, bias=bcol, scale=gcol),
                      reads=[Z[jj], CP], writes=[MIX])
        yield from outproj_postnorm(cs, layer, 1, "a_w_out", e)

    def attn_sublayer(cs, layer):
        for c in cs:
            l = common_alloc(c)
            ar = c.ar
            l.Q = ar.alloc("Q", [KC, T], BF16)
            l.PT = [ar.alloc("PT%d" % i, [2, T], BF16) for i in range(2)]
            l.RC = [ar.alloc("RC%d" % i, [T], F32) for i in range(2)]
            l.KTb = ar.alloc("KTb%d" % c.s, [KC, NMEM], BF16)
            l.Vb = ar.alloc("Vb%d" % c.s, [2, D], BF16)
            fw.dma(sp, l.KTb.ap.rearrange("p a b -> p (a b)"), kvd[c.s, layer, :, 0:2048], reads=[c.KVD[layer]], writes=[l.KTb])
            fw.dma(sp, l.Vb.ap.rearrange("p a b -> p (a b)"), kvd[c.s, layer, :, 2048:4096], reads=[c.KVD[layer]], writes=[l.Vb])
            prenorm(c, layer, 2)

        def evq(c, j, ps):
            fw.op(act, lambda h: h.activation(out=c.l.Q.ap[:, j, :], in_=ps.ap[:, 0:T], func=AF.Copy), reads=[ps], writes=[c.l.Q])
        yield from linear_fm(cs, lambda c: c.l.H, KC, "x_w_q", layer, 0, D, evq)
        for hd in range(4):
            for c in cs:
                l = c.l
                Q, MIX = l.Q, l.MIX
                pt = l.PT[hd % 2]
                rc = l.RC[hd % 2]
                for mc in range(2):
                    (ps,) = fw.psum(1)
                    for dj in range(2):
                        fw.op(pe, lambda h: h.matmul(ps.ap[:, 0:T], l.KTb.ap[:, 2 * hd + dj, mc * 128:(mc + 1) * 128], Q.ap[:, 2 * hd + dj, :],
                                                     start=(dj == 0), stop=(dj == 1)), reads=[l.KTb, Q], writes=[ps])
                    fw.op(act, lambda h: h.activation(out=pt.ap[:, mc, :], in_=ps.ap[:, 0:T], func=AF.Exp, scale=1.0 / 16.0),
                          reads=[ps], writes=[pt])
                (pd,) = fw.psum(1)
                for mc in range(2):
                    fw.op(pe, lambda h: h.matmul(pd.ap[:, 0:T], ONESB, pt.ap[:, mc, :], start=(mc == 0), stop=(mc == 1)),
                          reads=[pt, CSB], writes=[pd])
                fw.op(dve, lambda h: h.reciprocal(out=rc.ap, in_=pd.ap[:, 0:T]), reads=[pd], writes=[rc])
                for dj in range(2):
                    (po,) = fw.psum(1)
                    for mc in range(2):
                        fw.op(pe, lambda h: h.matmul(po.ap[:, 0:T], l.Vb.ap[:, mc, (2 * hd + dj) * 128:(2 * hd + dj + 1) * 128], pt.ap[:, mc, :],
                                                     start=(mc == 0), stop=(mc == 1)), reads=[l.Vb, pt], writes=[po])
                    fw.op(dve, lambda h: h.tensor_tensor(out=MIX.ap[:, 2 * hd + dj, :], in0=po.ap[:, 0:T], in1=rc.ap, op=ALU.mult),
                          reads=[po, rc], writes=[MIX])
        yield from outproj_postnorm(cs, layer, 3, "x_w_o", layer)

    def ffn_sublayer(cs, layer):
        for c in cs:
            l = common_alloc(c)
            l.A = c.ar.alloc("A", [22, T], BF16)
            l.SG = [c.ar.alloc("SG%d" % i, [T], F32) for i in range(2)]
            prenorm(c, layer, 4)
        for cc in range(11):
            slot, v = yield ("f_w_gu", layer, 0, KC, ((cc * 256, 256), (DFF + cc * 256, 256)))
            for jj in range(2):
                for c in cs:
                    l = c.l
                    H = l.H
                    (pg,) = fw.psum(1)
                    (pu,) = fw.psum(1)
                    for k in range(KC):
                        fw.op(pe, lambda h: h.matmul(pg.ap[:, 0:T], v[:, k, jj * 128:(jj + 1) * 128], H.ap[:, k, :],
                                                     start=(k == 0), stop=(k == KC - 1)), reads=[slot, H], writes=[pg])
                    for k in range(KC):
                        fw.op(pe, lambda h: h.matmul(pu.ap[:, 0:T], v[:, k, 256 + jj * 128:256 + (jj + 1) * 128], H.ap[:, k, :],
                                                     start=(k == 0), stop=(k == KC - 1)), reads=[slot, H], writes=[pu])
                    sg = l.SG[jj]
                    fw.op(act, lambda h: h.activation(out=sg.ap, in_=pg.ap[:, 0:T], func=AF.Silu), reads=[pg], writes=[sg])
                    fw.op(dve, lambda h: h.tensor_tensor(out=l.A.ap[:, 2 * cc + jj, :], in0=pu.ap[:, 0:T], in1=sg.ap, op=ALU.mult),
                          reads=[pu, sg], writes=[l.A])
        for m in range(8):
            slot, v = yield ("f_w_down", layer, 0, 22, ((m * 128, 128),))
            for c in cs:
                A = c.l.A
                (pb,) = fw.psum(1)
                for kc in range(22):
                    fw.op(pe, lambda h: h.matmul(pb.ap[:, 0:T], v[:, kc, :], A.ap[:, kc, :], start=(kc == 0), stop=(kc == 21)),
                          reads=[slot, A], writes=[pb])
                evac_y(c, m, pb, layer, 5)
        for c in cs:
            postnorm(c, layer, 5)

    def mlstm_sublayer(cs, layer):
        o = layer // 2
        W = "m_w_in"
        for c in cs:
            l = common_alloc(c)
            ar = c.ar
            l.Q = ar.alloc("Q", [KC, T], BF16)
            l.KT = ar.alloc("KT", [TC, D], BF16)
            l.KTT = ar.alloc("KTT", [KC, T], BF16)
            l.VA = ar.alloc("VA", [TC, 8, 129], BF16)
            l.SO = ar.alloc("SO", [TC, D], BF16)
            l.GT = ar.alloc("GT", [TC, 16], F32)
            l.LL = ar.alloc("LL", [TC, 8], F32)
            l.EQ = ar.alloc("EQ", [TC, 8], F32)
            l.EK = ar.alloc("EK", [TC, 8], F32)
            l.GG = ar.alloc("GG", [TC, 8], F32)
            l.WT = [ar.alloc("WT%d" % i, [8, 128], BF16) for i in range(2)]
            l.HH = ar.alloc("HH", [8, 128], F32)
            l.H2 = ar.alloc("H2", [8, 128], F32)
            l.YT = ar.alloc("YT", [8, 128], BF16)
            l.CB = ar.alloc("CB", [8, 129], BF16)
            l.ST = [ar.alloc("ST%d" % i, [8], F32) for i in range(6)]
            prenorm(c, layer, 0)

        def tm_linear(col0, ncols, evac):
            slot, v = yield (W, o, 0, KC, ((col0, ncols),))
            for c in cs:
                H = c.l.H
                for tc in range(TC):
                    (ps,) = fw.psum(1)
                    for k in range(KC):
                        fw.op(pe, lambda h: h.matmul(ps.ap[:, 0:ncols], H.ap[:, k, tc * 128:(tc + 1) * 128], v[:, k, :],
                                                     start=(k == 0), stop=(k == KC - 1)), reads=[slot, H], writes=[ps])
                    evac(c, tc, ps)

        def ev_g(c, tc, ps):
            fw.op(dve, lambda h: h.tensor_tensor(out=c.l.GT.ap[:, tc, :], in0=ps.ap[:, 0:16], in1=RPt[:, o, :], op=ALU.add),
                  reads=[ps, RP], writes=[c.l.GT])
        yield from tm_linear(4 * D, 16, ev_g)
        for c in cs:
            l = c.l
            GT, LL, EQ, EK, GG = l.GT, l.LL, l.EQ, l.EK, l.GG
            fw.op(act, lambda h: h.activation(out=LL.ap, in_=GT.ap[:, :, 8:16], func=AF.Exp, scale=-1.0), reads=[GT], writes=[LL])
            fw.op(act, lambda h: h.activation(out=LL.ap, in_=LL.ap, func=AF.Ln, bias=1.0), reads=[LL], writes=[LL])
            (pc,) = fw.psum(1)
            (pg,) = fw.psum(1)
            for tc in range(TC):
                fw.op(pe, lambda h: h.matmul(pc.ap[:, tc * 8:(tc + 1) * 8], TRIF, LL.ap[:, tc, :], start=True, stop=True),
                      reads=[CST, LL], writes=[pc])
                fw.op(pe, lambda h: h.matmul(pg.ap[:, tc * 8:(tc + 1) * 8], ONESF, LL.ap[:, tc, :], start=True, stop=True),
                      reads=[CST, LL], writes=[pg])
            pcv = pc.ap[:, 0:TC * 8].rearrange("p (a b) -> p a b", b=8)
            pgv = pg.ap[:, 0:TC * 8].rearrange("p (a b) -> p a b", b=8)
            fw.op(act, lambda h: h.activation(out=EQ.ap, in_=pcv, func=AF.Exp, scale=-1.0), reads=[pc], writes=[EQ])
            fw.op(act, lambda h: h.activation(out=GG.ap, in_=pgv, func=AF.Exp, scale=-1.0), reads=[pg], writes=[GG])
            fw.op(dve, lambda h: h.tensor_tensor(out=EK.ap, in0=pcv, in1=GT.ap[:, :, 0:8], op=ALU.add), reads=[pc, GT], writes=[EK])
            fw.op(act, lambda h: h.activation(out=EK.ap, in_=EK.ap, func=AF.Exp), reads=[EK], writes=[EK])

        def evq(c, j, ps):
            fw.op(act, lambda h: h.activation(out=c.l.Q.ap[:, j, :], in_=ps.ap[:, 0:T], func=AF.Copy), reads=[ps], writes=[c.l.Q])
        yield from linear_fm(cs, lambda c: c.l.H, KC, W, o, 0, D, evq)
        for cc in range(2):
            def ev_k(c, tc, ps):
                l = c.l
                fw.op(dve, lambda h: h.scalar_tensor_tensor(
                    out=l.KT.ap[:, tc, cc * 512:(cc + 1) * 512].rearrange("p (a b) -> p a b", b=128),
                    in0=ps.ap.rearrange("p (a b) -> p a b", b=128), scalar=KSCALE,
                    in1=l.EK.ap[:, tc, cc * 4:(cc + 1) * 4].unsqueeze(2).broadcast_to([128, 4, 128]),
                    op0=ALU.mult, op1=ALU.mult), reads=[ps, l.EK], writes=[l.KT])
            yield from tm_linear(D + cc * 512, 512, ev_k)
        for c in cs:
            fw.op(dve, lambda h: h.memset(c.l.VA.ap[:, :, :, 128:129], 1.0), writes=[c.l.VA])
        for cc in range(2):
            def ev_v(c, tc, ps):
                fw.op(act, lambda h: h.activation(out=c.l.VA.ap[:, tc, cc * 4:(cc + 1) * 4, 0:128],
                                                  in_=ps.ap.rearrange("p (a b) -> p a b", b=128), func=AF.Copy),
                      reads=[ps], writes=[c.l.VA])
            yield from tm_linear(2 * D + cc * 512, 512, ev_v)
        for cc in range(2):
            def ev_o(c, tc, ps):
                fw.op(act, lambda h: h.activation(out=c.l.SO.ap[:, tc, cc * 512:(cc + 1) * 512], in_=ps.ap, func=AF.Sigmoid),
                      reads=[ps], writes=[c.l.SO])
            yield from tm_linear(3 * D + cc * 512, 512, ev_o)
        for c in cs:
            l = c.l
            for tc in range(TC):
                (pt,) = fw.psum(1)
                ptb = pt.ap.bitcast(BF16)
                for hd in range(8):
                    fw.op(pe, lambda h: h.transpose(ptb[:, hd * 128:(hd + 1) * 128], l.KT.ap[:, tc, hd * 128:(hd + 1) * 128], IDENTB),
                          reads=[l.KT, CSB], writes=[pt])
                fw.op(act, lambda h: h.activation(out=l.KTT.ap[:, :, tc * 128:(tc + 1) * 128],
                                                  in_=ptb.rearrange("p (a b) -> p a b", b=128), func=AF.Copy),
                      reads=[pt], writes=[l.KTT])
            fw.op(act, lambda h: h.activation(out=l.CB.ap, in_=c.C32t[:, o], func=AF.Copy), reads=[c.C32[o]], writes=[l.CB])
        for tc in range(TC):
            for c in cs:
                chunk(c, o, tc)
        yield from outproj_postnorm(cs, layer, 1, "m_w_out", o)

    def chunk(c, o, tc):
        l = c.l
        Q, KT, KTT, VA, SO, EQ, GG, HH, H2, YT, CB, MIX = l.Q, l.KT, l.KTT, l.VA, l.SO, l.EQ, l.GG, l.HH, l.H2, l.YT, l.CB, l.MIX
        C32t, C32b = c.C32t, c.C32[o]
        sl = slice(tc * 128, (tc + 1) * 128)
        wt = l.WT[tc % 2]
        pss = fw.psum(2)
        for hd in range(8):
            b = pss[hd // 4]
            fw.op(pe, lambda h: h.matmul(b.ap[:, (hd % 4) * 128:(hd % 4 + 1) * 128], KTT.ap[:, hd, sl], Q.ap[:, hd, sl],
                                         start=True, stop=True), reads=[KTT, Q], writes=[b])
        for g2 in range(2):
            fw.op(dve, lambda h: h.tensor_tensor(out=wt.ap[:, g2 * 4:(g2 + 1) * 4, :],
                                                 in0=pss[g2].ap.rearrange("p (a b) -> p a b", b=128),
                                                 in1=CSTt[:, 1:2, :].broadcast_to([128, 4, 128]), op=ALU.mult),
                  reads=[pss[g2], CST], writes=[wt])
        psp = fw.psum(3)
        psn = fw.psum(3)
        for hd in range(8):
            bp = psp[hd // 3]
            cs_ = slice((hd % 3) * 129, (hd % 3 + 1) * 129)
            fw.op(pe, lambda h: h.matmul(bp.ap[:, cs_], KT.ap[:, tc, hd * 128:(hd + 1) * 128], VA.ap[:, tc, hd, :],
                                         start=True, stop=True), reads=[KT, VA], writes=[bp])
        for hd in range(8):
            bn = psn[hd // 3]
            cs_ = slice((hd % 3) * 129, (hd % 3 + 1) * 129)
            fw.op(pe, lambda h: h.matmul(bn.ap[:, cs_], Q.ap[:, hd, sl], CB.ap[:, hd, :], start=True, stop=False),
                  reads=[Q, CB], writes=[bn])
            fw.op(pe, lambda h: h.matmul(bn.ap[:, cs_], wt.ap[:, hd, :], VA.ap[:, tc, hd, :], start=False, stop=True),
                  reads=[wt, VA], writes=[bn])
        for g3 in range(3):
            nh = 3 if g3 < 2 else 2
            hs = slice(g3 * 3, g3 * 3 + nh)
            fw.op(dve, lambda h: h.tensor_tensor(out=C32t[:, o, hs, :], in0=psp[g3].ap[:, 0:nh * 129].rearrange("p (a b) -> p a b", b=129),
                                                 in1=C32t[:, o, hs, :], op=ALU.add), reads=[psp[g3], C32b], writes=[C32b])
        fw.op(dve, lambda h: h.tensor_tensor(out=C32t[:, o], in0=C32t[:, o],
                                             in1=GG.ap[:, tc, :].unsqueeze(2).broadcast_to([128, 8, 129]), op=ALU.mult),
              reads=[C32b, GG], writes=[C32b])
        fw.op(act, lambda h: h.activation(out=CB.ap, in_=C32t[:, o], func=AF.Copy), reads=[C32b], writes=[CB])
        DN, RR, S1, S2, MUh, RS = l.ST
        for g3 in range(3):
            nh = 3 if g3 < 2 else 2
            hs = slice(g3 * 3, g3 * 3 + nh)
            v3 = psn[g3].ap[:, 0:nh * 129].rearrange("p (a b) -> p a b", b=129)
            fw.op(act, lambda h: h.activation(out=DN.ap[:, hs].unsqueeze(2), in_=v3[:, :, 128:129], func=AF.Abs),
                  reads=[psn[g3]], writes=[DN])
        fw.op(dve, lambda h: h.tensor_tensor(out=DN.ap, in0=DN.ap, in1=EQ.ap[:, tc, :], op=ALU.mult), reads=[DN, EQ], writes=[DN])
        fw.op(dve, lambda h: h.tensor_scalar(out=DN.ap, in0=DN.ap, scalar1=1.0, scalar2=None, op0=ALU.max), reads=[DN], writes=[DN])
        fw.op(dve, lambda h: h.reciprocal(out=DN.ap, in_=DN.ap), reads=[DN], writes=[DN])
        fw.op(dve, lambda h: h.tensor_tensor(out=RR.ap, in0=EQ.ap[:, tc, :], in1=DN.ap, op=ALU.mult), reads=[DN, EQ], writes=[RR])
        for g3 in range(3):
            nh = 3 if g3 < 2 else 2
            hs = slice(g3 * 3, g3 * 3 + nh)
            v3 = psn[g3].ap[:, 0:nh * 129].rearrange("p (a b) -> p a b", b=129)
            fw.op(dve, lambda h: h.tensor_tensor(out=HH.ap[:, hs, :], in0=v3[:, :, 0:128],
                                                 in1=RR.ap[:, hs].unsqueeze(2).broadcast_to([128, nh, 128]), op=ALU.mult),
                  reads=[psn[g3], RR], writes=[HH])
        fw.op(dve, lambda h: h.tensor_reduce(out=S1.ap, in_=HH.ap, axis=AX.X, op=ALU.add), reads=[HH], writes=[S1])
        fw.op(act, lambda h: h.activation(out=H2.ap, in_=HH.ap, func=AF.Square), reads=[HH], writes=[H2])
        fw.op(dve, lambda h: h.tensor_reduce(out=S2.ap, in_=H2.ap, axis=AX.X, op=ALU.add), reads=[H2], writes=[S2])
        fw.op(dve, lambda h: h.tensor_scalar(out=MUh.ap, in0=S1.ap, scalar1=1.0 / 128, scalar2=None, op0=ALU.mult), reads=[S1], writes=[MUh])
        fw.op(dve, lambda h: h.tensor_tensor(out=S1.ap, in0=MUh.ap, in1=MUh.ap, op=ALU.mult), reads=[MUh], writes=[S1])
        fw.op(dve, lambda h: h.scalar_tensor_tensor(out=RS.ap, in0=S2.ap, scalar=1.0 / 128, in1=S1.ap, op0=ALU.mult, op1=ALU.subtract),
              reads=[S2, S1], writes=[RS])
        fw.op(dve, lambda h: h.tensor_scalar(out=RS.ap, in0=RS.ap, scalar1=EPS, scalar2=None, op0=ALU.add), reads=[RS], writes=[RS])
        fw.op(act, lambda h: h.activation(out=RS.ap, in_=RS.ap, func=AF.Sqrt), reads=[RS], writes=[RS])
        fw.op(dve, lambda h: h.reciprocal(out=RS.ap, in_=RS.ap), reads=[RS], writes=[RS])
        fw.op(dve, lambda h: h.tensor_tensor(out=HH.ap, in0=HH.ap, in1=MUh.ap.unsqueeze(2).broadcast_to([128, 8, 128]), op=ALU.subtract),
              reads=[HH, MUh], writes=[HH])
        fw.op(dve, lambda h: h.tensor_tensor(out=HH.ap, in0=HH.ap, in1=RS.ap.unsqueeze(2).broadcast_to([128, 8, 128]), op=ALU.mult),
              reads=[HH, RS], writes=[HH])
        fw.op(dve, lambda h: h.tensor_tensor(out=YT.ap, in0=HH.ap, in1=SO.ap[:, tc, :].rearrange("p (a b) -> p a b", b=128), op=ALU.mult),
              reads=[HH, SO], writes=[YT])
        (py,) = fw.psum(1)
        pyb = py.ap.bitcast(BF16)
        for hd in range(8):
            fw.op(pe, lambda h: h.transpose(pyb[:, hd * 128:(hd + 1) * 128], YT.ap[:, hd, :], IDENTB), reads=[YT, CSB], writes=[py])
        mg = CPt[:, CPO["m_g"] + o * 8: CPO["m_g"] + o * 8 + 8]
        fw.op(dve, lambda h: h.tensor_tensor(out=MIX.ap[:, :, sl], in0=pyb.rearrange("p (a b) -> p a b", b=128),
                                             in1=mg.unsqueeze(2).broadcast_to([128, 8, 128]), op=ALU.mult),
              reads=[py, CP], writes=[MIX])

    def ctx_program(c):
        cs = [c]
        for o in range(2):
            fw.op(dve, lambda h: h.memset(c.C32t[:, o], 0.0), writes=[c.C32[o]])
        for e in range(2):
            fw.op(dve, lambda h: h.memset(c.GLUt[:, e], 0.0), writes=[c.GLU[e]])
            fw.op(dve, lambda h: h.memset(c.PBt[:, e], 0.0), writes=[c.PB[e]])
        for layer in range(DEPTH):
            yield from kv_prep(cs, layer)
        for t in range(NT):
            fw.dma(sp, c.Xt[:], io["x"][c.s][:, t * T:(t + 1) * T].rearrange("(k p) t -> p k t", p=128), writes=[c.X])
            for layer in range(DEPTH):
                if layer % 2 == 0:
                    yield from conv_sublayer(cs, layer)
                else:
                    yield from mlstm_sublayer(cs, layer)
                yield from attn_sublayer(cs, layer)
                yield from ffn_sublayer(cs, layer)
            fw.dma(sp, io["y"][c.s][:, t * T:(t + 1) * T].rearrange("(k p) t -> p k t", p=128), c.Xt[:], reads=[c.X])

    run = ctxs[:1] if dry else ctxs
    gens = [ctx_program(c) for c in run]
    n = len(gens)
    nxt = [None] * n
    blk = [0] * n
    done = [False] * n
    for i, g in enumerate(gens):
        try:
            nxt[i] = next(g)
        except StopIteration:
            done[i] = True
    while not all(done):
        if n == 1 or done[1]:
            i = 0
        elif done[0]:
            i = 1
        else:
            i = 0 if (blk[0] - blk[1]) < SKEW else 1
        bmin = min(blk[j] for j in range(n) if not done[j])
        slot, v = wq.get(blk[i], nxt[i], bmin)
        blk[i] += 1
        try:
            nxt[i] = gens[i].send((slot, v))
        except StopIteration:
            done[i] = True
    fw.wait_all(sp, [c.X for c in ctxs])
    return fw


def build_nc(NSEQ=2, NT=8, DEPTH=4):
    S = NT * T
    nc = bass.Bass("TRN2", target_bir_lowering=False)

    def din(name, shape):
        return nc.dram_tensor(name, shape, F32, kind="ExternalInput").ap()
    io = {}
    io["x"] = din("x", [NSEQ, D, S])
    io["mem"] = din("mem", [NSEQ, D, NMEM])
    io["consts"] = din("consts", [128, 3, 128])
    io["cp"] = din("cp", [128, NCP])
    io["rp"] = din("rp", [128, 2, 16])
    io["w"] = {
        "a_w_in": din("a_w_in", [2, D, 2560]), "a_w_out": din("a_w_out", [2, D, D]),
        "m_w_in": din("m_w_in", [2, D, 4112]), "m_w_out": din("m_w_out", [2, D, D]),
        "x_w_q": din("x_w_q", [4, D, D]), "x_w_kv": din("x_w_kv", [4, D, 2 * D]), "x_w_o": din("x_w_o", [4, D, D]),
        "f_w_gu": din("f_w_gu", [4, D, 2 * DFF]), "f_w_down": din("f_w_down", [4, DFF, D]),
    }
    io["y"] = nc.dram_tensor("y", [NSEQ, D, S], F32, kind="ExternalOutput").ap()
    io["kvd"] = nc.dram_tensor("kvd", [NSEQ, 4, 128, 4096], BF16, kind="Internal").ap()
    plan = []
    with contextlib.ExitStack() as es:
        _emit(nc, es, True, plan, NSEQ, NT, DEPTH, io)
    es2 = contextlib.ExitStack()
    with es2:
        fw = _emit(nc, es2, False, plan, NSEQ, NT, DEPTH, io)
    return nc, fw


def host_tables(inp):
    cp = np.zeros((128, NCP), np.float32)

    def put(name, idx, vec128):
        cp[:, CPO[name] + idx] = vec128
    ng = np.asarray(inp["norm_g"], np.float32)
    for l in range(4):
        for j in range(6):
            for k in range(8):
                put("norm_g", (l * 6 + j) * 8 + k, ng[l, j, k * 128:(k + 1) * 128])
    mg = np.asarray(inp["mem_norm_g"], np.float32)
    for l in range(4):
        for k in range(8):
            put("mem_g", l * 8 + k, mg[l, k * 128:(k + 1) * 128])
    ca = np.asarray(inp["a_conv_a"], np.float32)
    cb = np.asarray(inp["a_conv_b"], np.float32)
    cbb = np.asarray(inp["a_conv_b_bias"], np.float32)
    lg = np.asarray(inp["a_ln_g"], np.float32)
    lb = np.asarray(inp["a_ln_b"], np.float32)
    for e in range(2):
        for c in range(4):
            for k in range(3):
                put("conv_a", (e * 4 + c) * 3 + k, ca[e, k, c * 128:(c + 1) * 128])
            for k in range(31):
                put("conv_b", (e * 4 + c) * 31 + k, cb[e, k, c * 128:(c + 1) * 128])
            put("cb_bias", e * 4 + c, cbb[e, c * 128:(c + 1) * 128])
            put("ln_g", e * 4 + c, lg[e, c * 128:(c + 1) * 128])
            put("ln_b", e * 4 + c, lb[e, c * 128:(c + 1) * 128])
    mng = np.asarray(inp["m_norm_g"], np.float32)
    for o in range(2):
        for k in range(8):
            put("m_g", o * 8 + k, mng[o, k * 128:(k + 1) * 128])
    rp = np.zeros((128, 2, 16), np.float32)
    rp[:, :, 0:8] = np.asarray(inp["m_i_bias"], np.float32)[None]
    rp[:, :, 8:16] = np.asarray(inp["m_f_bias"], np.float32)[None]
    consts = np.zeros((128, 3, 128), np.float32)
    consts[:, 0, :] = np.eye(128, dtype=np.float32)
    consts[:, 1, :] = np.triu(np.ones((128, 128), np.float32))
    consts[:, 2, :] = 1.0
    return cp, rp, consts


WNAMES = ["a_w_in", "a_w_out", "m_w_in", "m_w_out", "x_w_q", "x_w_kv", "x_w_o", "f_w_gu", "f_w_down"]


def kernel(**inp):
    x = np.asarray(inp["x"], np.float32)
    mem = np.asarray(inp["mem"], np.float32)
    B = x.shape[0]
    nseq = B // NCORES
    cp, rp, consts = host_tables(inp)
    nc, _ = build_nc(NSEQ=nseq, NT=x.shape[1] // T, DEPTH=4)
    shared = {"consts": consts, "cp": cp, "rp": rp}
    for w in WNAMES:
        shared[w] = np.ascontiguousarray(np.asarray(inp[w], np.float32))
    in_maps = []
    for c in range(NCORES):
        m = dict(shared)
        m["x"] = np.ascontiguousarray(x[c * nseq:(c + 1) * nseq].transpose(0, 2, 1))
        m["mem"] = np.ascontiguousarray(mem[c * nseq:(c + 1) * nseq].transpose(0, 2, 1))
        in_maps.append(m)
    res = run_bass_kernel_spmd(nc, in_maps, core_ids=list(range(NCORES)))
    out = np.empty_like(x)
    for c in range(NCORES):
        y = res.results[c]["y"]
        out[c * nseq:(c + 1) * nseq] = y.transpose(0, 2, 1)
    return out
```

```python
import contextlib
import numpy as np
import concourse.bass as bass
import concourse.mybir as mybir
from concourse.bass_utils import run_bass_kernel_spmd

F32 = mybir.dt.float32
BF16 = mybir.dt.bfloat16
AF = mybir.ActivationFunctionType
ALU = mybir.AluOpType
AX = mybir.AxisListType

D = 1024
KC = 8
T = 256
NMEM = 256
DFF = 2816
SEQ = 4096
NCORES = 8
EPS = 1e-6
NRING = 5
SKEW = 2
SLOT = 4096
KSCALE = 128.0 ** -0.5

def _cp_layout():
    off = {}
    n = 0
    for name, cnt in (("norm_g", 4 * 6 * 8), ("mem_g", 4 * 8), ("conv_a", 2 * 4 * 3), ("conv_b", 2 * 4 * 31),
                      ("cb_bias", 2 * 4), ("ln_g", 2 * 4), ("ln_b", 2 * 4), ("m_g", 2 * 8)):
        off[name] = n
        n += cnt
    return off, n


CPO, NCP = _cp_layout()


class Ctr:
    LIMIT = 16000

    def __init__(self, fw, name, owner=None):
        self.fw, self.name, self.owner, self.k, self.val = fw, name, owner, 0, 0
        self.sem = fw.new_sem(name + "_0")

    def bump(self, inc):
        if self.val + inc > self.LIMIT:
            self.k += 1
            self.sem = self.fw.new_sem("%s_%d" % (self.name, self.k))
            self.val = 0
        self.val += inc
        return (self.sem, self.val, self.owner)


class Eng:
    def __init__(self, fw, name, h, selfsync=True):
        self.h, self.name, self.selfsync = h, name, selfsync
        self.ctr = Ctr(fw, name, self)
        self.waited = {}


class Buf:
    def __init__(self, name, ap=None):
        self.name, self.ap = name, ap
        self.w = None
        self.r = {}
        self.dctr = None

    def deps(self):
        d = list(self.r.values())
        if self.w is not None:
            d.append(self.w)
        return d


class FW:
    def __init__(self, nc, es, dry):
        self.nc, self.es, self.dry = nc, es, dry
        self.nsem = 0
        self.nops = 0
        self.dctrs = {}
        if not dry:
            self.pe = Eng(self, "pe", nc.tensor, selfsync=False)
            self.act = Eng(self, "act", nc.scalar)
            self.dve = Eng(self, "dve", nc.vector)
            self.pool = Eng(self, "pool", nc.gpsimd)
            self.sp = Eng(self, "sp", nc.sync)
        else:
            self.pe = self.act = self.dve = self.pool = self.sp = None
        self.psbanks = None
        self.psptr = 0

    def new_sem(self, name):
        if self.dry:
            return None
        self.nsem += 1
        return self.es.enter_context(self.nc.semaphore(name))

    def _wait(self, eng, deps):
        for (sem, val, owner) in deps:
            if owner is eng and not eng.selfsync:
                continue
            k = id(sem)
            if eng.waited.get(k, 0) < val:
                eng.h.wait_ge(sem, val)
                eng.waited[k] = val

    def _deps(self, reads, writes):
        deps = []
        for b in reads:
            if b.w is not None:
                deps.append(b.w)
        for b in writes:
            deps.extend(b.deps())
        return deps

    def _commit(self, d, reads, writes):
        k = id(d[0])
        for b in reads:
            old = b.r.get(k)
            if old is None or old[1] < d[1]:
                b.r[k] = d
        for b in writes:
            b.w = d
            b.r = {}

    def op(self, eng, fn, reads=(), writes=()):
        if self.dry:
            return
        self._wait(eng, self._deps(reads, writes))
        ins = fn(eng.h)
        d = eng.ctr.bump(1)
        ins.then_inc(d[0], 1)
        self._commit(d, reads, writes)
        self.nops += 1

    def dma(self, eng, out, in_, reads=(), writes=()):
        if self.dry:
            return
        self._wait(eng, self._deps(reads, writes))
        ins = eng.h.dma_start(out=out, in_=in_)
        b = writes[0] if writes else reads[0]
        if b.dctr is None:
            if b.name not in self.dctrs:
                self.dctrs[b.name] = Ctr(self, "d_" + b.name, None)
            b.dctr = self.dctrs[b.name]
        d = b.dctr.bump(16)
        ins.then_inc(d[0], 16)
        self._commit(d, reads, writes)
        self.nops += 1

    def wait_all(self, eng, bufs):
        if self.dry:
            return
        deps = []
        for b in bufs:
            deps.extend(b.deps())
        self._wait(eng, deps)

    def psum(self, n=1):
        if self.psptr + n > 8:
            self.psptr = 0
        bs = self.psbanks[self.psptr:self.psptr + n]
        self.psptr = (self.psptr + n) % 8
        return bs


class Arena:
    def __init__(self, fw, tensor, nelem):
        self.fw, self.t, self.n = fw, tensor, nelem
        self.hist = []
        self.cur = []
        self.off = 0

    def reset(self):
        newh = list(self.cur)
        for (a, b, buf) in self.hist:
            if not any(a < cb and ca < b for (ca, cb, _) in self.cur):
                newh.append((a, b, buf))
        self.hist = newh
        self.cur = []
        self.off = 0

    def alloc(self, name, shape, dtype):
        n = 1
        for s in shape:
            n *= s
        nb = n * (2 if dtype == F32 else 1)
        nb = (nb + 15) // 16 * 16
        a, b = self.off, self.off + nb
        assert b <= self.n, "arena overflow %s %d > %d" % (name, b, self.n)
        self.off = b
        ap = self.t[:, a:a + n * (2 if dtype == F32 else 1)]
        if dtype == F32:
            ap = ap.bitcast(F32)
        if len(shape) == 2:
            ap = ap.rearrange("p (a b) -> p a b", b=shape[1])
        elif len(shape) == 3:
            ap = ap.rearrange("p (a b c) -> p a b c", b=shape[1], c=shape[2])
        buf = Buf(name, ap)
        for (ha, hb, hbuf) in self.hist:
            if ha < b and a < hb:
                for d in hbuf.deps():
                    k = id(d[0])
                    old = buf.r.get(k)
                    if old is None or old[1] < d[1]:
                        buf.r[k] = d
        self.cur.append((a, b, buf))
        return buf


class WQ:
    def __init__(self, fw, slots, wdram, plan):
        self.fw, self.slots, self.wdram = fw, slots, wdram
        self.plan = plan
        self.i = 0
        self.issued = 0

    def _view(self, slot, nkc, ncols):
        return slot.ap[:, 0:nkc * ncols].rearrange("p (k m) -> p k m", m=ncols)

    def _issue(self, j):
        (wname, li, row0, nkc, segs) = self.plan[j]
        slot = self.slots[j % NRING]
        ncols = sum(n for (_, n) in segs)
        v = self._view(slot, nkc, ncols)
        W = self.wdram[wname][li]
        off = 0
        for (c0, n) in segs:
            src = W[row0:row0 + nkc * 128, c0:c0 + n].rearrange("(k p) m -> p k m", p=128)
            self.fw.dma(self.fw.pool, v[:, :, off:off + n], src, writes=[slot])
            off += n

    def get(self, k, spec, bmin):
        (wname, li, row0, nkc, segs) = spec
        ncols = sum(n for (_, n) in segs)
        assert nkc * ncols <= SLOT
        if self.fw.dry:
            assert k == len(self.plan)
            self.plan.append(spec)
            return self.slots[0], self._view(self.slots[0], nkc, ncols)
        assert self.plan[k] == spec, (k, self.plan[k], spec)
        while self.issued < min(len(self.plan), bmin + NRING):
            self._issue(self.issued)
            self.issued += 1
        assert k < self.issued
        slot = self.slots[k % NRING]
        return slot, self._view(slot, nkc, ncols)


class Ctx:
    pass


class L:
    pass


def _emit(nc, es, dry, plan, NSEQ, NT, DEPTH, io):
    fw = FW(nc, es, dry)
    E = es.enter_context
    sfx = "d" if dry else "r"
    TC = T // 128

    def sb(name, shape, dt):
        return E(nc.sbuf_tensor(name + sfx, shape, dt))

    ring = [Buf("ring%d" % i, sb("ring%d" % i, [128, SLOT], BF16)[:]) for i in range(NRING)]
    CSTt = sb("CST", [128, 3, 128], F32)
    CSBt = sb("CSB", [128, 3, 128], BF16)
    CST = Buf("CST", CSTt[:])
    CSB = Buf("CSB", CSBt[:])
    CPt = sb("CP", [128, NCP], F32)
    CP = Buf("CP", CPt[:])
    RPt = sb("RP", [128, 2, 16], F32)
    RP = Buf("RP", RPt[:])
    NA = 28544
    ctxs = []
    for s in range(NSEQ):
        c = Ctx()
        c.s = s
        c.Xt = sb("X%d" % s, [128, KC, T], F32)
        c.X = Buf("X%d" % s, c.Xt[:])
        c.C32t = sb("C32_%d" % s, [128, 2, 8, 129], F32)
        c.C32 = [Buf("C32_%d_%d" % (s, o), c.C32t[:, o]) for o in range(2)]
        c.GLUt = sb("GLU%d" % s, [128, 2, 4, 30 + T], BF16)
        c.PBt = sb("PB%d" % s, [128, 2, 4, 2 + T], BF16)
        c.GLU = [Buf("GLU%d_%d" % (s, e), c.GLUt[:, e]) for e in range(2)]
        c.PB = [Buf("PB%d_%d" % (s, e), c.PBt[:, e]) for e in range(2)]
        c.ARt = sb("AR%d" % s, [128, NA], BF16)
        c.ar = Arena(fw, c.ARt, NA)
        c.KVD = [Buf("kvd%d_%d" % (s, l)) for l in range(4)]
        ctxs.append(c)
    PSt = E(nc.psum_tensor("PS" + sfx, [128, 8, 512], F32))
    fw.psbanks = [Buf("ps%d" % i, PSt[:, i, :]) for i in range(8)]
    kvd = io["kvd"]

    wq = WQ(fw, ring, io["w"], plan)
    pe, act, dve, pool, sp = fw.pe, fw.act, fw.dve, fw.pool, fw.sp

    IDENTB = CSBt[:, 0, :]
    ONESB = CSBt[:, 2, :]
    TRIF = CSTt[:, 1, :]
    ONESF = CSTt[:, 2, :]

    def cpcol(name, idx):
        cc = CPO[name] + idx
        return CPt[:, cc:cc + 1]

    fw.dma(sp, CSTt[:], io["consts"], writes=[CST])
    fw.dma(sp, CPt[:], io["cp"], writes=[CP])
    fw.dma(sp, RPt[:], io["rp"], writes=[RP])
    fw.op(dve, lambda h: h.tensor_copy(CSBt[:], CSTt[:]), reads=[CST], writes=[CSB])

    def rms_rstd(srcs, srcbufs, nk, ncols, RB, SQ, dn):
        (ps,) = fw.psum(1)
        for k in range(nk):
            sq = SQ[k % 2]
            fw.op(act, lambda h: h.activation(out=sq.ap[:, 0:ncols], in_=srcs[k], func=AF.Square),
                  reads=[srcbufs[k]], writes=[sq])
            fw.op(pe, lambda h: h.matmul(ps.ap[:, 0:ncols], ONESB, sq.ap[:, 0:ncols], start=(k == 0), stop=(k == nk - 1)),
                  reads=[sq, CSB], writes=[ps])
        fw.op(dve, lambda h: h.tensor_scalar(out=RB.ap[:, 0:ncols], in0=ps.ap[:, 0:ncols], scalar1=1.0 / dn,
                                             scalar2=EPS, op0=ALU.mult, op1=ALU.add), reads=[ps], writes=[RB])
        fw.op(act, lambda h: h.activation(out=RB.ap[:, 0:ncols], in_=RB.ap[:, 0:ncols], func=AF.Sqrt),
              reads=[RB], writes=[RB])
        fw.op(dve, lambda h: h.reciprocal(out=RB.ap[:, 0:ncols], in_=RB.ap[:, 0:ncols]), reads=[RB], writes=[RB])

    def common_alloc(c):
        l = L()
        ar = c.ar
        ar.reset()
        l.H = ar.alloc("H", [KC, T], BF16)
        l.SQ = [ar.alloc("SQ%d" % i, [NMEM], BF16) for i in range(2)]
        l.RB = ar.alloc("RB", [NMEM], F32)
        l.RB2 = ar.alloc("RB2", [T], F32)
        l.YA = ar.alloc("YA", [KC, T], F32)
        l.SQA = ar.alloc("SQA", [KC, T], BF16)
        l.MIX = l.H
        c.l = l
        return l

    def rstd_from_ps(ps, RB):
        fw.op(act, lambda h: h.activation(out=RB.ap[:, 0:T], in_=ps.ap[:, 0:T], func=AF.Ln, scale=1.0 / D, bias=EPS),
              reads=[ps], writes=[RB])
        fw.op(act, lambda h: h.activation(out=RB.ap[:, 0:T], in_=RB.ap[:, 0:T], func=AF.Exp, scale=-0.5),
              reads=[RB], writes=[RB])

    def prenorm(c, layer, j):
        l = c.l
        fw.op(act, lambda h: h.activation(out=l.SQA.ap, in_=c.Xt[:], func=AF.Square), reads=[c.X], writes=[l.SQA])
        (ps,) = fw.psum(1)
        for k in range(KC):
            fw.op(pe, lambda h: h.matmul(ps.ap[:, 0:T], ONESB, l.SQA.ap[:, k, :], start=(k == 0), stop=(k == KC - 1)),
                  reads=[l.SQA, CSB], writes=[ps])
        rstd_from_ps(ps, l.RB)
        for k in range(KC):
            g = cpcol("norm_g", (layer * 6 + j) * 8 + k)
            fw.op(dve, lambda h: h.scalar_tensor_tensor(out=l.H.ap[:, k, :], in0=c.Xt[:, k, :], scalar=g,
                                                        in1=l.RB.ap[:, 0:T], op0=ALU.mult, op1=ALU.mult),
                  reads=[c.X, l.RB, CP], writes=[l.H])

    def linear_fm(cs, getsrc, nk, wname, li, col0, ncols_total, evac, blk=512):
        j = 0
        for cc in range(0, ncols_total, blk):
            n = min(blk, ncols_total - cc)
            slot, v = yield (wname, li, 0, nk, ((col0 + cc, n),))
            for m in range(n // 128):
                for c in cs:
                    src = getsrc(c)
                    (ps,) = fw.psum(1)
                    for k in range(nk):
                        fw.op(pe, lambda h: h.matmul(ps.ap[:, 0:T], v[:, k, m * 128:(m + 1) * 128], src.ap[:, k, :],
                                                     start=(k == 0), stop=(k == nk - 1)),
                              reads=[slot, src], writes=[ps])
                    evac(c, j, ps)
                j += 1

    def evac_y(c, j, ps, layer, jg):
        l = c.l
        g = cpcol("norm_g", (layer * 6 + jg) * 8 + j)
        sq = l.SQ[j % 2]
        fw.op(act, lambda h: h.activation(out=l.YA.ap[:, j, :], in_=ps.ap[:, 0:T], func=AF.Copy, scale=g),
              reads=[ps, CP], writes=[l.YA])
        fw.op(act, lambda h: h.activation(out=sq.ap[:, 0:T], in_=ps.ap[:, 0:T], func=AF.Square), reads=[ps], writes=[sq])
        flush_stats(c)

        def deferred():
            (pst,) = fw.psum(1)
            fw.op(pe, lambda h: h.matmul(pst.ap[:, 0:T], ONESB, sq.ap[:, 0:T], start=True, stop=True), reads=[sq, CSB], writes=[pst])
            if j == 0:
                fw.op(dve, lambda h: h.tensor_copy(l.RB2.ap, pst.ap[:, 0:T]), reads=[pst], writes=[l.RB2])
            else:
                fw.op(dve, lambda h: h.tensor_tensor(out=l.RB2.ap, in0=pst.ap[:, 0:T], in1=l.RB2.ap, op=ALU.add),
                      reads=[pst, l.RB2], writes=[l.RB2])
        l.pending = deferred

    def flush_stats(c):
        f = getattr(c.l, "pending", None)
        if f is not None:
            c.l.pending = None
            f()

    def postnorm(c, layer, jg):
        l = c.l
        flush_stats(c)
        rstd_from_ps(l.RB2, l.RB2)
        fw.op(dve, lambda h: h.tensor_tensor(out=l.YA.ap, in0=l.YA.ap, in1=l.RB2.ap.unsqueeze(1).broadcast_to([128, KC, T]), op=ALU.mult),
              reads=[l.YA, l.RB2], writes=[l.YA])
        fw.op(dve, lambda h: h.tensor_tensor(out=c.Xt[:], in0=c.Xt[:], in1=l.YA.ap, op=ALU.add), reads=[c.X, l.YA], writes=[c.X])

    def outproj_postnorm(cs, layer, jg, wname, li):
        def ev(c, j, ps):
            evac_y(c, j, ps, layer, jg)
        yield from linear_fm(cs, lambda c: c.l.MIX, KC, wname, li, 0, D, ev)
        for c in cs:
            postnorm(c, layer, jg)
        yield None
        yield None

    def kv_prep(cs, layer):
        for c in cs:
            ar = c.ar
            ar.reset()
            l = L()
            c.l = l
            l.MT = ar.alloc("MT%d" % c.s, [KC, NMEM], F32)
            l.HM = ar.alloc("HM", [KC, NMEM], BF16)
            l.SQ = [ar.alloc("SQ%d" % i, [NMEM], BF16) for i in range(2)]
            l.RB = ar.alloc("RB", [NMEM], F32)
            l.KTb = ar.alloc("KTs%d" % c.s, [KC, NMEM], BF16)
            l.Vb = ar.alloc("Vs%d" % c.s, [2, D], BF16)
            fw.dma(sp, l.MT.ap, io["mem"][c.s].rearrange("(k p) m -> p k m", p=128), writes=[l.MT])
            rms_rstd([l.MT.ap[:, k, :] for k in range(KC)], [l.MT] * KC, KC, NMEM, l.RB, l.SQ, float(D))
            for k in range(KC):
                g = cpcol("mem_g", layer * 8 + k)
                fw.op(dve, lambda h: h.scalar_tensor_tensor(out=l.HM.ap[:, k, :], in0=l.MT.ap[:, k, :], scalar=g,
                                                            in1=l.RB.ap[:, 0:NMEM], op0=ALU.mult, op1=ALU.mult),
                      reads=[l.MT, l.RB, CP], writes=[l.HM])
        for cc in range(2):
            slot, v = yield ("x_w_kv", layer, 0, KC, ((cc * 512, 512),))
            for m in range(4):
                for c in cs:
                    l = c.l
                    (ps,) = fw.psum(1)
                    for k in range(KC):
                        fw.op(pe, lambda h: h.matmul(ps.ap[:, 0:NMEM], v[:, k, m * 128:(m + 1) * 128], l.HM.ap[:, k, :],
                                                     start=(k == 0), stop=(k == KC - 1)), reads=[slot, l.HM], writes=[ps])
                    fw.op(act, lambda h: h.activation(out=l.KTb.ap[:, cc * 4 + m, :], in_=ps.ap[:, 0:NMEM], func=AF.Copy),
                          reads=[ps], writes=[l.KTb])
        for cc in range(2):
            slot, v = yield ("x_w_kv", layer, 0, KC, ((D + cc * 512, 512),))
            for tc in range(2):
                for c in cs:
                    l = c.l
                    (ps,) = fw.psum(1)
                    for k in range(KC):
                        fw.op(pe, lambda h: h.matmul(ps.ap, l.HM.ap[:, k, tc * 128:(tc + 1) * 128], v[:, k, :],
                                                     start=(k == 0), stop=(k == KC - 1)), reads=[slot, l.HM], writes=[ps])
                    fw.op(act, lambda h: h.activation(out=l.Vb.ap[:, tc, cc * 512:(cc + 1) * 512], in_=ps.ap, func=AF.Copy),
                          reads=[ps], writes=[l.Vb])
        for c in cs:
            l = c.l
            fw.dma(sp, kvd[c.s, layer, :, 0:2048], l.KTb.ap.rearrange("p a b -> p (a b)"), reads=[l.KTb], writes=[c.KVD[layer]])
            fw.dma(sp, kvd[c.s, layer, :, 2048:4096], l.Vb.ap.rearrange("p a b -> p (a b)"), reads=[l.Vb], writes=[c.KVD[layer]])

    def conv_sublayer(cs, layer):
        e = layer // 2
        for c in cs:
            l = common_alloc(c)
            ar = c.ar
            l.GB = ar.alloc("GB", [4, T], BF16)
            l.GC = ar.alloc("GC", [4, T], F32)
            l.UA = ar.alloc("UA", [4, T], F32)
            l.Z = [ar.alloc("Z%d" % j, [T], F32) for j in range(4)]
            l.SG = [ar.alloc("SG%d" % i, [T], F32) for i in range(2)]
            l.DG = [ar.alloc("DG%d" % i, [34, 128], BF16) for i in range(2)]
            l.MU = ar.alloc("MU", [T], F32)
            l.VR = ar.alloc("VR", [T], F32)
            l.cnt = 0
            prenorm(c, layer, 0)

        def ev(c, j, ps):
            l = c.l
            grp, jj = j // 4, j % 4
            pv = ps.ap[:, 0:T]
            if grp == 0:
                fw.op(act, lambda h: h.activation(out=l.GB.ap[:, jj, :], in_=pv, func=AF.Copy), reads=[ps], writes=[l.GB])
            elif grp == 1:
                fw.op(act, lambda h: h.activation(out=l.GC.ap[:, jj, :], in_=pv, func=AF.Copy), reads=[ps], writes=[l.GC])
            elif grp == 2:
                fw.op(dve, lambda h: h.tensor_tensor(out=c.PBt[:, e, jj, 2:2 + T], in0=pv, in1=l.GC.ap[:, jj, :], op=ALU.mult),
                      reads=[ps, l.GC], writes=[c.PB[e]])
            elif grp == 3:
                fw.op(act, lambda h: h.activation(out=l.UA.ap[:, jj, :], in_=pv, func=AF.Copy), reads=[ps], writes=[l.UA])
            else:
                sg = l.SG[l.cnt % 2]
                l.cnt += 1
                fw.op(act, lambda h: h.activation(out=sg.ap, in_=pv, func=AF.Sigmoid), reads=[ps], writes=[sg])
                fw.op(dve, lambda h: h.tensor_tensor(out=c.GLUt[:, e, jj, 30:30 + T], in0=sg.ap, in1=l.UA.ap[:, jj, :], op=ALU.mult),
                      reads=[sg, l.UA], writes=[c.GLU[e]])
        yield from linear_fm(cs, lambda c: c.l.H, KC, "a_w_in", e, 0, 2560, ev)
        for c in cs:
            l = c.l
            MIX = l.MIX
            Z = l.Z
            def build_dg(jj):
                dg = l.DG[jj % 2]
                wa = CPt[:, CPO["conv_a"] + (e * 4 + jj) * 3: CPO["conv_a"] + (e * 4 + jj) * 3 + 3]
                wb = CPt[:, CPO["conv_b"] + (e * 4 + jj) * 31: CPO["conv_b"] + (e * 4 + jj) * 31 + 31]
                fw.op(dve, lambda h: h.tensor_tensor(out=dg.ap[:, 0:3, :], in0=CSTt[:, 0:1, :].broadcast_to([128, 3, 128]),
                                                     in1=wa.unsqueeze(2).broadcast_to([128, 3, 128]), op=ALU.mult),
                      reads=[CST, CP], writes=[dg])
                fw.op(dve, lambda h: h.tensor_tensor(out=dg.ap[:, 3:34, :], in0=CSTt[:, 0:1, :].broadcast_to([128, 31, 128]),
                                                     in1=wb.unsqueeze(2).broadcast_to([128, 31, 128]), op=ALU.mult),
                      reads=[CST, CP], writes=[dg])
            build_dg(0)
            for jj in range(4):
                dg = l.DG[jj % 2]
                (ps,) = fw.psum(1)
                for k in range(3):
                    fw.op(pe, lambda h: h.matmul(ps.ap[:, 0:T], dg.ap[:, k, :], c.PBt[:, e, jj, k:k + T], start=(k == 0), stop=(k == 2)),
                          reads=[dg, c.PB[e]], writes=[ps])
                (ps2,) = fw.psum(1)
                for k in range(31):
                    fw.op(pe, lambda h: h.matmul(ps2.ap[:, 0:T], dg.ap[:, 3 + k, :], c.GLUt[:, e, jj, k:k + T], start=(k == 0), stop=(k == 30)),
                          reads=[dg, c.GLU[e]], writes=[ps2])
                if jj < 3:
                    build_dg(jj + 1)
                fw.op(dve, lambda h: h.tensor_tensor(out=MIX.ap[:, jj, :], in0=ps.ap[:, 0:T], in1=l.GB.ap[:, jj, :], op=ALU.mult),
                      reads=[ps, l.GB], writes=[MIX])
                bcol = cpcol("cb_bias", e * 4 + jj)
                fw.op(dve, lambda h: h.tensor_scalar(out=Z[jj].ap, in0=ps2.ap[:, 0:T], scalar1=bcol, scalar2=None, op0=ALU.add),
                      reads=[ps2, CP], writes=[Z[jj]])
            fw.op(dve, lambda h: h.tensor_copy(c.PBt[:, e, :, 0:2], c.PBt[:, e, :, T:T + 2]), reads=[c.PB[e]], writes=[c.PB[e]])
            fw.op(dve, lambda h: h.tensor_copy(c.GLUt[:, e, :, 0:30], c.GLUt[:, e, :, T:T + 30]), reads=[c.GLU[e]], writes=[c.GLU[e]])
            (pm,) = fw.psum(1)
            (pq,) = fw.psum(1)
            MU, VR = l.MU, l.VR
            for jj in range(4):
                zb = l.SQ[0]
                fw.op(act, lambda h: h.activation(out=zb.ap[:, 0:T], in_=Z[jj].ap, func=AF.Copy), reads=[Z[jj]], writes=[zb])
                fw.op(pe, lambda h: h.matmul(pm.ap[:, 0:T], ONESB, zb.ap[:, 0:T], start=(jj == 0), stop=(jj == 3)), reads=[zb, CSB], writes=[pm])
                zq = l.SQ[1]
                fw.op(act, lambda h: h.activation(out=zq.ap[:, 0:T], in_=Z[jj].ap, func=AF.Square), reads=[Z[jj]], writes=[zq])
                fw.op(pe, lambda h: h.matmul(pq.ap[:, 0:T], ONESB, zq.ap[:, 0:T], start=(jj == 0), stop=(jj == 3)), reads=[zq, CSB], writes=[pq])
            fw.op(act, lambda h: h.activation(out=MU.ap, in_=pm.ap[:, 0:T], func=AF.Copy, scale=1.0 / 512), reads=[pm], writes=[MU])
            fw.op(dve, lambda h: h.tensor_tensor(out=VR.ap, in0=MU.ap, in1=MU.ap, op=ALU.mult), reads=[MU], writes=[VR])
            fw.op(dve, lambda h: h.scalar_tensor_tensor(out=VR.ap, in0=pq.ap[:, 0:T], scalar=1.0 / 512, in1=VR.ap, op0=ALU.mult,
                                                        op1=ALU.subtract), reads=[pq, VR], writes=[VR])
            fw.op(dve, lambda h: h.tensor_scalar(out=VR.ap, in0=VR.ap, scalar1=EPS, scalar2=None, op0=ALU.add), reads=[VR], writes=[VR])
            fw.op(act, lambda h: h.activation(out=VR.ap, in_=VR.ap, func=AF.Sqrt), reads=[VR], writes=[VR])
            fw.op(dve, lambda h: h.reciprocal(out=VR.ap, in_=VR.ap), reads=[VR], writes=[VR])
            for jj in range(4):
                fw.op(dve, lambda h: h.tensor_tensor(out=Z[jj].ap, in0=Z[jj].ap, in1=MU.ap, op=ALU.subtract), reads=[Z[jj], MU], writes=[Z[jj]])
                fw.op(dve, lambda h: h.tensor_tensor(out=Z[jj].ap, in0=Z[jj].ap, in1=VR.ap, op=ALU.mult), reads=[Z[jj], VR], writes=[Z[jj]])
                gcol = cpcol("ln_g", e * 4 + jj)
                bcol = cpcol("ln_b", e * 4 + jj)
                fw.op(act, lambda h: h.activation(out=MIX.ap[:, 4 + jj, :], in_=Z[jj].ap, func=AF.Silu, bias=bcol, scale=gcol),
                      reads=[Z[jj], CP], writes=[MIX])
        yield from outproj_postnorm(cs, layer, 1, "a_w_out", e)

    def attn_sublayer(cs, layer):
        for c in cs:
            l = common_alloc(c)
            ar = c.ar
            l.Q = ar.alloc("Q", [KC, T], BF16)
            l.PT = [ar.alloc("PT%d" % i, [2, T], BF16) for i in range(2)]
            l.RC = [ar.alloc("RC%d" % i, [T], F32) for i in range(2)]
            l.KTb = ar.alloc("KTb%d" % c.s, [KC, NMEM], BF16)
            l.Vb = ar.alloc("Vb%d" % c.s, [2, D], BF16)
            fw.dma(sp, l.KTb.ap.rearrange("p a b -> p (a b)"), kvd[c.s, layer, :, 0:2048], reads=[c.KVD[layer]], writes=[l.KTb])
            fw.dma(sp, l.Vb.ap.rearrange("p a b -> p (a b)"), kvd[c.s, layer, :, 2048:4096], reads=[c.KVD[layer]], writes=[l.Vb])
            prenorm(c, layer, 2)

        def evq(c, j, ps):
            fw.op(act, lambda h: h.activation(out=c.l.Q.ap[:, j, :], in_=ps.ap[:, 0:T], func=AF.Copy), reads=[ps], writes=[c.l.Q])
        yield from linear_fm(cs, lambda c: c.l.H, KC, "x_w_q", layer, 0, D, evq)
        for hd in range(4):
            for c in cs:
                l = c.l
                Q, MIX = l.Q, l.MIX
                pt = l.PT[hd % 2]
                rc = l.RC[hd % 2]
                for mc in range(2):
                    (ps,) = fw.psum(1)
                    for dj in range(2):
                        fw.op(pe, lambda h: h.matmul(ps.ap[:, 0:T], l.KTb.ap[:, 2 * hd + dj, mc * 128:(mc + 1) * 128], Q.ap[:, 2 * hd + dj, :],
                                                     start=(dj == 0), stop=(dj == 1)), reads=[l.KTb, Q], writes=[ps])
                    fw.op(act, lambda h: h.activation(out=pt.ap[:, mc, :], in_=ps.ap[:, 0:T], func=AF.Exp, scale=1.0 / 16.0),
                          reads=[ps], writes=[pt])
                yield None
                (pd,) = fw.psum(1)
                for mc in range(2):
                    fw.op(pe, lambda h: h.matmul(pd.ap[:, 0:T], ONESB, pt.ap[:, mc, :], start=(mc == 0), stop=(mc == 1)),
                          reads=[pt, CSB], writes=[pd])
                fw.op(dve, lambda h: h.reciprocal(out=rc.ap, in_=pd.ap[:, 0:T]), reads=[pd], writes=[rc])
                for dj in range(2):
                    (po,) = fw.psum(1)
                    for mc in range(2):
                        fw.op(pe, lambda h: h.matmul(po.ap[:, 0:T], l.Vb.ap[:, mc, (2 * hd + dj) * 128:(2 * hd + dj + 1) * 128], pt.ap[:, mc, :],
                                                     start=(mc == 0), stop=(mc == 1)), reads=[l.Vb, pt], writes=[po])
                    fw.op(dve, lambda h: h.tensor_tensor(out=MIX.ap[:, 2 * hd + dj, :], in0=po.ap[:, 0:T], in1=rc.ap, op=ALU.mult),
                          reads=[po, rc], writes=[MIX])
        yield from outproj_postnorm(cs, layer, 3, "x_w_o", layer)

    def ffn_sublayer(cs, layer):
        for c in cs:
            l = common_alloc(c)
            l.A = c.ar.alloc("A", [22, T], BF16)
            l.SG = [c.ar.alloc("SG%d" % i, [T], F32) for i in range(2)]
            prenorm(c, layer, 4)
        for cc in range(11):
            slot, v = yield ("f_w_gu", layer, 0, KC, ((cc * 256, 256), (DFF + cc * 256, 256)))
            for jj in range(2):
                for c in cs:
                    l = c.l
                    H = l.H
                    (pg,) = fw.psum(1)
                    (pu,) = fw.psum(1)
                    for k in range(KC):
                        fw.op(pe, lambda h: h.matmul(pg.ap[:, 0:T], v[:, k, jj * 128:(jj + 1) * 128], H.ap[:, k, :],
                                                     start=(k == 0), stop=(k == KC - 1)), reads=[slot, H], writes=[pg])
                    for k in range(KC):
                        fw.op(pe, lambda h: h.matmul(pu.ap[:, 0:T], v[:, k, 256 + jj * 128:256 + (jj + 1) * 128], H.ap[:, k, :],
                                                     start=(k == 0), stop=(k == KC - 1)), reads=[slot, H], writes=[pu])
                    sg = l.SG[jj]
                    fw.op(act, lambda h: h.activation(out=sg.ap, in_=pg.ap[:, 0:T], func=AF.Silu), reads=[pg], writes=[sg])
                    fw.op(dve, lambda h: h.tensor_tensor(out=l.A.ap[:, 2 * cc + jj, :], in0=pu.ap[:, 0:T], in1=sg.ap, op=ALU.mult),
                          reads=[pu, sg], writes=[l.A])
        for m in range(8):
            slot, v = yield ("f_w_down", layer, 0, 22, ((m * 128, 128),))
            for c in cs:
                A = c.l.A
                (pb,) = fw.psum(1)
                for kc in range(22):
                    fw.op(pe, lambda h: h.matmul(pb.ap[:, 0:T], v[:, kc, :], A.ap[:, kc, :], start=(kc == 0), stop=(kc == 21)),
                          reads=[slot, A], writes=[pb])
                evac_y(c, m, pb, layer, 5)
        for c in cs:
            postnorm(c, layer, 5)
        yield None
        yield None

    def mlstm_sublayer(cs, layer):
        o = layer // 2
        W = "m_w_in"
        for c in cs:
            l = common_alloc(c)
            ar = c.ar
            l.Q = ar.alloc("Q", [KC, T], BF16)
            l.KT = ar.alloc("KT", [TC, D], BF16)
            l.KTT = ar.alloc("KTT", [KC, T], BF16)
            l.VA = ar.alloc("VA", [TC, 8, 129], BF16)
            l.SO = ar.alloc("SO", [TC, D], BF16)
            l.GT = ar.alloc("GT", [TC, 16], F32)
            l.LL = ar.alloc("LL", [TC, 8], F32)
            l.EQ = ar.alloc("EQ", [TC, 8], F32)
            l.EK = ar.alloc("EK", [TC, 8], F32)
            l.GG = ar.alloc("GG", [TC, 8], F32)
            l.WT = [ar.alloc("WT%d" % i, [8, 128], BF16) for i in range(2)]
            l.HH = ar.alloc("HH", [8, 128], F32)
            l.H2 = ar.alloc("H2", [8, 128], F32)
            l.YT = ar.alloc("YT", [8, 128], BF16)
            l.CB = ar.alloc("CB", [8, 129], BF16)
            l.ST = [ar.alloc("ST%d" % i, [8], F32) for i in range(6)]
            prenorm(c, layer, 0)

        def tm_linear(col0, ncols, evac):
            slot, v = yield (W, o, 0, KC, ((col0, ncols),))
            for c in cs:
                H = c.l.H
                for tc in range(TC):
                    (ps,) = fw.psum(1)
                    for k in range(KC):
                        fw.op(pe, lambda h: h.matmul(ps.ap[:, 0:ncols], H.ap[:, k, tc * 128:(tc + 1) * 128], v[:, k, :],
                                                     start=(k == 0), stop=(k == KC - 1)), reads=[slot, H], writes=[ps])
                    evac(c, tc, ps)

        def ev_g(c, tc, ps):
            fw.op(dve, lambda h: h.tensor_tensor(out=c.l.GT.ap[:, tc, :], in0=ps.ap[:, 0:16], in1=RPt[:, o, :], op=ALU.add),
                  reads=[ps, RP], writes=[c.l.GT])
        yield from tm_linear(4 * D, 16, ev_g)
        for c in cs:
            l = c.l
            GT, LL, EQ, EK, GG = l.GT, l.LL, l.EQ, l.EK, l.GG
            fw.op(act, lambda h: h.activation(out=LL.ap, in_=GT.ap[:, :, 8:16], func=AF.Exp, scale=-1.0), reads=[GT], writes=[LL])
            fw.op(act, lambda h: h.activation(out=LL.ap, in_=LL.ap, func=AF.Ln, bias=1.0), reads=[LL], writes=[LL])
            (pc,) = fw.psum(1)
            (pg,) = fw.psum(1)
            for tc in range(TC):
                fw.op(pe, lambda h: h.matmul(pc.ap[:, tc * 8:(tc + 1) * 8], TRIF, LL.ap[:, tc, :], start=True, stop=True),
                      reads=[CST, LL], writes=[pc])
                fw.op(pe, lambda h: h.matmul(pg.ap[:, tc * 8:(tc + 1) * 8], ONESF, LL.ap[:, tc, :], start=True, stop=True),
                      reads=[CST, LL], writes=[pg])
            pcv = pc.ap[:, 0:TC * 8].rearrange("p (a b) -> p a b", b=8)
            pgv = pg.ap[:, 0:TC * 8].rearrange("p (a b) -> p a b", b=8)
            fw.op(act, lambda h: h.activation(out=EQ.ap, in_=pcv, func=AF.Exp, scale=-1.0), reads=[pc], writes=[EQ])
            fw.op(act, lambda h: h.activation(out=GG.ap, in_=pgv, func=AF.Exp, scale=-1.0), reads=[pg], writes=[GG])
            fw.op(dve, lambda h: h.tensor_tensor(out=EK.ap, in0=pcv, in1=GT.ap[:, :, 0:8], op=ALU.add), reads=[pc, GT], writes=[EK])
            fw.op(act, lambda h: h.activation(out=EK.ap, in_=EK.ap, func=AF.Exp), reads=[EK], writes=[EK])

        def evq(c, j, ps):
            fw.op(act, lambda h: h.activation(out=c.l.Q.ap[:, j, :], in_=ps.ap[:, 0:T], func=AF.Copy), reads=[ps], writes=[c.l.Q])
        yield from linear_fm(cs, lambda c: c.l.H, KC, W, o, 0, D, evq)
        for cc in range(2):
            def ev_k(c, tc, ps):
                l = c.l
                fw.op(dve, lambda h: h.scalar_tensor_tensor(
                    out=l.KT.ap[:, tc, cc * 512:(cc + 1) * 512].rearrange("p (a b) -> p a b", b=128),
                    in0=ps.ap.rearrange("p (a b) -> p a b", b=128), scalar=KSCALE,
                    in1=l.EK.ap[:, tc, cc * 4:(cc + 1) * 4].unsqueeze(2).broadcast_to([128, 4, 128]),
                    op0=ALU.mult, op1=ALU.mult), reads=[ps, l.EK], writes=[l.KT])
            yield from tm_linear(D + cc * 512, 512, ev_k)
        for c in cs:
            fw.op(dve, lambda h: h.memset(c.l.VA.ap[:, :, :, 128:129], 1.0), writes=[c.l.VA])
        for cc in range(2):
            def ev_v(c, tc, ps):
                fw.op(act, lambda h: h.activation(out=c.l.VA.ap[:, tc, cc * 4:(cc + 1) * 4, 0:128],
                                                  in_=ps.ap.rearrange("p (a b) -> p a b", b=128), func=AF.Copy),
                      reads=[ps], writes=[c.l.VA])
            yield from tm_linear(2 * D + cc * 512, 512, ev_v)
        for cc in range(2):
            def ev_o(c, tc, ps):
                fw.op(act, lambda h: h.activation(out=c.l.SO.ap[:, tc, cc * 512:(cc + 1) * 512], in_=ps.ap, func=AF.Sigmoid),
                      reads=[ps], writes=[c.l.SO])
            yield from tm_linear(3 * D + cc * 512, 512, ev_o)
        for c in cs:
            l = c.l
            for tc in range(TC):
                (pt,) = fw.psum(1)
                ptb = pt.ap.bitcast(BF16)
                for hd in range(8):
                    fw.op(pe, lambda h: h.transpose(ptb[:, hd * 128:(hd + 1) * 128], l.KT.ap[:, tc, hd * 128:(hd + 1) * 128], IDENTB),
                          reads=[l.KT, CSB], writes=[pt])
                fw.op(act, lambda h: h.activation(out=l.KTT.ap[:, :, tc * 128:(tc + 1) * 128],
                                                  in_=ptb.rearrange("p (a b) -> p a b", b=128), func=AF.Copy),
                      reads=[pt], writes=[l.KTT])
            fw.op(act, lambda h: h.activation(out=l.CB.ap, in_=c.C32t[:, o], func=AF.Copy), reads=[c.C32[o]], writes=[l.CB])
        for tc in range(TC):
            for c in cs:
                yield from chunk(c, o, tc)
        yield from outproj_postnorm(cs, layer, 1, "m_w_out", o)

    def chunk(c, o, tc):
        l = c.l
        Q, KT, KTT, VA, SO, EQ, GG, HH, H2, YT, CB, MIX = l.Q, l.KT, l.KTT, l.VA, l.SO, l.EQ, l.GG, l.HH, l.H2, l.YT, l.CB, l.MIX
        C32t, C32b = c.C32t, c.C32[o]
        sl = slice(tc * 128, (tc + 1) * 128)
        wt = l.WT[tc % 2]
        pss = fw.psum(2)
        for hd in range(8):
            b = pss[hd // 4]
            fw.op(pe, lambda h: h.matmul(b.ap[:, (hd % 4) * 128:(hd % 4 + 1) * 128], KTT.ap[:, hd, sl], Q.ap[:, hd, sl],
                                         start=True, stop=True), reads=[KTT, Q], writes=[b])
        for g2 in range(2):
            fw.op(dve, lambda h: h.tensor_tensor(out=wt.ap[:, g2 * 4:(g2 + 1) * 4, :],
                                                 in0=pss[g2].ap.rearrange("p (a b) -> p a b", b=128),
                                                 in1=CSTt[:, 1:2, :].broadcast_to([128, 4, 128]), op=ALU.mult),
                  reads=[pss[g2], CST], writes=[wt])
        psp = fw.psum(3)
        for hd in range(8):
            bp = psp[hd // 3]
            cs_ = slice((hd % 3) * 129, (hd % 3 + 1) * 129)
            fw.op(pe, lambda h: h.matmul(bp.ap[:, cs_], KT.ap[:, tc, hd * 128:(hd + 1) * 128], VA.ap[:, tc, hd, :],
                                         start=True, stop=True), reads=[KT, VA], writes=[bp])
        for g3 in range(3):
            nh = 3 if g3 < 2 else 2
            hs = slice(g3 * 3, g3 * 3 + nh)
            fw.op(dve, lambda h: h.tensor_tensor(out=C32t[:, o, hs, :], in0=psp[g3].ap[:, 0:nh * 129].rearrange("p (a b) -> p a b", b=129),
                                                 in1=C32t[:, o, hs, :], op=ALU.add), reads=[psp[g3], C32b], writes=[C32b])
        yield None
        psn = fw.psum(3)
        for hd in range(8):
            bn = psn[hd // 3]
            cs_ = slice((hd % 3) * 129, (hd % 3 + 1) * 129)
            fw.op(pe, lambda h: h.matmul(bn.ap[:, cs_], Q.ap[:, hd, sl], CB.ap[:, hd, :], start=True, stop=False),
                  reads=[Q, CB], writes=[bn])
            fw.op(pe, lambda h: h.matmul(bn.ap[:, cs_], wt.ap[:, hd, :], VA.ap[:, tc, hd, :], start=False, stop=True),
                  reads=[wt, VA], writes=[bn])
        fw.op(dve, lambda h: h.tensor_tensor(out=C32t[:, o], in0=C32t[:, o],
                                             in1=GG.ap[:, tc, :].unsqueeze(2).broadcast_to([128, 8, 129]), op=ALU.mult),
              reads=[C32b, GG], writes=[C32b])
        fw.op(act, lambda h: h.activation(out=CB.ap, in_=C32t[:, o], func=AF.Copy), reads=[C32b], writes=[CB])
        DN, RR, S1, S2, MUh, RS = l.ST
        for g3 in range(3):
            nh = 3 if g3 < 2 else 2
            hs = slice(g3 * 3, g3 * 3 + nh)
            v3 = psn[g3].ap[:, 0:nh * 129].rearrange("p (a b) -> p a b", b=129)
            fw.op(act, lambda h: h.activation(out=DN.ap[:, hs].unsqueeze(2), in_=v3[:, :, 128:129], func=AF.Abs),
                  reads=[psn[g3]], writes=[DN])
        fw.op(dve, lambda h: h.tensor_tensor(out=DN.ap, in0=DN.ap, in1=EQ.ap[:, tc, :], op=ALU.mult), reads=[DN, EQ], writes=[DN])
        fw.op(dve, lambda h: h.tensor_scalar(out=DN.ap, in0=DN.ap, scalar1=1.0, scalar2=None, op0=ALU.max), reads=[DN], writes=[DN])
        fw.op(dve, lambda h: h.reciprocal(out=DN.ap, in_=DN.ap), reads=[DN], writes=[DN])
        fw.op(dve, lambda h: h.tensor_tensor(out=RR.ap, in0=EQ.ap[:, tc, :], in1=DN.ap, op=ALU.mult), reads=[DN, EQ], writes=[RR])
        for g3 in range(3):
            nh = 3 if g3 < 2 else 2
            hs = slice(g3 * 3, g3 * 3 + nh)
            v3 = psn[g3].ap[:, 0:nh * 129].rearrange("p (a b) -> p a b", b=129)
            fw.op(dve, lambda h: h.tensor_tensor(out=HH.ap[:, hs, :], in0=v3[:, :, 0:128],
                                                 in1=RR.ap[:, hs].unsqueeze(2).broadcast_to([128, nh, 128]), op=ALU.mult),
                  reads=[psn[g3], RR], writes=[HH])
        fw.op(dve, lambda h: h.tensor_reduce(out=S1.ap, in_=HH.ap, axis=AX.X, op=ALU.add), reads=[HH], writes=[S1])
        fw.op(act, lambda h: h.activation(out=H2.ap, in_=HH.ap, func=AF.Square), reads=[HH], writes=[H2])
        fw.op(dve, lambda h: h.tensor_reduce(out=S2.ap, in_=H2.ap, axis=AX.X, op=ALU.add), reads=[H2], writes=[S2])
        fw.op(dve, lambda h: h.tensor_scalar(out=MUh.ap, in0=S1.ap, scalar1=1.0 / 128, scalar2=None, op0=ALU.mult), reads=[S1], writes=[MUh])
        fw.op(dve, lambda h: h.tensor_tensor(out=S1.ap, in0=MUh.ap, in1=MUh.ap, op=ALU.mult), reads=[MUh], writes=[S1])
        fw.op(dve, lambda h: h.scalar_tensor_tensor(out=RS.ap, in0=S2.ap, scalar=1.0 / 128, in1=S1.ap, op0=ALU.mult, op1=ALU.subtract),
              reads=[S2, S1], writes=[RS])
        fw.op(dve, lambda h: h.tensor_scalar(out=RS.ap, in0=RS.ap, scalar1=EPS, scalar2=None, op0=ALU.add), reads=[RS], writes=[RS])
        fw.op(act, lambda h: h.activation(out=RS.ap, in_=RS.ap, func=AF.Sqrt), reads=[RS], writes=[RS])
        fw.op(dve, lambda h: h.reciprocal(out=RS.ap, in_=RS.ap), reads=[RS], writes=[RS])
        fw.op(dve, lambda h: h.tensor_tensor(out=HH.ap, in0=HH.ap, in1=MUh.ap.unsqueeze(2).broadcast_to([128, 8, 128]), op=ALU.subtract),
              reads=[HH, MUh], writes=[HH])
        fw.op(dve, lambda h: h.tensor_tensor(out=HH.ap, in0=HH.ap, in1=RS.ap.unsqueeze(2).broadcast_to([128, 8, 128]), op=ALU.mult),
              reads=[HH, RS], writes=[HH])
        fw.op(dve, lambda h: h.tensor_tensor(out=YT.ap, in0=HH.ap, in1=SO.ap[:, tc, :].rearrange("p (a b) -> p a b", b=128), op=ALU.mult),
              reads=[HH, SO], writes=[YT])
        yield None
        (py,) = fw.psum(1)
        pyb = py.ap.bitcast(BF16)
        for hd in range(8):
            fw.op(pe, lambda h: h.transpose(pyb[:, hd * 128:(hd + 1) * 128], YT.ap[:, hd, :], IDENTB), reads=[YT, CSB], writes=[py])
        mg = CPt[:, CPO["m_g"] + o * 8: CPO["m_g"] + o * 8 + 8]
        fw.op(dve, lambda h: h.tensor_tensor(out=MIX.ap[:, :, sl], in0=pyb.rearrange("p (a b) -> p a b", b=128),
                                             in1=mg.unsqueeze(2).broadcast_to([128, 8, 128]), op=ALU.mult),
              reads=[py, CP], writes=[MIX])

    def ctx_program(c):
        cs = [c]
        for o in range(2):
            fw.op(dve, lambda h: h.memset(c.C32t[:, o], 0.0), writes=[c.C32[o]])
        for e in range(2):
            fw.op(dve, lambda h: h.memset(c.GLUt[:, e], 0.0), writes=[c.GLU[e]])
            fw.op(dve, lambda h: h.memset(c.PBt[:, e], 0.0), writes=[c.PB[e]])
        for layer in range(DEPTH):
            yield from kv_prep(cs, layer)
        for t in range(NT):
            fw.dma(sp, c.Xt[:], io["x"][c.s][:, t * T:(t + 1) * T].rearrange("(k p) t -> p k t", p=128), writes=[c.X])
            for layer in range(DEPTH):
                if layer % 2 == 0:
                    yield from conv_sublayer(cs, layer)
                else:
                    yield from mlstm_sublayer(cs, layer)
                yield from attn_sublayer(cs, layer)
                yield from ffn_sublayer(cs, layer)
            fw.dma(sp, io["y"][c.s][:, t * T:(t + 1) * T].rearrange("(k p) t -> p k t", p=128), c.Xt[:], reads=[c.X])

    run = ctxs[:1] if dry else ctxs
    gens = [ctx_program(c) for c in run]
    n = len(gens)
    nxt = [None] * n
    blk = [0] * n
    prog = [0] * n
    done = [False] * n
    for i, g in enumerate(gens):
        try:
            nxt[i] = next(g)
        except StopIteration:
            done[i] = True
    while not all(done):
        if n == 1 or done[1]:
            i = 0
        elif done[0]:
            i = 1
        else:
            i = 0 if (prog[0] - prog[1]) < SKEW else 1
        if nxt[i] is None:
            val = None
        else:
            bmin = min(blk[j] for j in range(n) if not done[j])
            val = wq.get(blk[i], nxt[i], bmin)
            blk[i] += 1
        prog[i] += 1
        try:
            nxt[i] = gens[i].send(val)
        except StopIteration:
            done[i] = True
    fw.wait_all(sp, [c.X for c in ctxs])
    return fw


def build_nc(NSEQ=2, NT=8, DEPTH=4):
    S = NT * T
    nc = bass.Bass("TRN2", target_bir_lowering=False)

    def din(name, shape):
        return nc.dram_tensor(name, shape, F32, kind="ExternalInput").ap()
    io = {}
    io["x"] = din("x", [NSEQ, D, S])
    io["mem"] = din("mem", [NSEQ, D, NMEM])
    io["consts"] = din("consts", [128, 3, 128])
    io["cp"] = din("cp", [128, NCP])
    io["rp"] = din("rp", [128, 2, 16])
    io["w"] = {
        "a_w_in": din("a_w_in", [2, D, 2560]), "a_w_out": din("a_w_out", [2, D, D]),
        "m_w_in": din("m_w_in", [2, D, 4112]), "m_w_out": din("m_w_out", [2, D, D]),
        "x_w_q": din("x_w_q", [4, D, D]), "x_w_kv": din("x_w_kv", [4, D, 2 * D]), "x_w_o": din("x_w_o", [4, D, D]),
        "f_w_gu": din("f_w_gu", [4, D, 2 * DFF]), "f_w_down": din("f_w_down", [4, DFF, D]),
    }
    io["y"] = nc.dram_tensor("y", [NSEQ, D, S], F32, kind="ExternalOutput").ap()
    io["kvd"] = nc.dram_tensor("kvd", [NSEQ, 4, 128, 4096], BF16, kind="Internal").ap()
    plan = []
    with contextlib.ExitStack() as es:
        _emit(nc, es, True, plan, NSEQ, NT, DEPTH, io)
    es2 = contextlib.ExitStack()
    with es2:
        fw = _emit(nc, es2, False, plan, NSEQ, NT, DEPTH, io)
    return nc, fw


def host_tables(inp):
    cp = np.zeros((128, NCP), np.float32)

    def put(name, idx, vec128):
        cp[:, CPO[name] + idx] = vec128
    ng = np.asarray(inp["norm_g"], np.float32)
    for l in range(4):
        for j in range(6):
            for k in range(8):
                put("norm_g", (l * 6 + j) * 8 + k, ng[l, j, k * 128:(k + 1) * 128])
    mg = np.asarray(inp["mem_norm_g"], np.float32)
    for l in range(4):
        for k in range(8):
            put("mem_g", l * 8 + k, mg[l, k * 128:(k + 1) * 128])
    ca = np.asarray(inp["a_conv_a"], np.float32)
    cb = np.asarray(inp["a_conv_b"], np.float32)
    cbb = np.asarray(inp["a_conv_b_bias"], np.float32)
    lg = np.asarray(inp["a_ln_g"], np.float32)
    lb = np.asarray(inp["a_ln_b"], np.float32)
    for e in range(2):
        for c in range(4):
            for k in range(3):
                put("conv_a", (e * 4 + c) * 3 + k, ca[e, k, c * 128:(c + 1) * 128])
            for k in range(31):
                put("conv_b", (e * 4 + c) * 31 + k, cb[e, k, c * 128:(c + 1) * 128])
            put("cb_bias", e * 4 + c, cbb[e, c * 128:(c + 1) * 128])
            put("ln_g", e * 4 + c, lg[e, c * 128:(c + 1) * 128])
            put("ln_b", e * 4 + c, lb[e, c * 128:(c + 1) * 128])
    mng = np.asarray(inp["m_norm_g"], np.float32)
    for o in range(2):
        for k in range(8):
            put("m_g", o * 8 + k, mng[o, k * 128:(k + 1) * 128])
    rp = np.zeros((128, 2, 16), np.float32)
    rp[:, :, 0:8] = np.asarray(inp["m_i_bias"], np.float32)[None]
    rp[:, :, 8:16] = np.asarray(inp["m_f_bias"], np.float32)[None]
    consts = np.zeros((128, 3, 128), np.float32)
    consts[:, 0, :] = np.eye(128, dtype=np.float32)
    consts[:, 1, :] = np.triu(np.ones((128, 128), np.float32))
    consts[:, 2, :] = 1.0
    return cp, rp, consts


WNAMES = ["a_w_in", "a_w_out", "m_w_in", "m_w_out", "x_w_q", "x_w_kv", "x_w_o", "f_w_gu", "f_w_down"]


def kernel(**inp):
    x = np.asarray(inp["x"], np.float32)
    mem = np.asarray(inp["mem"], np.float32)
    B = x.shape[0]
    nseq = B // NCORES
    cp, rp, consts = host_tables(inp)
    nc, _ = build_nc(NSEQ=nseq, NT=x.shape[1] // T, DEPTH=4)
    shared = {"consts": consts, "cp": cp, "rp": rp}
    for w in WNAMES:
        shared[w] = np.ascontiguousarray(np.asarray(inp[w], np.float32))
    in_maps = []
    for c in range(NCORES):
        m = dict(shared)
        m["x"] = np.ascontiguousarray(x[c * nseq:(c + 1) * nseq].transpose(0, 2, 1))
        m["mem"] = np.ascontiguousarray(mem[c * nseq:(c + 1) * nseq].transpose(0, 2, 1))
        in_maps.append(m)
    res = run_bass_kernel_spmd(nc, in_maps, core_ids=list(range(NCORES)))
    out = np.empty_like(x)
    for c in range(NCORES):
        y = res.results[c]["y"]
        out[c * nseq:(c + 1) * nseq] = y.transpose(0, 2, 1)
    return out
```

```python
import contextlib
import numpy as np
import concourse.bass as bass
import concourse.mybir as mybir
from concourse.bass_utils import run_bass_kernel_spmd

F32 = mybir.dt.float32
BF16 = mybir.dt.bfloat16
AF = mybir.ActivationFunctionType
ALU = mybir.AluOpType
AX = mybir.AxisListType

D = 1024
KC = 8
T = 256
NMEM = 256
DFF = 2816
SEQ = 4096
NCORES = 8
EPS = 1e-6
NRING = 5
SKEW = 2
SLOT = 4096
KSCALE = 128.0 ** -0.5

def _cp_layout():
    off = {}
    n = 0
    for name, cnt in (("norm_g", 4 * 6 * 8), ("mem_g", 4 * 8), ("conv_a", 2 * 4 * 3), ("conv_b", 2 * 4 * 31),
                      ("cb_bias", 2 * 4), ("ln_g", 2 * 4), ("ln_b", 2 * 4), ("m_g", 2 * 8)):
        off[name] = n
        n += cnt
    return off, n


CPO, NCP = _cp_layout()


class Ctr:
    LIMIT = 16000

    def __init__(self, fw, name, owner=None):
        self.fw, self.name, self.owner, self.k, self.val = fw, name, owner, 0, 0
        self.sem = fw.new_sem(name + "_0")

    def bump(self, inc):
        if self.val + inc > self.LIMIT:
            self.k += 1
            self.sem = self.fw.new_sem("%s_%d" % (self.name, self.k))
            self.val = 0
        self.val += inc
        return (self.sem, self.val, self.owner)


class Eng:
    def __init__(self, fw, name, h, selfsync=True):
        self.h, self.name, self.selfsync = h, name, selfsync
        self.ctr = Ctr(fw, name, self)
        self.waited = {}


class Buf:
    def __init__(self, name, ap=None):
        self.name, self.ap = name, ap
        self.w = None
        self.r = {}
        self.dctr = None

    def deps(self):
        d = list(self.r.values())
        if self.w is not None:
            d.append(self.w)
        return d


class FW:
    def __init__(self, nc, es, dry):
        self.nc, self.es, self.dry = nc, es, dry
        self.nsem = 0
        self.nops = 0
        self.dctrs = {}
        if not dry:
            self.pe = Eng(self, "pe", nc.tensor, selfsync=False)
            self.act = Eng(self, "act", nc.scalar)
            self.dve = Eng(self, "dve", nc.vector)
            self.pool = Eng(self, "pool", nc.gpsimd)
            self.sp = Eng(self, "sp", nc.sync)
        else:
            self.pe = self.act = self.dve = self.pool = self.sp = None
        self.psbanks = None
        self.psptr = 0

    def new_sem(self, name):
        if self.dry:
            return None
        self.nsem += 1
        return self.es.enter_context(self.nc.semaphore(name))

    def _wait(self, eng, deps):
        for (sem, val, owner) in deps:
            if owner is eng and not eng.selfsync:
                continue
            k = id(sem)
            if eng.waited.get(k, 0) < val:
                eng.h.wait_ge(sem, val)
                eng.waited[k] = val

    def _deps(self, reads, writes):
        deps = []
        for b in reads:
            if b.w is not None:
                deps.append(b.w)
        for b in writes:
            deps.extend(b.deps())
        return deps

    def _commit(self, d, reads, writes):
        k = id(d[0])
        for b in reads:
            old = b.r.get(k)
            if old is None or old[1] < d[1]:
                b.r[k] = d
        for b in writes:
            b.w = d
            b.r = {}

    def op(self, eng, fn, reads=(), writes=()):
        if self.dry:
            return
        self._wait(eng, self._deps(reads, writes))
        ins = fn(eng.h)
        d = eng.ctr.bump(1)
        ins.then_inc(d[0], 1)
        self._commit(d, reads, writes)
        self.nops += 1

    def dma(self, eng, out, in_, reads=(), writes=()):
        if self.dry:
            return
        self._wait(eng, self._deps(reads, writes))
        ins = eng.h.dma_start(out=out, in_=in_)
        b = writes[0] if writes else reads[0]
        if b.dctr is None:
            if b.name not in self.dctrs:
                self.dctrs[b.name] = Ctr(self, "d_" + b.name, None)
            b.dctr = self.dctrs[b.name]
        d = b.dctr.bump(16)
        ins.then_inc(d[0], 16)
        self._commit(d, reads, writes)
        self.nops += 1

    def wait_all(self, eng, bufs):
        if self.dry:
            return
        deps = []
        for b in bufs:
            deps.extend(b.deps())
        self._wait(eng, deps)

    def psum(self, n=1):
        if self.psptr + n > 8:
            self.psptr = 0
        bs = self.psbanks[self.psptr:self.psptr + n]
        self.psptr = (self.psptr + n) % 8
        return bs


class Arena:
    def __init__(self, fw, tensor, nelem):
        self.fw, self.t, self.n = fw, tensor, nelem
        self.hist = []
        self.cur = []
        self.off = 0

    def reset(self):
        newh = list(self.cur)
        for (a, b, buf) in self.hist:
            if not any(a < cb and ca < b for (ca, cb, _) in self.cur):
                newh.append((a, b, buf))
        self.hist = newh
        self.cur = []
        self.off = 0

    def alloc(self, name, shape, dtype):
        n = 1
        for s in shape:
            n *= s
        nb = n * (2 if dtype == F32 else 1)
        nb = (nb + 15) // 16 * 16
        a, b = self.off, self.off + nb
        assert b <= self.n, "arena overflow %s %d > %d" % (name, b, self.n)
        self.off = b
        ap = self.t[:, a:a + n * (2 if dtype == F32 else 1)]
        if dtype == F32:
            ap = ap.bitcast(F32)
        if len(shape) == 2:
            ap = ap.rearrange("p (a b) -> p a b", b=shape[1])
        elif len(shape) == 3:
            ap = ap.rearrange("p (a b c) -> p a b c", b=shape[1], c=shape[2])
        buf = Buf(name, ap)
        for (ha, hb, hbuf) in self.hist:
            if ha < b and a < hb:
                for d in hbuf.deps():
                    k = id(d[0])
                    old = buf.r.get(k)
                    if old is None or old[1] < d[1]:
                        buf.r[k] = d
        self.cur.append((a, b, buf))
        return buf


class WQ:
    def __init__(self, fw, slots, wdram, plan):
        self.fw, self.slots, self.wdram = fw, slots, wdram
        self.plan = plan
        self.i = 0
        self.issued = 0

    def _view(self, slot, nkc, ncols):
        return slot.ap[:, 0:nkc * ncols].rearrange("p (k m) -> p k m", m=ncols)

    def _issue(self, j):
        (wname, li, row0, nkc, segs) = self.plan[j]
        slot = self.slots[j % NRING]
        ncols = sum(n for (_, n) in segs)
        v = self._view(slot, nkc, ncols)
        W = self.wdram[wname][li]
        off = 0
        for (c0, n) in segs:
            src = W[row0:row0 + nkc * 128, c0:c0 + n].rearrange("(k p) m -> p k m", p=128)
            self.fw.dma(self.fw.pool, v[:, :, off:off + n], src, writes=[slot])
            off += n

    def get(self, k, spec, bmin):
        (wname, li, row0, nkc, segs) = spec
        ncols = sum(n for (_, n) in segs)
        assert nkc * ncols <= SLOT
        if self.fw.dry:
            assert k == len(self.plan)
            self.plan.append(spec)
            return self.slots[0], self._view(self.slots[0], nkc, ncols)
        assert self.plan[k] == spec, (k, self.plan[k], spec)
        while self.issued < min(len(self.plan), bmin + NRING):
            self._issue(self.issued)
            self.issued += 1
        assert k < self.issued
        slot = self.slots[k % NRING]
        return slot, self._view(slot, nkc, ncols)


class Ctx:
    pass


class L:
    pass


def _emit(nc, es, dry, plan, NSEQ, NT, DEPTH, io):
    fw = FW(nc, es, dry)
    E = es.enter_context
    sfx = "d" if dry else "r"
    TC = T // 128

    def sb(name, shape, dt):
        return E(nc.sbuf_tensor(name + sfx, shape, dt))

    ring = [Buf("ring%d" % i, sb("ring%d" % i, [128, SLOT], BF16)[:]) for i in range(NRING)]
    CSTt = sb("CST", [128, 3, 128], F32)
    CSBt = sb("CSB", [128, 3, 128], BF16)
    CST = Buf("CST", CSTt[:])
    CSB = Buf("CSB", CSBt[:])
    CPt = sb("CP", [128, NCP], F32)
    CP = Buf("CP", CPt[:])
    RPt = sb("RP", [128, 2, 16], F32)
    RP = Buf("RP", RPt[:])
    NA = 28544
    ctxs = []
    for s in range(NSEQ):
        c = Ctx()
        c.s = s
        c.Xt = sb("X%d" % s, [128, KC, T], F32)
        c.X = Buf("X%d" % s, c.Xt[:])
        c.C32t = sb("C32_%d" % s, [128, 2, 8, 129], F32)
        c.C32 = [Buf("C32_%d_%d" % (s, o), c.C32t[:, o]) for o in range(2)]
        c.GLUt = sb("GLU%d" % s, [128, 2, 4, 30 + T], BF16)
        c.PBt = sb("PB%d" % s, [128, 2, 4, 2 + T], BF16)
        c.GLU = [Buf("GLU%d_%d" % (s, e), c.GLUt[:, e]) for e in range(2)]
        c.PB = [Buf("PB%d_%d" % (s, e), c.PBt[:, e]) for e in range(2)]
        c.ARt = sb("AR%d" % s, [128, NA], BF16)
        c.ar = Arena(fw, c.ARt, NA)
        c.KVD = [Buf("kvd%d_%d" % (s, l)) for l in range(4)]
        ctxs.append(c)
    PSt = E(nc.psum_tensor("PS" + sfx, [128, 8, 512], F32))
    fw.psbanks = [Buf("ps%d" % i, PSt[:, i, :]) for i in range(8)]
    kvd = io["kvd"]

    wq = WQ(fw, ring, io["w"], plan)
    pe, act, dve, pool, sp = fw.pe, fw.act, fw.dve, fw.pool, fw.sp

    IDENTB = CSBt[:, 0, :]
    ONESB = CSBt[:, 2, :]
    TRIF = CSTt[:, 1, :]
    ONESF = CSTt[:, 2, :]

    def cpcol(name, idx):
        cc = CPO[name] + idx
        return CPt[:, cc:cc + 1]

    fw.dma(sp, CSTt[:], io["consts"], writes=[CST])
    fw.dma(sp, CPt[:], io["cp"], writes=[CP])
    fw.dma(sp, RPt[:], io["rp"], writes=[RP])
    fw.op(dve, lambda h: h.tensor_copy(CSBt[:], CSTt[:]), reads=[CST], writes=[CSB])

    def rms_rstd(srcs, srcbufs, nk, ncols, RB, SQ, dn):
        (ps,) = fw.psum(1)
        for k in range(nk):
            sq = SQ[k % 2]
            fw.op(act, lambda h: h.activation(out=sq.ap[:, 0:ncols], in_=srcs[k], func=AF.Square),
                  reads=[srcbufs[k]], writes=[sq])
            fw.op(pe, lambda h: h.matmul(ps.ap[:, 0:ncols], ONESB, sq.ap[:, 0:ncols], start=(k == 0), stop=(k == nk - 1)),
                  reads=[sq, CSB], writes=[ps])
        fw.op(dve, lambda h: h.tensor_scalar(out=RB.ap[:, 0:ncols], in0=ps.ap[:, 0:ncols], scalar1=1.0 / dn,
                                             scalar2=EPS, op0=ALU.mult, op1=ALU.add), reads=[ps], writes=[RB])
        fw.op(act, lambda h: h.activation(out=RB.ap[:, 0:ncols], in_=RB.ap[:, 0:ncols], func=AF.Sqrt),
              reads=[RB], writes=[RB])
        fw.op(dve, lambda h: h.reciprocal(out=RB.ap[:, 0:ncols], in_=RB.ap[:, 0:ncols]), reads=[RB], writes=[RB])

    def common_alloc(c):
        l = L()
        ar = c.ar
        ar.reset()
        l.H = ar.alloc("H", [KC, T], BF16)
        l.SQ = [ar.alloc("SQ%d" % i, [NMEM], BF16) for i in range(2)]
        l.RB = ar.alloc("RB", [NMEM], F32)
        l.RB2 = ar.alloc("RB2", [T], F32)
        l.YA = ar.alloc("YA", [KC, T], F32)
        l.SQA = ar.alloc("SQA", [KC, T], BF16)
        l.MIX = l.H
        c.l = l
        return l

    def rstd_from_ps(ps, RB):
        fw.op(act, lambda h: h.activation(out=RB.ap[:, 0:T], in_=ps.ap[:, 0:T], func=AF.Ln, scale=1.0 / D, bias=EPS),
              reads=[ps], writes=[RB])
        fw.op(act, lambda h: h.activation(out=RB.ap[:, 0:T], in_=RB.ap[:, 0:T], func=AF.Exp, scale=-0.5),
              reads=[RB], writes=[RB])

    def prenorm(c, layer, j):
        l = c.l
        fw.op(act, lambda h: h.activation(out=l.SQA.ap, in_=c.Xt[:], func=AF.Square), reads=[c.X], writes=[l.SQA])
        (ps,) = fw.psum(1)
        for k in range(KC):
            fw.op(pe, lambda h: h.matmul(ps.ap[:, 0:T], ONESB, l.SQA.ap[:, k, :], start=(k == 0), stop=(k == KC - 1)),
                  reads=[l.SQA, CSB], writes=[ps])
        rstd_from_ps(ps, l.RB)
        yield None
        for k in range(KC):
            g = cpcol("norm_g", (layer * 6 + j) * 8 + k)
            fw.op(dve, lambda h: h.scalar_tensor_tensor(out=l.H.ap[:, k, :], in0=c.Xt[:, k, :], scalar=g,
                                                        in1=l.RB.ap[:, 0:T], op0=ALU.mult, op1=ALU.mult),
                  reads=[c.X, l.RB, CP], writes=[l.H])

    def linear_fm(cs, getsrc, nk, wname, li, col0, ncols_total, evac, blk=512):
        j = 0
        for cc in range(0, ncols_total, blk):
            n = min(blk, ncols_total - cc)
            slot, v = yield (wname, li, 0, nk, ((col0 + cc, n),))
            for m in range(n // 128):
                for c in cs:
                    src = getsrc(c)
                    (ps,) = fw.psum(1)
                    for k in range(nk):
                        fw.op(pe, lambda h: h.matmul(ps.ap[:, 0:T], v[:, k, m * 128:(m + 1) * 128], src.ap[:, k, :],
                                                     start=(k == 0), stop=(k == nk - 1)),
                              reads=[slot, src], writes=[ps])
                    evac(c, j, ps)
                j += 1

    def evac_y(c, j, ps, layer, jg):
        l = c.l
        g = cpcol("norm_g", (layer * 6 + jg) * 8 + j)
        sq = l.SQ[j % 2]
        fw.op(act, lambda h: h.activation(out=l.YA.ap[:, j, :], in_=ps.ap[:, 0:T], func=AF.Copy, scale=g),
              reads=[ps, CP], writes=[l.YA])
        fw.op(act, lambda h: h.activation(out=sq.ap[:, 0:T], in_=ps.ap[:, 0:T], func=AF.Square), reads=[ps], writes=[sq])
        flush_stats(c)

        def deferred():
            (pst,) = fw.psum(1)
            fw.op(pe, lambda h: h.matmul(pst.ap[:, 0:T], ONESB, sq.ap[:, 0:T], start=True, stop=True), reads=[sq, CSB], writes=[pst])
            if j == 0:
                fw.op(dve, lambda h: h.tensor_copy(l.RB2.ap, pst.ap[:, 0:T]), reads=[pst], writes=[l.RB2])
            else:
                fw.op(dve, lambda h: h.tensor_tensor(out=l.RB2.ap, in0=pst.ap[:, 0:T], in1=l.RB2.ap, op=ALU.add),
                      reads=[pst, l.RB2], writes=[l.RB2])
        l.pending = deferred

    def flush_stats(c):
        f = getattr(c.l, "pending", None)
        if f is not None:
            c.l.pending = None
            f()

    def postnorm(c, layer, jg):
        l = c.l
        flush_stats(c)
        rstd_from_ps(l.RB2, l.RB2)
        fw.op(dve, lambda h: h.tensor_tensor(out=l.YA.ap, in0=l.YA.ap, in1=l.RB2.ap.unsqueeze(1).broadcast_to([128, KC, T]), op=ALU.mult),
              reads=[l.YA, l.RB2], writes=[l.YA])
        fw.op(dve, lambda h: h.tensor_tensor(out=c.Xt[:], in0=c.Xt[:], in1=l.YA.ap, op=ALU.add), reads=[c.X, l.YA], writes=[c.X])

    def outproj_postnorm(cs, layer, jg, wname, li):
        def ev(c, j, ps):
            evac_y(c, j, ps, layer, jg)
        yield from linear_fm(cs, lambda c: c.l.MIX, KC, wname, li, 0, D, ev)
        for c in cs:
            postnorm(c, layer, jg)
        yield None

    def kv_prep(cs, layer):
        for c in cs:
            ar = c.ar
            ar.reset()
            l = L()
            c.l = l
            l.MT = ar.alloc("MT%d" % c.s, [KC, NMEM], F32)
            l.HM = ar.alloc("HM", [KC, NMEM], BF16)
            l.SQ = [ar.alloc("SQ%d" % i, [NMEM], BF16) for i in range(2)]
            l.RB = ar.alloc("RB", [NMEM], F32)
            l.KTb = ar.alloc("KTs%d" % c.s, [KC, NMEM], BF16)
            l.Vb = ar.alloc("Vs%d" % c.s, [2, D], BF16)
            fw.dma(sp, l.MT.ap, io["mem"][c.s].rearrange("(k p) m -> p k m", p=128), writes=[l.MT])
            rms_rstd([l.MT.ap[:, k, :] for k in range(KC)], [l.MT] * KC, KC, NMEM, l.RB, l.SQ, float(D))
            for k in range(KC):
                g = cpcol("mem_g", layer * 8 + k)
                fw.op(dve, lambda h: h.scalar_tensor_tensor(out=l.HM.ap[:, k, :], in0=l.MT.ap[:, k, :], scalar=g,
                                                            in1=l.RB.ap[:, 0:NMEM], op0=ALU.mult, op1=ALU.mult),
                      reads=[l.MT, l.RB, CP], writes=[l.HM])
        for cc in range(2):
            slot, v = yield ("x_w_kv", layer, 0, KC, ((cc * 512, 512),))
            for m in range(4):
                for c in cs:
                    l = c.l
                    (ps,) = fw.psum(1)
                    for k in range(KC):
                        fw.op(pe, lambda h: h.matmul(ps.ap[:, 0:NMEM], v[:, k, m * 128:(m + 1) * 128], l.HM.ap[:, k, :],
                                                     start=(k == 0), stop=(k == KC - 1)), reads=[slot, l.HM], writes=[ps])
                    fw.op(act, lambda h: h.activation(out=l.KTb.ap[:, cc * 4 + m, :], in_=ps.ap[:, 0:NMEM], func=AF.Copy),
                          reads=[ps], writes=[l.KTb])
        for cc in range(2):
            slot, v = yield ("x_w_kv", layer, 0, KC, ((D + cc * 512, 512),))
            for tc in range(2):
                for c in cs:
                    l = c.l
                    (ps,) = fw.psum(1)
                    for k in range(KC):
                        fw.op(pe, lambda h: h.matmul(ps.ap, l.HM.ap[:, k, tc * 128:(tc + 1) * 128], v[:, k, :],
                                                     start=(k == 0), stop=(k == KC - 1)), reads=[slot, l.HM], writes=[ps])
                    fw.op(act, lambda h: h.activation(out=l.Vb.ap[:, tc, cc * 512:(cc + 1) * 512], in_=ps.ap, func=AF.Copy),
                          reads=[ps], writes=[l.Vb])
        for c in cs:
            l = c.l
            fw.dma(sp, kvd[c.s, layer, :, 0:2048], l.KTb.ap.rearrange("p a b -> p (a b)"), reads=[l.KTb], writes=[c.KVD[layer]])
            fw.dma(sp, kvd[c.s, layer, :, 2048:4096], l.Vb.ap.rearrange("p a b -> p (a b)"), reads=[l.Vb], writes=[c.KVD[layer]])

    def conv_sublayer(cs, layer):
        e = layer // 2
        for c in cs:
            l = common_alloc(c)
            ar = c.ar
            l.GB = ar.alloc("GB", [4, T], BF16)
            l.GC = ar.alloc("GC", [4, T], F32)
            l.UA = ar.alloc("UA", [4, T], F32)
            l.Z = [ar.alloc("Z%d" % j, [T], F32) for j in range(4)]
            l.SG = [ar.alloc("SG%d" % i, [T], F32) for i in range(2)]
            l.DG = [ar.alloc("DG%d" % i, [34, 128], BF16) for i in range(2)]
            l.MU = ar.alloc("MU", [T], F32)
            l.VR = ar.alloc("VR", [T], F32)
            l.cnt = 0
            yield from prenorm(c, layer, 0)

        def ev(c, j, ps):
            l = c.l
            grp, jj = j // 4, j % 4
            pv = ps.ap[:, 0:T]
            if grp == 0:
                fw.op(act, lambda h: h.activation(out=l.GB.ap[:, jj, :], in_=pv, func=AF.Copy), reads=[ps], writes=[l.GB])
            elif grp == 1:
                fw.op(act, lambda h: h.activation(out=l.GC.ap[:, jj, :], in_=pv, func=AF.Copy), reads=[ps], writes=[l.GC])
            elif grp == 2:
                fw.op(dve, lambda h: h.tensor_tensor(out=c.PBt[:, e, jj, 2:2 + T], in0=pv, in1=l.GC.ap[:, jj, :], op=ALU.mult),
                      reads=[ps, l.GC], writes=[c.PB[e]])
            elif grp == 3:
                fw.op(act, lambda h: h.activation(out=l.UA.ap[:, jj, :], in_=pv, func=AF.Copy), reads=[ps], writes=[l.UA])
            else:
                sg = l.SG[l.cnt % 2]
                l.cnt += 1
                fw.op(act, lambda h: h.activation(out=sg.ap, in_=pv, func=AF.Sigmoid), reads=[ps], writes=[sg])
                fw.op(dve, lambda h: h.tensor_tensor(out=c.GLUt[:, e, jj, 30:30 + T], in0=sg.ap, in1=l.UA.ap[:, jj, :], op=ALU.mult),
                      reads=[sg, l.UA], writes=[c.GLU[e]])
        yield from linear_fm(cs, lambda c: c.l.H, KC, "a_w_in", e, 0, 2560, ev)
        for c in cs:
            l = c.l
            MIX = l.MIX
            Z = l.Z
            def build_dg(jj):
                dg = l.DG[jj % 2]
                wa = CPt[:, CPO["conv_a"] + (e * 4 + jj) * 3: CPO["conv_a"] + (e * 4 + jj) * 3 + 3]
                wb = CPt[:, CPO["conv_b"] + (e * 4 + jj) * 31: CPO["conv_b"] + (e * 4 + jj) * 31 + 31]
                fw.op(dve, lambda h: h.tensor_tensor(out=dg.ap[:, 0:3, :], in0=CSTt[:, 0:1, :].broadcast_to([128, 3, 128]),
                                                     in1=wa.unsqueeze(2).broadcast_to([128, 3, 128]), op=ALU.mult),
                      reads=[CST, CP], writes=[dg])
                fw.op(dve, lambda h: h.tensor_tensor(out=dg.ap[:, 3:34, :], in0=CSTt[:, 0:1, :].broadcast_to([128, 31, 128]),
                                                     in1=wb.unsqueeze(2).broadcast_to([128, 31, 128]), op=ALU.mult),
                      reads=[CST, CP], writes=[dg])
            build_dg(0)
            for jj in range(4):
                dg = l.DG[jj % 2]
                (ps,) = fw.psum(1)
                for k in range(3):
                    fw.op(pe, lambda h: h.matmul(ps.ap[:, 0:T], dg.ap[:, k, :], c.PBt[:, e, jj, k:k + T], start=(k == 0), stop=(k == 2)),
                          reads=[dg, c.PB[e]], writes=[ps])
                (ps2,) = fw.psum(1)
                for k in range(31):
                    fw.op(pe, lambda h: h.matmul(ps2.ap[:, 0:T], dg.ap[:, 3 + k, :], c.GLUt[:, e, jj, k:k + T], start=(k == 0), stop=(k == 30)),
                          reads=[dg, c.GLU[e]], writes=[ps2])
                if jj < 3:
                    build_dg(jj + 1)
                fw.op(dve, lambda h: h.tensor_tensor(out=MIX.ap[:, jj, :], in0=ps.ap[:, 0:T], in1=l.GB.ap[:, jj, :], op=ALU.mult),
                      reads=[ps, l.GB], writes=[MIX])
                bcol = cpcol("cb_bias", e * 4 + jj)
                fw.op(dve, lambda h: h.tensor_scalar(out=Z[jj].ap, in0=ps2.ap[:, 0:T], scalar1=bcol, scalar2=None, op0=ALU.add),
                      reads=[ps2, CP], writes=[Z[jj]])
            fw.op(dve, lambda h: h.tensor_copy(c.PBt[:, e, :, 0:2], c.PBt[:, e, :, T:T + 2]), reads=[c.PB[e]], writes=[c.PB[e]])
            fw.op(dve, lambda h: h.tensor_copy(c.GLUt[:, e, :, 0:30], c.GLUt[:, e, :, T:T + 30]), reads=[c.GLU[e]], writes=[c.GLU[e]])
            (pm,) = fw.psum(1)
            (pq,) = fw.psum(1)
            MU, VR = l.MU, l.VR
            for jj in range(4):
                zb = l.SQ[0]
                fw.op(act, lambda h: h.activation(out=zb.ap[:, 0:T], in_=Z[jj].ap, func=AF.Copy), reads=[Z[jj]], writes=[zb])
                fw.op(pe, lambda h: h.matmul(pm.ap[:, 0:T], ONESB, zb.ap[:, 0:T], start=(jj == 0), stop=(jj == 3)), reads=[zb, CSB], writes=[pm])
                zq = l.SQ[1]
                fw.op(act, lambda h: h.activation(out=zq.ap[:, 0:T], in_=Z[jj].ap, func=AF.Square), reads=[Z[jj]], writes=[zq])
                fw.op(pe, lambda h: h.matmul(pq.ap[:, 0:T], ONESB, zq.ap[:, 0:T], start=(jj == 0), stop=(jj == 3)), reads=[zq, CSB], writes=[pq])
            fw.op(act, lambda h: h.activation(out=MU.ap, in_=pm.ap[:, 0:T], func=AF.Copy, scale=1.0 / 512), reads=[pm], writes=[MU])
            fw.op(dve, lambda h: h.tensor_tensor(out=VR.ap, in0=MU.ap, in1=MU.ap, op=ALU.mult), reads=[MU], writes=[VR])
            fw.op(dve, lambda h: h.scalar_tensor_tensor(out=VR.ap, in0=pq.ap[:, 0:T], scalar=1.0 / 512, in1=VR.ap, op0=ALU.mult,
                                                        op1=ALU.subtract), reads=[pq, VR], writes=[VR])
            fw.op(dve, lambda h: h.tensor_scalar(out=VR.ap, in0=VR.ap, scalar1=EPS, scalar2=None, op0=ALU.add), reads=[VR], writes=[VR])
            fw.op(act, lambda h: h.activation(out=VR.ap, in_=VR.ap, func=AF.Sqrt), reads=[VR], writes=[VR])
            fw.op(dve, lambda h: h.reciprocal(out=VR.ap, in_=VR.ap), reads=[VR], writes=[VR])
            for jj in range(4):
                fw.op(dve, lambda h: h.tensor_tensor(out=Z[jj].ap, in0=Z[jj].ap, in1=MU.ap, op=ALU.subtract), reads=[Z[jj], MU], writes=[Z[jj]])
                fw.op(dve, lambda h: h.tensor_tensor(out=Z[jj].ap, in0=Z[jj].ap, in1=VR.ap, op=ALU.mult), reads=[Z[jj], VR], writes=[Z[jj]])
                gcol = cpcol("ln_g", e * 4 + jj)
                bcol = cpcol("ln_b", e * 4 + jj)
                fw.op(act, lambda h: h.activation(out=MIX.ap[:, 4 + jj, :], in_=Z[jj].ap, func=AF.Silu, bias=bcol, scale=gcol),
                      reads=[Z[jj], CP], writes=[MIX])
        yield from outproj_postnorm(cs, layer, 1, "a_w_out", e)

    def attn_sublayer(cs, layer):
        for c in cs:
            l = common_alloc(c)
            ar = c.ar
            l.Q = ar.alloc("Q", [KC, T], BF16)
            l.PT = [ar.alloc("PT%d" % i, [2, T], BF16) for i in range(2)]
            l.RC = [ar.alloc("RC%d" % i, [T], F32) for i in range(2)]
            l.KTb = ar.alloc("KTb%d" % c.s, [KC, NMEM], BF16)
            l.Vb = ar.alloc("Vb%d" % c.s, [2, D], BF16)
            fw.dma(sp, l.KTb.ap.rearrange("p a b -> p (a b)"), kvd[c.s, layer, :, 0:2048], reads=[c.KVD[layer]], writes=[l.KTb])
            fw.dma(sp, l.Vb.ap.rearrange("p a b -> p (a b)"), kvd[c.s, layer, :, 2048:4096], reads=[c.KVD[layer]], writes=[l.Vb])
            yield from prenorm(c, layer, 2)

        def evq(c, j, ps):
            fw.op(act, lambda h: h.activation(out=c.l.Q.ap[:, j, :], in_=ps.ap[:, 0:T], func=AF.Copy), reads=[ps], writes=[c.l.Q])
        yield from linear_fm(cs, lambda c: c.l.H, KC, "x_w_q", layer, 0, D, evq)
        for hd in range(4):
            for c in cs:
                l = c.l
                Q, MIX = l.Q, l.MIX
                pt = l.PT[hd % 2]
                rc = l.RC[hd % 2]
                for mc in range(2):
                    (ps,) = fw.psum(1)
                    for dj in range(2):
                        fw.op(pe, lambda h: h.matmul(ps.ap[:, 0:T], l.KTb.ap[:, 2 * hd + dj, mc * 128:(mc + 1) * 128], Q.ap[:, 2 * hd + dj, :],
                                                     start=(dj == 0), stop=(dj == 1)), reads=[l.KTb, Q], writes=[ps])
                    fw.op(act, lambda h: h.activation(out=pt.ap[:, mc, :], in_=ps.ap[:, 0:T], func=AF.Exp, scale=1.0 / 16.0),
                          reads=[ps], writes=[pt])
                yield None
                (pd,) = fw.psum(1)
                for mc in range(2):
                    fw.op(pe, lambda h: h.matmul(pd.ap[:, 0:T], ONESB, pt.ap[:, mc, :], start=(mc == 0), stop=(mc == 1)),
                          reads=[pt, CSB], writes=[pd])
                fw.op(dve, lambda h: h.reciprocal(out=rc.ap, in_=pd.ap[:, 0:T]), reads=[pd], writes=[rc])
                for dj in range(2):
                    (po,) = fw.psum(1)
                    for mc in range(2):
                        fw.op(pe, lambda h: h.matmul(po.ap[:, 0:T], l.Vb.ap[:, mc, (2 * hd + dj) * 128:(2 * hd + dj + 1) * 128], pt.ap[:, mc, :],
                                                     start=(mc == 0), stop=(mc == 1)), reads=[l.Vb, pt], writes=[po])
                    fw.op(dve, lambda h: h.tensor_tensor(out=MIX.ap[:, 2 * hd + dj, :], in0=po.ap[:, 0:T], in1=rc.ap, op=ALU.mult),
                          reads=[po, rc], writes=[MIX])
        yield from outproj_postnorm(cs, layer, 3, "x_w_o", layer)

    def ffn_sublayer(cs, layer):
        for c in cs:
            l = common_alloc(c)
            l.A = c.ar.alloc("A", [22, T], BF16)
            l.SG = [c.ar.alloc("SG%d" % i, [T], F32) for i in range(2)]
            yield from prenorm(c, layer, 4)
        for cc in range(11):
            slot, v = yield ("f_w_gu", layer, 0, KC, ((cc * 256, 256), (DFF + cc * 256, 256)))
            for jj in range(2):
                for c in cs:
                    l = c.l
                    H = l.H
                    (pg,) = fw.psum(1)
                    (pu,) = fw.psum(1)
                    for k in range(KC):
                        fw.op(pe, lambda h: h.matmul(pg.ap[:, 0:T], v[:, k, jj * 128:(jj + 1) * 128], H.ap[:, k, :],
                                                     start=(k == 0), stop=(k == KC - 1)), reads=[slot, H], writes=[pg])
                    for k in range(KC):
                        fw.op(pe, lambda h: h.matmul(pu.ap[:, 0:T], v[:, k, 256 + jj * 128:256 + (jj + 1) * 128], H.ap[:, k, :],
                                                     start=(k == 0), stop=(k == KC - 1)), reads=[slot, H], writes=[pu])
                    sg = l.SG[jj]
                    fw.op(act, lambda h: h.activation(out=sg.ap, in_=pg.ap[:, 0:T], func=AF.Silu), reads=[pg], writes=[sg])
                    fw.op(dve, lambda h: h.tensor_tensor(out=l.A.ap[:, 2 * cc + jj, :], in0=pu.ap[:, 0:T], in1=sg.ap, op=ALU.mult),
                          reads=[pu, sg], writes=[l.A])
        for m in range(8):
            slot, v = yield ("f_w_down", layer, 0, 22, ((m * 128, 128),))
            for c in cs:
                A = c.l.A
                (pb,) = fw.psum(1)
                for kc in range(22):
                    fw.op(pe, lambda h: h.matmul(pb.ap[:, 0:T], v[:, kc, :], A.ap[:, kc, :], start=(kc == 0), stop=(kc == 21)),
                          reads=[slot, A], writes=[pb])
                evac_y(c, m, pb, layer, 5)
        for c in cs:
            postnorm(c, layer, 5)
        yield None

    def mlstm_sublayer(cs, layer):
        o = layer // 2
        W = "m_w_in"
        for c in cs:
            l = common_alloc(c)
            ar = c.ar
            l.Q = ar.alloc("Q", [KC, T], BF16)
            l.KT = ar.alloc("KT", [TC, D], BF16)
            l.KTT = ar.alloc("KTT", [KC, T], BF16)
            l.VA = ar.alloc("VA", [TC, 8, 129], BF16)
            l.SO = ar.alloc("SO", [TC, D], BF16)
            l.GT = ar.alloc("GT", [TC, 16], F32)
            l.LL = ar.alloc("LL", [TC, 8], F32)
            l.EQ = ar.alloc("EQ", [TC, 8], F32)
            l.EK = ar.alloc("EK", [TC, 8], F32)
            l.GG = ar.alloc("GG", [TC, 8], F32)
            l.WT = [ar.alloc("WT%d" % i, [8, 128], BF16) for i in range(2)]
            l.HH = ar.alloc("HH", [8, 128], F32)
            l.H2 = ar.alloc("H2", [8, 128], F32)
            l.YT = ar.alloc("YT", [8, 128], BF16)
            l.CB = ar.alloc("CB", [8, 129], BF16)
            l.ST = [ar.alloc("ST%d" % i, [8], F32) for i in range(6)]
            yield from prenorm(c, layer, 0)

        def tm_linear(col0, ncols, evac):
            slot, v = yield (W, o, 0, KC, ((col0, ncols),))
            for c in cs:
                H = c.l.H
                for tc in range(TC):
                    (ps,) = fw.psum(1)
                    for k in range(KC):
                        fw.op(pe, lambda h: h.matmul(ps.ap[:, 0:ncols], H.ap[:, k, tc * 128:(tc + 1) * 128], v[:, k, :],
                                                     start=(k == 0), stop=(k == KC - 1)), reads=[slot, H], writes=[ps])
                    evac(c, tc, ps)

        def ev_g(c, tc, ps):
            fw.op(dve, lambda h: h.tensor_tensor(out=c.l.GT.ap[:, tc, :], in0=ps.ap[:, 0:16], in1=RPt[:, o, :], op=ALU.add),
                  reads=[ps, RP], writes=[c.l.GT])
        yield from tm_linear(4 * D, 16, ev_g)
        for c in cs:
            l = c.l
            GT, LL, EQ, EK, GG = l.GT, l.LL, l.EQ, l.EK, l.GG
            fw.op(act, lambda h: h.activation(out=LL.ap, in_=GT.ap[:, :, 8:16], func=AF.Exp, scale=-1.0), reads=[GT], writes=[LL])
            fw.op(act, lambda h: h.activation(out=LL.ap, in_=LL.ap, func=AF.Ln, bias=1.0), reads=[LL], writes=[LL])
            (pc,) = fw.psum(1)
            (pg,) = fw.psum(1)
            for tc in range(TC):
                fw.op(pe, lambda h: h.matmul(pc.ap[:, tc * 8:(tc + 1) * 8], TRIF, LL.ap[:, tc, :], start=True, stop=True),
                      reads=[CST, LL], writes=[pc])
                fw.op(pe, lambda h: h.matmul(pg.ap[:, tc * 8:(tc + 1) * 8], ONESF, LL.ap[:, tc, :], start=True, stop=True),
                      reads=[CST, LL], writes=[pg])
            pcv = pc.ap[:, 0:TC * 8].rearrange("p (a b) -> p a b", b=8)
            pgv = pg.ap[:, 0:TC * 8].rearrange("p (a b) -> p a b", b=8)
            fw.op(act, lambda h: h.activation(out=EQ.ap, in_=pcv, func=AF.Exp, scale=-1.0), reads=[pc], writes=[EQ])
            fw.op(act, lambda h: h.activation(out=GG.ap, in_=pgv, func=AF.Exp, scale=-1.0), reads=[pg], writes=[GG])
            fw.op(dve, lambda h: h.tensor_tensor(out=EK.ap, in0=pcv, in1=GT.ap[:, :, 0:8], op=ALU.add), reads=[pc, GT], writes=[EK])
            fw.op(act, lambda h: h.activation(out=EK.ap, in_=EK.ap, func=AF.Exp), reads=[EK], writes=[EK])

        def evq(c, j, ps):
            fw.op(act, lambda h: h.activation(out=c.l.Q.ap[:, j, :], in_=ps.ap[:, 0:T], func=AF.Copy), reads=[ps], writes=[c.l.Q])
        yield from linear_fm(cs, lambda c: c.l.H, KC, W, o, 0, D, evq)
        for cc in range(2):
            def ev_k(c, tc, ps):
                l = c.l
                fw.op(dve, lambda h: h.scalar_tensor_tensor(
                    out=l.KT.ap[:, tc, cc * 512:(cc + 1) * 512].rearrange("p (a b) -> p a b", b=128),
                    in0=ps.ap.rearrange("p (a b) -> p a b", b=128), scalar=KSCALE,
                    in1=l.EK.ap[:, tc, cc * 4:(cc + 1) * 4].unsqueeze(2).broadcast_to([128, 4, 128]),
                    op0=ALU.mult, op1=ALU.mult), reads=[ps, l.EK], writes=[l.KT])
            yield from tm_linear(D + cc * 512, 512, ev_k)
        for c in cs:
            fw.op(dve, lambda h: h.memset(c.l.VA.ap[:, :, :, 128:129], 1.0), writes=[c.l.VA])
        for cc in range(2):
            def ev_v(c, tc, ps):
                fw.op(act, lambda h: h.activation(out=c.l.VA.ap[:, tc, cc * 4:(cc + 1) * 4, 0:128],
                                                  in_=ps.ap.rearrange("p (a b) -> p a b", b=128), func=AF.Copy),
                      reads=[ps], writes=[c.l.VA])
            yield from tm_linear(2 * D + cc * 512, 512, ev_v)
        for cc in range(2):
            def ev_o(c, tc, ps):
                fw.op(act, lambda h: h.activation(out=c.l.SO.ap[:, tc, cc * 512:(cc + 1) * 512], in_=ps.ap, func=AF.Sigmoid),
                      reads=[ps], writes=[c.l.SO])
            yield from tm_linear(3 * D + cc * 512, 512, ev_o)
        for c in cs:
            l = c.l
            for tc in range(TC):
                (pt,) = fw.psum(1)
                ptb = pt.ap.bitcast(BF16)
                for hd in range(8):
                    fw.op(pe, lambda h: h.transpose(ptb[:, hd * 128:(hd + 1) * 128], l.KT.ap[:, tc, hd * 128:(hd + 1) * 128], IDENTB),
                          reads=[l.KT, CSB], writes=[pt])
                fw.op(act, lambda h: h.activation(out=l.KTT.ap[:, :, tc * 128:(tc + 1) * 128],
                                                  in_=ptb.rearrange("p (a b) -> p a b", b=128), func=AF.Copy),
                      reads=[pt], writes=[l.KTT])
            fw.op(act, lambda h: h.activation(out=l.CB.ap, in_=c.C32t[:, o], func=AF.Copy), reads=[c.C32[o]], writes=[l.CB])
        for tc in range(TC):
            for c in cs:
                yield from chunk(c, o, tc)
        yield from outproj_postnorm(cs, layer, 1, "m_w_out", o)

    def chunk(c, o, tc):
        l = c.l
        Q, KT, KTT, VA, SO, EQ, GG, HH, H2, YT, CB, MIX = l.Q, l.KT, l.KTT, l.VA, l.SO, l.EQ, l.GG, l.HH, l.H2, l.YT, l.CB, l.MIX
        C32t, C32b = c.C32t, c.C32[o]
        sl = slice(tc * 128, (tc + 1) * 128)
        wt = l.WT[tc % 2]
        pss = fw.psum(2)
        for hd in range(8):
            b = pss[hd // 4]
            fw.op(pe, lambda h: h.matmul(b.ap[:, (hd % 4) * 128:(hd % 4 + 1) * 128], KTT.ap[:, hd, sl], Q.ap[:, hd, sl],
                                         start=True, stop=True), reads=[KTT, Q], writes=[b])
        for g2 in range(2):
            fw.op(dve, lambda h: h.tensor_tensor(out=wt.ap[:, g2 * 4:(g2 + 1) * 4, :],
                                                 in0=pss[g2].ap.rearrange("p (a b) -> p a b", b=128),
                                                 in1=CSTt[:, 1:2, :].broadcast_to([128, 4, 128]), op=ALU.mult),
                  reads=[pss[g2], CST], writes=[wt])
        psp = fw.psum(3)
        for hd in range(8):
            bp = psp[hd // 3]
            cs_ = slice((hd % 3) * 129, (hd % 3 + 1) * 129)
            fw.op(pe, lambda h: h.matmul(bp.ap[:, cs_], KT.ap[:, tc, hd * 128:(hd + 1) * 128], VA.ap[:, tc, hd, :],
                                         start=True, stop=True), reads=[KT, VA], writes=[bp])
        for g3 in range(3):
            nh = 3 if g3 < 2 else 2
            hs = slice(g3 * 3, g3 * 3 + nh)
            fw.op(dve, lambda h: h.tensor_tensor(out=C32t[:, o, hs, :], in0=psp[g3].ap[:, 0:nh * 129].rearrange("p (a b) -> p a b", b=129),
                                                 in1=C32t[:, o, hs, :], op=ALU.add), reads=[psp[g3], C32b], writes=[C32b])
        yield None
        psn = fw.psum(3)
        for hd in range(8):
            bn = psn[hd // 3]
            cs_ = slice((hd % 3) * 129, (hd % 3 + 1) * 129)
            fw.op(pe, lambda h: h.matmul(bn.ap[:, cs_], Q.ap[:, hd, sl], CB.ap[:, hd, :], start=True, stop=False),
                  reads=[Q, CB], writes=[bn])
            fw.op(pe, lambda h: h.matmul(bn.ap[:, cs_], wt.ap[:, hd, :], VA.ap[:, tc, hd, :], start=False, stop=True),
                  reads=[wt, VA], writes=[bn])
        fw.op(dve, lambda h: h.tensor_tensor(out=C32t[:, o], in0=C32t[:, o],
                                             in1=GG.ap[:, tc, :].unsqueeze(2).broadcast_to([128, 8, 129]), op=ALU.mult),
              reads=[C32b, GG], writes=[C32b])
        fw.op(act, lambda h: h.activation(out=CB.ap, in_=C32t[:, o], func=AF.Copy), reads=[C32b], writes=[CB])
        DN, RR, S1, S2, MUh, RS = l.ST
        for g3 in range(3):
            nh = 3 if g3 < 2 else 2
            hs = slice(g3 * 3, g3 * 3 + nh)
            v3 = psn[g3].ap[:, 0:nh * 129].rearrange("p (a b) -> p a b", b=129)
            fw.op(act, lambda h: h.activation(out=DN.ap[:, hs].unsqueeze(2), in_=v3[:, :, 128:129], func=AF.Abs),
                  reads=[psn[g3]], writes=[DN])
        fw.op(dve, lambda h: h.tensor_tensor(out=DN.ap, in0=DN.ap, in1=EQ.ap[:, tc, :], op=ALU.mult), reads=[DN, EQ], writes=[DN])
        fw.op(dve, lambda h: h.tensor_scalar(out=DN.ap, in0=DN.ap, scalar1=1.0, scalar2=None, op0=ALU.max), reads=[DN], writes=[DN])
        fw.op(dve, lambda h: h.reciprocal(out=DN.ap, in_=DN.ap), reads=[DN], writes=[DN])
        fw.op(dve, lambda h: h.tensor_tensor(out=RR.ap, in0=EQ.ap[:, tc, :], in1=DN.ap, op=ALU.mult), reads=[DN, EQ], writes=[RR])
        for g3 in range(3):
            nh = 3 if g3 < 2 else 2
            hs = slice(g3 * 3, g3 * 3 + nh)
            v3 = psn[g3].ap[:, 0:nh * 129].rearrange("p (a b) -> p a b", b=129)
            fw.op(dve, lambda h: h.tensor_tensor(out=HH.ap[:, hs, :], in0=v3[:, :, 0:128],
                                                 in1=RR.ap[:, hs].unsqueeze(2).broadcast_to([128, nh, 128]), op=ALU.mult),
                  reads=[psn[g3], RR], writes=[HH])
        fw.op(dve, lambda h: h.tensor_reduce(out=S1.ap, in_=HH.ap, axis=AX.X, op=ALU.add), reads=[HH], writes=[S1])
        fw.op(act, lambda h: h.activation(out=H2.ap, in_=HH.ap, func=AF.Square), reads=[HH], writes=[H2])
        fw.op(dve, lambda h: h.tensor_reduce(out=S2.ap, in_=H2.ap, axis=AX.X, op=ALU.add), reads=[H2], writes=[S2])
        fw.op(dve, lambda h: h.tensor_scalar(out=MUh.ap, in0=S1.ap, scalar1=1.0 / 128, scalar2=None, op0=ALU.mult), reads=[S1], writes=[MUh])
        fw.op(dve, lambda h: h.tensor_tensor(out=S1.ap, in0=MUh.ap, in1=MUh.ap, op=ALU.mult), reads=[MUh], writes=[S1])
        fw.op(dve, lambda h: h.scalar_tensor_tensor(out=RS.ap, in0=S2.ap, scalar=1.0 / 128, in1=S1.ap, op0=ALU.mult, op1=ALU.subtract),
              reads=[S2, S1], writes=[RS])
        fw.op(dve, lambda h: h.tensor_scalar(out=RS.ap, in0=RS.ap, scalar1=EPS, scalar2=None, op0=ALU.add), reads=[RS], writes=[RS])
        fw.op(act, lambda h: h.activation(out=RS.ap, in_=RS.ap, func=AF.Sqrt), reads=[RS], writes=[RS])
        fw.op(dve, lambda h: h.reciprocal(out=RS.ap, in_=RS.ap), reads=[RS], writes=[RS])
        fw.op(dve, lambda h: h.tensor_tensor(out=HH.ap, in0=HH.ap, in1=MUh.ap.unsqueeze(2).broadcast_to([128, 8, 128]), op=ALU.subtract),
              reads=[HH, MUh], writes=[HH])
        fw.op(dve, lambda h: h.tensor_tensor(out=HH.ap, in0=HH.ap, in1=RS.ap.unsqueeze(2).broadcast_to([128, 8, 128]), op=ALU.mult),
              reads=[HH, RS], writes=[HH])
        fw.op(dve, lambda h: h.tensor_tensor(out=YT.ap, in0=HH.ap, in1=SO.ap[:, tc, :].rearrange("p (a b) -> p a b", b=128), op=ALU.mult),
              reads=[HH, SO], writes=[YT])
        yield None
        (py,) = fw.psum(1)
        pyb = py.ap.bitcast(BF16)
        for hd in range(8):
            fw.op(pe, lambda h: h.transpose(pyb[:, hd * 128:(hd + 1) * 128], YT.ap[:, hd, :], IDENTB), reads=[YT, CSB], writes=[py])
        mg = CPt[:, CPO["m_g"] + o * 8: CPO["m_g"] + o * 8 + 8]
        fw.op(dve, lambda h: h.tensor_tensor(out=MIX.ap[:, :, sl], in0=pyb.rearrange("p (a b) -> p a b", b=128),
                                             in1=mg.unsqueeze(2).broadcast_to([128, 8, 128]), op=ALU.mult),
              reads=[py, CP], writes=[MIX])

    def ctx_program(c):
        cs = [c]
        for o in range(2):
            fw.op(dve, lambda h: h.memset(c.C32t[:, o], 0.0), writes=[c.C32[o]])
        for e in range(2):
            fw.op(dve, lambda h: h.memset(c.GLUt[:, e], 0.0), writes=[c.GLU[e]])
            fw.op(dve, lambda h: h.memset(c.PBt[:, e], 0.0), writes=[c.PB[e]])
        for layer in range(DEPTH):
            yield from kv_prep(cs, layer)
        for t in range(NT):
            fw.dma(sp, c.Xt[:], io["x"][c.s][:, t * T:(t + 1) * T].rearrange("(k p) t -> p k t", p=128), writes=[c.X])
            for layer in range(DEPTH):
                if layer % 2 == 0:
                    yield from conv_sublayer(cs, layer)
                else:
                    yield from mlstm_sublayer(cs, layer)
                yield from attn_sublayer(cs, layer)
                yield from ffn_sublayer(cs, layer)
            fw.dma(sp, io["y"][c.s][:, t * T:(t + 1) * T].rearrange("(k p) t -> p k t", p=128), c.Xt[:], reads=[c.X])

    run = ctxs[:1] if dry else ctxs
    gens = [ctx_program(c) for c in run]
    n = len(gens)
    nxt = [None] * n
    blk = [0] * n
    prog = [0] * n
    done = [False] * n
    for i, g in enumerate(gens):
        try:
            nxt[i] = next(g)
        except StopIteration:
            done[i] = True
    while not all(done):
        if n == 1 or done[1]:
            i = 0
        elif done[0]:
            i = 1
        else:
            i = 0 if (prog[0] - prog[1]) < SKEW else 1
        if nxt[i] is None:
            val = None
        else:
            bmin = min(blk[j] for j in range(n) if not done[j])
            val = wq.get(blk[i], nxt[i], bmin)
            blk[i] += 1
        prog[i] += 1
        try:
            nxt[i] = gens[i].send(val)
        except StopIteration:
            done[i] = True
    fw.wait_all(sp, [c.X for c in ctxs])
    return fw


def build_nc(NSEQ=2, NT=8, DEPTH=4):
    S = NT * T
    nc = bass.Bass("TRN2", target_bir_lowering=False)

    def din(name, shape):
        return nc.dram_tensor(name, shape, F32, kind="ExternalInput").ap()
    io = {}
    io["x"] = din("x", [NSEQ, D, S])
    io["mem"] = din("mem", [NSEQ, D, NMEM])
    io["consts"] = din("consts", [128, 3, 128])
    io["cp"] = din("cp", [128, NCP])
    io["rp"] = din("rp", [128, 2, 16])
    io["w"] = {
        "a_w_in": din("a_w_in", [2, D, 2560]), "a_w_out": din("a_w_out", [2, D, D]),
        "m_w_in": din("m_w_in", [2, D, 4112]), "m_w_out": din("m_w_out", [2, D, D]),
        "x_w_q": din("x_w_q", [4, D, D]), "x_w_kv": din("x_w_kv", [4, D, 2 * D]), "x_w_o": din("x_w_o", [4, D, D]),
        "f_w_gu": din("f_w_gu", [4, D, 2 * DFF]), "f_w_down": din("f_w_down", [4, DFF, D]),
    }
    io["y"] = nc.dram_tensor("y", [NSEQ, D, S], F32, kind="ExternalOutput").ap()
    io["kvd"] = nc.dram_tensor("kvd", [NSEQ, 4, 128, 4096], BF16, kind="Internal").ap()
    plan = []
    with contextlib.ExitStack() as es:
        _emit(nc, es, True, plan, NSEQ, NT, DEPTH, io)
    es2 = contextlib.ExitStack()
    with es2:
        fw = _emit(nc, es2, False, plan, NSEQ, NT, DEPTH, io)
    return nc, fw


def host_tables(inp):
    cp = np.zeros((128, NCP), np.float32)

    def put(name, idx, vec128):
        cp[:, CPO[name] + idx] = vec128
    ng = np.asarray(inp["norm_g"], np.float32)
    for l in range(4):
        for j in range(6):
            for k in range(8):
                put("norm_g", (l * 6 + j) * 8 + k, ng[l, j, k * 128:(k + 1) * 128])
    mg = np.asarray(inp["mem_norm_g"], np.float32)
    for l in range(4):
        for k in range(8):
            put("mem_g", l * 8 + k, mg[l, k * 128:(k + 1) * 128])
    ca = np.asarray(inp["a_conv_a"], np.float32)
    cb = np.asarray(inp["a_conv_b"], np.float32)
    cbb = np.asarray(inp["a_conv_b_bias"], np.float32)
    lg = np.asarray(inp["a_ln_g"], np.float32)
    lb = np.asarray(inp["a_ln_b"], np.float32)
    for e in range(2):
        for c in range(4):
            for k in range(3):
                put("conv_a", (e * 4 + c) * 3 + k, ca[e, k, c * 128:(c + 1) * 128])
            for k in range(31):
                put("conv_b", (e * 4 + c) * 31 + k, cb[e, k, c * 128:(c + 1) * 128])
            put("cb_bias", e * 4 + c, cbb[e, c * 128:(c + 1) * 128])
            put("ln_g", e * 4 + c, lg[e, c * 128:(c + 1) * 128])
            put("ln_b", e * 4 + c, lb[e, c * 128:(c + 1) * 128])
    mng = np.asarray(inp["m_norm_g"], np.float32)
    for o in range(2):
        for k in range(8):
            put("m_g", o * 8 + k, mng[o, k * 128:(k + 1) * 128])
    rp = np.zeros((128, 2, 16), np.float32)
    rp[:, :, 0:8] = np.asarray(inp["m_i_bias"], np.float32)[None]
    rp[:, :, 8:16] = np.asarray(inp["m_f_bias"], np.float32)[None]
    consts = np.zeros((128, 3, 128), np.float32)
    consts[:, 0, :] = np.eye(128, dtype=np.float32)
    consts[:, 1, :] = np.triu(np.ones((128, 128), np.float32))
    consts[:, 2, :] = 1.0
    return cp, rp, consts


WNAMES = ["a_w_in", "a_w_out", "m_w_in", "m_w_out", "x_w_q", "x_w_kv", "x_w_o", "f_w_gu", "f_w_down"]


def kernel(**inp):
    x = np.asarray(inp["x"], np.float32)
    mem = np.asarray(inp["mem"], np.float32)
    B = x.shape[0]
    nseq = B // NCORES
    cp, rp, consts = host_tables(inp)
    nc, _ = build_nc(NSEQ=nseq, NT=x.shape[1] // T, DEPTH=4)
    shared = {"consts": consts, "cp": cp, "rp": rp}
    for w in WNAMES:
        shared[w] = np.ascontiguousarray(np.asarray(inp[w], np.float32))
    in_maps = []
    for c in range(NCORES):
        m = dict(shared)
        m["x"] = np.ascontiguousarray(x[c * nseq:(c + 1) * nseq].transpose(0, 2, 1))
        m["mem"] = np.ascontiguousarray(mem[c * nseq:(c + 1) * nseq].transpose(0, 2, 1))
        in_maps.append(m)
    res = run_bass_kernel_spmd(nc, in_maps, core_ids=list(range(NCORES)))
    out = np.empty_like(x)
    for c in range(NCORES):
        y = res.results[c]["y"]
        out[c * nseq:(c + 1) * nseq] = y.transpose(0, 2, 1)
    return out
```

```python
import contextlib
import numpy as np
import concourse.bass as bass
import concourse.mybir as mybir
from concourse.bass_utils import run_bass_kernel_spmd

F32 = mybir.dt.float32
BF16 = mybir.dt.bfloat16
AF = mybir.ActivationFunctionType
ALU = mybir.AluOpType
AX = mybir.AxisListType

D = 1024
KC = 8
T = 256
NMEM = 256
DFF = 2816
SEQ = 4096
NCORES = 8
EPS = 1e-6
NRING = 5
SKEW = 2
SLOT = 4096
KSCALE = 128.0 ** -0.5

def _cp_layout():
    off = {}
    n = 0
    for name, cnt in (("norm_g", 4 * 6 * 8), ("mem_g", 4 * 8), ("conv_a", 2 * 4 * 3), ("conv_b", 2 * 4 * 31),
                      ("cb_bias", 2 * 4), ("ln_g", 2 * 4), ("ln_b", 2 * 4), ("m_g", 2 * 8)):
        off[name] = n
        n += cnt
    return off, n


CPO, NCP = _cp_layout()


class Ctr:
    LIMIT = 16000

    def __init__(self, fw, name, owner=None):
        self.fw, self.name, self.owner, self.k, self.val = fw, name, owner, 0, 0
        self.sem = fw.new_sem(name + "_0")

    def bump(self, inc):
        if self.val + inc > self.LIMIT:
            self.k += 1
            self.sem = self.fw.new_sem("%s_%d" % (self.name, self.k))
            self.val = 0
        self.val += inc
        return (self.sem, self.val, self.owner)


class Eng:
    def __init__(self, fw, name, h, selfsync=True):
        self.h, self.name, self.selfsync = h, name, selfsync
        self.ctr = Ctr(fw, name, self)
        self.waited = {}


class Buf:
    def __init__(self, name, ap=None):
        self.name, self.ap = name, ap
        self.w = None
        self.r = {}
        self.dctr = None

    def deps(self):
        d = list(self.r.values())
        if self.w is not None:
            d.append(self.w)
        return d


class FW:
    def __init__(self, nc, es, dry):
        self.nc, self.es, self.dry = nc, es, dry
        self.nsem = 0
        self.nops = 0
        self.dctrs = {}
        if not dry:
            self.pe = Eng(self, "pe", nc.tensor, selfsync=False)
            self.act = Eng(self, "act", nc.scalar)
            self.dve = Eng(self, "dve", nc.vector)
            self.pool = Eng(self, "pool", nc.gpsimd)
            self.sp = Eng(self, "sp", nc.sync)
        else:
            self.pe = self.act = self.dve = self.pool = self.sp = None
        self.psbanks = None
        self.psptr = 0

    def new_sem(self, name):
        if self.dry:
            return None
        self.nsem += 1
        return self.es.enter_context(self.nc.semaphore(name))

    def _wait(self, eng, deps):
        for (sem, val, owner) in deps:
            if owner is eng and not eng.selfsync:
                continue
            k = id(sem)
            if eng.waited.get(k, 0) < val:
                eng.h.wait_ge(sem, val)
                eng.waited[k] = val

    def _deps(self, reads, writes):
        deps = []
        for b in reads:
            if b.w is not None:
                deps.append(b.w)
        for b in writes:
            deps.extend(b.deps())
        return deps

    def _commit(self, d, reads, writes):
        k = id(d[0])
        for b in reads:
            old = b.r.get(k)
            if old is None or old[1] < d[1]:
                b.r[k] = d
        for b in writes:
            b.w = d
            b.r = {}

    def op(self, eng, fn, reads=(), writes=()):
        if self.dry:
            return
        self._wait(eng, self._deps(reads, writes))
        ins = fn(eng.h)
        d = eng.ctr.bump(1)
        ins.then_inc(d[0], 1)
        self._commit(d, reads, writes)
        self.nops += 1

    def dma(self, eng, out, in_, reads=(), writes=()):
        if self.dry:
            return
        self._wait(eng, self._deps(reads, writes))
        ins = eng.h.dma_start(out=out, in_=in_)
        b = writes[0] if writes else reads[0]
        if b.dctr is None:
            if b.name not in self.dctrs:
                self.dctrs[b.name] = Ctr(self, "d_" + b.name, None)
            b.dctr = self.dctrs[b.name]
        d = b.dctr.bump(16)
        ins.then_inc(d[0], 16)
        self._commit(d, reads, writes)
        self.nops += 1

    def wait_all(self, eng, bufs):
        if self.dry:
            return
        deps = []
        for b in bufs:
            deps.extend(b.deps())
        self._wait(eng, deps)

    def psum(self, n=1):
        if self.psptr + n > 8:
            self.psptr = 0
        bs = self.psbanks[self.psptr:self.psptr + n]
        self.psptr = (self.psptr + n) % 8
        return bs


class Arena:
    def __init__(self, fw, tensor, nelem):
        self.fw, self.t, self.n = fw, tensor, nelem
        self.hist = []
        self.cur = []
        self.off = 0

    def reset(self):
        newh = list(self.cur)
        for (a, b, buf) in self.hist:
            if not any(a < cb and ca < b for (ca, cb, _) in self.cur):
                newh.append((a, b, buf))
        self.hist = newh
        self.cur = []
        self.off = 0

    def alloc(self, name, shape, dtype):
        n = 1
        for s in shape:
            n *= s
        nb = n * (2 if dtype == F32 else 1)
        nb = (nb + 15) // 16 * 16
        a, b = self.off, self.off + nb
        assert b <= self.n, "arena overflow %s %d > %d" % (name, b, self.n)
        self.off = b
        ap = self.t[:, a:a + n * (2 if dtype == F32 else 1)]
        if dtype == F32:
            ap = ap.bitcast(F32)
        if len(shape) == 2:
            ap = ap.rearrange("p (a b) -> p a b", b=shape[1])
        elif len(shape) == 3:
            ap = ap.rearrange("p (a b c) -> p a b c", b=shape[1], c=shape[2])
        buf = Buf(name, ap)
        for (ha, hb, hbuf) in self.hist:
            if ha < b and a < hb:
                for d in hbuf.deps():
                    k = id(d[0])
                    old = buf.r.get(k)
                    if old is None or old[1] < d[1]:
                        buf.r[k] = d
        self.cur.append((a, b, buf))
        return buf


class WQ:
    def __init__(self, fw, slots, wdram, plan):
        self.fw, self.slots, self.wdram = fw, slots, wdram
        self.plan = plan
        self.i = 0
        self.issued = 0

    def _view(self, slot, nkc, ncols):
        return slot.ap[:, 0:nkc * ncols].rearrange("p (k m) -> p k m", m=ncols)

    def _issue(self, j):
        (wname, li, row0, nkc, segs) = self.plan[j]
        slot = self.slots[j % NRING]
        ncols = sum(n for (_, n) in segs)
        v = self._view(slot, nkc, ncols)
        W = self.wdram[wname][li]
        off = 0
        for (c0, n) in segs:
            src = W[row0:row0 + nkc * 128, c0:c0 + n].rearrange("(k p) m -> p k m", p=128)
            self.fw.dma(self.fw.pool, v[:, :, off:off + n], src, writes=[slot])
            off += n

    def get(self, k, spec, bmin):
        (wname, li, row0, nkc, segs) = spec
        ncols = sum(n for (_, n) in segs)
        assert nkc * ncols <= SLOT
        if self.fw.dry:
            assert k == len(self.plan)
            self.plan.append(spec)
            return self.slots[0], self._view(self.slots[0], nkc, ncols)
        assert self.plan[k] == spec, (k, self.plan[k], spec)
        while self.issued < min(len(self.plan), bmin + NRING):
            self._issue(self.issued)
            self.issued += 1
        assert k < self.issued
        slot = self.slots[k % NRING]
        return slot, self._view(slot, nkc, ncols)


class Ctx:
    pass


class L:
    pass


def _emit(nc, es, dry, plan, NSEQ, NT, DEPTH, io):
    fw = FW(nc, es, dry)
    E = es.enter_context
    sfx = "d" if dry else "r"
    TC = T // 128

    def sb(name, shape, dt):
        return E(nc.sbuf_tensor(name + sfx, shape, dt))

    ring = [Buf("ring%d" % i, sb("ring%d" % i, [128, SLOT], BF16)[:]) for i in range(NRING)]
    CSTt = sb("CST", [128, 3, 128], F32)
    CSBt = sb("CSB", [128, 3, 128], BF16)
    CST = Buf("CST", CSTt[:])
    CSB = Buf("CSB", CSBt[:])
    CPt = sb("CP", [128, NCP], F32)
    CP = Buf("CP", CPt[:])
    RPt = sb("RP", [128, 2, 16], F32)
    RP = Buf("RP", RPt[:])
    NA = 28544
    ctxs = []
    for s in range(NSEQ):
        c = Ctx()
        c.s = s
        c.Xt = sb("X%d" % s, [128, KC, T], F32)
        c.X = Buf("X%d" % s, c.Xt[:])
        c.C32t = sb("C32_%d" % s, [128, 2, 8, 129], F32)
        c.C32 = [Buf("C32_%d_%d" % (s, o), c.C32t[:, o]) for o in range(2)]
        c.GLUt = sb("GLU%d" % s, [128, 2, 4, 30 + T], BF16)
        c.PBt = sb("PB%d" % s, [128, 2, 4, 2 + T], BF16)
        c.GLU = [Buf("GLU%d_%d" % (s, e), c.GLUt[:, e]) for e in range(2)]
        c.PB = [Buf("PB%d_%d" % (s, e), c.PBt[:, e]) for e in range(2)]
        c.ARt = sb("AR%d" % s, [128, NA], BF16)
        c.ar = Arena(fw, c.ARt, NA)
        c.KVD = [Buf("kvd%d_%d" % (s, l)) for l in range(4)]
        ctxs.append(c)
    PSt = E(nc.psum_tensor("PS" + sfx, [128, 8, 512], F32))
    fw.psbanks = [Buf("ps%d" % i, PSt[:, i, :]) for i in range(8)]
    kvd = io["kvd"]

    wq = WQ(fw, ring, io["w"], plan)
    pe, act, dve, pool, sp = fw.pe, fw.act, fw.dve, fw.pool, fw.sp

    IDENTB = CSBt[:, 0, :]
    ONESB = CSBt[:, 2, :]
    TRIF = CSTt[:, 1, :]
    ONESF = CSTt[:, 2, :]

    def cpcol(name, idx):
        cc = CPO[name] + idx
        return CPt[:, cc:cc + 1]

    fw.dma(sp, CSTt[:], io["consts"], writes=[CST])
    fw.dma(sp, CPt[:], io["cp"], writes=[CP])
    fw.dma(sp, RPt[:], io["rp"], writes=[RP])
    fw.op(dve, lambda h: h.tensor_copy(CSBt[:], CSTt[:]), reads=[CST], writes=[CSB])

    def rms_rstd(srcs, srcbufs, nk, ncols, RB, SQ, dn):
        (ps,) = fw.psum(1)
        for k in range(nk):
            sq = SQ[k % 2]
            fw.op(act, lambda h: h.activation(out=sq.ap[:, 0:ncols], in_=srcs[k], func=AF.Square),
                  reads=[srcbufs[k]], writes=[sq])
            fw.op(pe, lambda h: h.matmul(ps.ap[:, 0:ncols], ONESB, sq.ap[:, 0:ncols], start=(k == 0), stop=(k == nk - 1)),
                  reads=[sq, CSB], writes=[ps])
        fw.op(dve, lambda h: h.tensor_scalar(out=RB.ap[:, 0:ncols], in0=ps.ap[:, 0:ncols], scalar1=1.0 / dn,
                                             scalar2=EPS, op0=ALU.mult, op1=ALU.add), reads=[ps], writes=[RB])
        fw.op(act, lambda h: h.activation(out=RB.ap[:, 0:ncols], in_=RB.ap[:, 0:ncols], func=AF.Sqrt),
              reads=[RB], writes=[RB])
        fw.op(dve, lambda h: h.reciprocal(out=RB.ap[:, 0:ncols], in_=RB.ap[:, 0:ncols]), reads=[RB], writes=[RB])

    def common_alloc(c):
        l = L()
        ar = c.ar
        ar.reset()
        l.H = ar.alloc("H", [KC, T], BF16)
        l.SQ = [ar.alloc("SQ%d" % i, [NMEM], BF16) for i in range(2)]
        l.RB = ar.alloc("RB", [NMEM], F32)
        l.RB2 = ar.alloc("RB2", [T], F32)
        l.YA = ar.alloc("YA", [KC, T], F32)
        l.SQA = ar.alloc("SQA", [KC, T], BF16)
        l.MIX = l.H
        c.l = l
        return l

    def rstd_from_ps(ps, RB):
        fw.op(act, lambda h: h.activation(out=RB.ap[:, 0:T], in_=ps.ap[:, 0:T], func=AF.Ln, scale=1.0 / D, bias=EPS),
              reads=[ps], writes=[RB])
        fw.op(act, lambda h: h.activation(out=RB.ap[:, 0:T], in_=RB.ap[:, 0:T], func=AF.Exp, scale=-0.5),
              reads=[RB], writes=[RB])

    def prenorm(c, layer, j):
        l = c.l
        fw.op(act, lambda h: h.activation(out=l.SQA.ap, in_=c.Xt[:], func=AF.Square), reads=[c.X], writes=[l.SQA])
        (ps,) = fw.psum(1)
        for k in range(KC):
            fw.op(pe, lambda h: h.matmul(ps.ap[:, 0:T], ONESB, l.SQA.ap[:, k, :], start=(k == 0), stop=(k == KC - 1)),
                  reads=[l.SQA, CSB], writes=[ps])
        rstd_from_ps(ps, l.RB)
        yield None
        for k in range(KC):
            g = cpcol("norm_g", (layer * 6 + j) * 8 + k)
            fw.op(dve, lambda h: h.scalar_tensor_tensor(out=l.H.ap[:, k, :], in0=c.Xt[:, k, :], scalar=g,
                                                        in1=l.RB.ap[:, 0:T], op0=ALU.mult, op1=ALU.mult),
                  reads=[c.X, l.RB, CP], writes=[l.H])

    def linear_fm(cs, getsrc, nk, wname, li, col0, ncols_total, evac, blk=512):
        j = 0
        for cc in range(0, ncols_total, blk):
            n = min(blk, ncols_total - cc)
            slot, v = yield (wname, li, 0, nk, ((col0 + cc, n),))
            for m in range(n // 128):
                for c in cs:
                    src = getsrc(c)
                    (ps,) = fw.psum(1)
                    for k in range(nk):
                        fw.op(pe, lambda h: h.matmul(ps.ap[:, 0:T], v[:, k, m * 128:(m + 1) * 128], src.ap[:, k, :],
                                                     start=(k == 0), stop=(k == nk - 1)),
                              reads=[slot, src], writes=[ps])
                    evac(c, j, ps)
                j += 1

    def evac_y(c, j, ps, layer, jg):
        l = c.l
        g = cpcol("norm_g", (layer * 6 + jg) * 8 + j)
        sq = l.SQ[j % 2]
        fw.op(act, lambda h: h.activation(out=l.YA.ap[:, j, :], in_=ps.ap[:, 0:T], func=AF.Copy, scale=g),
              reads=[ps, CP], writes=[l.YA])
        fw.op(act, lambda h: h.activation(out=sq.ap[:, 0:T], in_=ps.ap[:, 0:T], func=AF.Square), reads=[ps], writes=[sq])
        flush_stats(c)

        def deferred():
            (pst,) = fw.psum(1)
            fw.op(pe, lambda h: h.matmul(pst.ap[:, 0:T], ONESB, sq.ap[:, 0:T], start=True, stop=True), reads=[sq, CSB], writes=[pst])
            if j == 0:
                fw.op(dve, lambda h: h.tensor_copy(l.RB2.ap, pst.ap[:, 0:T]), reads=[pst], writes=[l.RB2])
            else:
                fw.op(dve, lambda h: h.tensor_tensor(out=l.RB2.ap, in0=pst.ap[:, 0:T], in1=l.RB2.ap, op=ALU.add),
                      reads=[pst, l.RB2], writes=[l.RB2])
        l.pending = deferred

    def flush_stats(c):
        f = getattr(c.l, "pending", None)
        if f is not None:
            c.l.pending = None
            f()

    def postnorm(c, layer, jg):
        l = c.l
        flush_stats(c)
        rstd_from_ps(l.RB2, l.RB2)
        fw.op(dve, lambda h: h.tensor_tensor(out=l.YA.ap, in0=l.YA.ap, in1=l.RB2.ap.unsqueeze(1).broadcast_to([128, KC, T]), op=ALU.mult),
              reads=[l.YA, l.RB2], writes=[l.YA])
        fw.op(dve, lambda h: h.tensor_tensor(out=c.Xt[:], in0=c.Xt[:], in1=l.YA.ap, op=ALU.add), reads=[c.X, l.YA], writes=[c.X])

    def outproj_postnorm(cs, layer, jg, wname, li):
        def ev(c, j, ps):
            evac_y(c, j, ps, layer, jg)
        yield from linear_fm(cs, lambda c: c.l.MIX, KC, wname, li, 0, D, ev)
        yield None
        for c in cs:
            postnorm(c, layer, jg)
        yield None

    def kv_prep(cs, layer):
        for c in cs:
            ar = c.ar
            ar.reset()
            l = L()
            c.l = l
            l.MT = ar.alloc("MT%d" % c.s, [KC, NMEM], F32)
            l.HM = ar.alloc("HM", [KC, NMEM], BF16)
            l.SQ = [ar.alloc("SQ%d" % i, [NMEM], BF16) for i in range(2)]
            l.RB = ar.alloc("RB", [NMEM], F32)
            l.KTb = ar.alloc("KTs%d" % c.s, [KC, NMEM], BF16)
            l.Vb = ar.alloc("Vs%d" % c.s, [2, D], BF16)
            fw.dma(sp, l.MT.ap, io["mem"][c.s].rearrange("(k p) m -> p k m", p=128), writes=[l.MT])
            rms_rstd([l.MT.ap[:, k, :] for k in range(KC)], [l.MT] * KC, KC, NMEM, l.RB, l.SQ, float(D))
            for k in range(KC):
                g = cpcol("mem_g", layer * 8 + k)
                fw.op(dve, lambda h: h.scalar_tensor_tensor(out=l.HM.ap[:, k, :], in0=l.MT.ap[:, k, :], scalar=g,
                                                            in1=l.RB.ap[:, 0:NMEM], op0=ALU.mult, op1=ALU.mult),
                      reads=[l.MT, l.RB, CP], writes=[l.HM])
        for cc in range(2):
            slot, v = yield ("x_w_kv", layer, 0, KC, ((cc * 512, 512),))
            for m in range(4):
                for c in cs:
                    l = c.l
                    (ps,) = fw.psum(1)
                    for k in range(KC):
                        fw.op(pe, lambda h: h.matmul(ps.ap[:, 0:NMEM], v[:, k, m * 128:(m + 1) * 128], l.HM.ap[:, k, :],
                                                     start=(k == 0), stop=(k == KC - 1)), reads=[slot, l.HM], writes=[ps])
                    fw.op(act, lambda h: h.activation(out=l.KTb.ap[:, cc * 4 + m, :], in_=ps.ap[:, 0:NMEM], func=AF.Copy),
                          reads=[ps], writes=[l.KTb])
        for cc in range(2):
            slot, v = yield ("x_w_kv", layer, 0, KC, ((D + cc * 512, 512),))
            for tc in range(2):
                for c in cs:
                    l = c.l
                    (ps,) = fw.psum(1)
                    for k in range(KC):
                        fw.op(pe, lambda h: h.matmul(ps.ap, l.HM.ap[:, k, tc * 128:(tc + 1) * 128], v[:, k, :],
                                                     start=(k == 0), stop=(k == KC - 1)), reads=[slot, l.HM], writes=[ps])
                    fw.op(act, lambda h: h.activation(out=l.Vb.ap[:, tc, cc * 512:(cc + 1) * 512], in_=ps.ap, func=AF.Copy),
                          reads=[ps], writes=[l.Vb])
        for c in cs:
            l = c.l
            fw.dma(sp, kvd[c.s, layer, :, 0:2048], l.KTb.ap.rearrange("p a b -> p (a b)"), reads=[l.KTb], writes=[c.KVD[layer]])
            fw.dma(sp, kvd[c.s, layer, :, 2048:4096], l.Vb.ap.rearrange("p a b -> p (a b)"), reads=[l.Vb], writes=[c.KVD[layer]])

    def conv_sublayer(cs, layer):
        e = layer // 2
        for c in cs:
            l = common_alloc(c)
            ar = c.ar
            l.GB = ar.alloc("GB", [4, T], BF16)
            l.GC = ar.alloc("GC", [4, T], F32)
            l.UA = ar.alloc("UA", [4, T], F32)
            l.Z = [ar.alloc("Z%d" % j, [T], F32) for j in range(4)]
            l.SG = [ar.alloc("SG%d" % i, [T], F32) for i in range(2)]
            l.DG = [ar.alloc("DG%d" % i, [34, 128], BF16) for i in range(2)]
            l.MU = ar.alloc("MU", [T], F32)
            l.VR = ar.alloc("VR", [T], F32)
            l.cnt = 0
            yield from prenorm(c, layer, 0)

        def ev(c, j, ps):
            l = c.l
            grp, jj = j // 4, j % 4
            pv = ps.ap[:, 0:T]
            if grp == 0:
                fw.op(act, lambda h: h.activation(out=l.GB.ap[:, jj, :], in_=pv, func=AF.Copy), reads=[ps], writes=[l.GB])
            elif grp == 1:
                fw.op(act, lambda h: h.activation(out=l.GC.ap[:, jj, :], in_=pv, func=AF.Copy), reads=[ps], writes=[l.GC])
            elif grp == 2:
                fw.op(dve, lambda h: h.tensor_tensor(out=c.PBt[:, e, jj, 2:2 + T], in0=pv, in1=l.GC.ap[:, jj, :], op=ALU.mult),
                      reads=[ps, l.GC], writes=[c.PB[e]])
            elif grp == 3:
                fw.op(act, lambda h: h.activation(out=l.UA.ap[:, jj, :], in_=pv, func=AF.Copy), reads=[ps], writes=[l.UA])
            else:
                sg = l.SG[l.cnt % 2]
                l.cnt += 1
                fw.op(act, lambda h: h.activation(out=sg.ap, in_=pv, func=AF.Sigmoid), reads=[ps], writes=[sg])
                fw.op(dve, lambda h: h.tensor_tensor(out=c.GLUt[:, e, jj, 30:30 + T], in0=sg.ap, in1=l.UA.ap[:, jj, :], op=ALU.mult),
                      reads=[sg, l.UA], writes=[c.GLU[e]])
        yield from linear_fm(cs, lambda c: c.l.H, KC, "a_w_in", e, 0, 2560, ev)
        for c in cs:
            l = c.l
            MIX = l.MIX
            Z = l.Z
            def build_dg(jj):
                dg = l.DG[jj % 2]
                wa = CPt[:, CPO["conv_a"] + (e * 4 + jj) * 3: CPO["conv_a"] + (e * 4 + jj) * 3 + 3]
                wb = CPt[:, CPO["conv_b"] + (e * 4 + jj) * 31: CPO["conv_b"] + (e * 4 + jj) * 31 + 31]
                fw.op(dve, lambda h: h.tensor_tensor(out=dg.ap[:, 0:3, :], in0=CSTt[:, 0:1, :].broadcast_to([128, 3, 128]),
                                                     in1=wa.unsqueeze(2).broadcast_to([128, 3, 128]), op=ALU.mult),
                      reads=[CST, CP], writes=[dg])
                fw.op(dve, lambda h: h.tensor_tensor(out=dg.ap[:, 3:34, :], in0=CSTt[:, 0:1, :].broadcast_to([128, 31, 128]),
                                                     in1=wb.unsqueeze(2).broadcast_to([128, 31, 128]), op=ALU.mult),
                      reads=[CST, CP], writes=[dg])
            build_dg(0)
            for jj in range(4):
                dg = l.DG[jj % 2]
                (ps,) = fw.psum(1)
                for k in range(3):
                    fw.op(pe, lambda h: h.matmul(ps.ap[:, 0:T], dg.ap[:, k, :], c.PBt[:, e, jj, k:k + T], start=(k == 0), stop=(k == 2)),
                          reads=[dg, c.PB[e]], writes=[ps])
                (ps2,) = fw.psum(1)
                for k in range(31):
                    fw.op(pe, lambda h: h.matmul(ps2.ap[:, 0:T], dg.ap[:, 3 + k, :], c.GLUt[:, e, jj, k:k + T], start=(k == 0), stop=(k == 30)),
                          reads=[dg, c.GLU[e]], writes=[ps2])
                if jj < 3:
                    build_dg(jj + 1)
                fw.op(dve, lambda h: h.tensor_tensor(out=MIX.ap[:, jj, :], in0=ps.ap[:, 0:T], in1=l.GB.ap[:, jj, :], op=ALU.mult),
                      reads=[ps, l.GB], writes=[MIX])
                bcol = cpcol("cb_bias", e * 4 + jj)
                fw.op(dve, lambda h: h.tensor_scalar(out=Z[jj].ap, in0=ps2.ap[:, 0:T], scalar1=bcol, scalar2=None, op0=ALU.add),
                      reads=[ps2, CP], writes=[Z[jj]])
            fw.op(dve, lambda h: h.tensor_copy(c.PBt[:, e, :, 0:2], c.PBt[:, e, :, T:T + 2]), reads=[c.PB[e]], writes=[c.PB[e]])
            fw.op(dve, lambda h: h.tensor_copy(c.GLUt[:, e, :, 0:30], c.GLUt[:, e, :, T:T + 30]), reads=[c.GLU[e]], writes=[c.GLU[e]])
            (pm,) = fw.psum(1)
            (pq,) = fw.psum(1)
            MU, VR = l.MU, l.VR
            for jj in range(4):
                zb = l.SQ[0]
                fw.op(act, lambda h: h.activation(out=zb.ap[:, 0:T], in_=Z[jj].ap, func=AF.Copy), reads=[Z[jj]], writes=[zb])
                fw.op(pe, lambda h: h.matmul(pm.ap[:, 0:T], ONESB, zb.ap[:, 0:T], start=(jj == 0), stop=(jj == 3)), reads=[zb, CSB], writes=[pm])
                zq = l.SQ[1]
                fw.op(act, lambda h: h.activation(out=zq.ap[:, 0:T], in_=Z[jj].ap, func=AF.Square), reads=[Z[jj]], writes=[zq])
                fw.op(pe, lambda h: h.matmul(pq.ap[:, 0:T], ONESB, zq.ap[:, 0:T], start=(jj == 0), stop=(jj == 3)), reads=[zq, CSB], writes=[pq])
            fw.op(act, lambda h: h.activation(out=MU.ap, in_=pm.ap[:, 0:T], func=AF.Copy, scale=1.0 / 512), reads=[pm], writes=[MU])
            fw.op(dve, lambda h: h.tensor_tensor(out=VR.ap, in0=MU.ap, in1=MU.ap, op=ALU.mult), reads=[MU], writes=[VR])
            fw.op(dve, lambda h: h.scalar_tensor_tensor(out=VR.ap, in0=pq.ap[:, 0:T], scalar=1.0 / 512, in1=VR.ap, op0=ALU.mult,
                                                        op1=ALU.subtract), reads=[pq, VR], writes=[VR])
            fw.op(dve, lambda h: h.tensor_scalar(out=VR.ap, in0=VR.ap, scalar1=EPS, scalar2=None, op0=ALU.add), reads=[VR], writes=[VR])
            fw.op(act, lambda h: h.activation(out=VR.ap, in_=VR.ap, func=AF.Sqrt), reads=[VR], writes=[VR])
            fw.op(dve, lambda h: h.reciprocal(out=VR.ap, in_=VR.ap), reads=[VR], writes=[VR])
            for jj in range(4):
                fw.op(dve, lambda h: h.tensor_tensor(out=Z[jj].ap, in0=Z[jj].ap, in1=MU.ap, op=ALU.subtract), reads=[Z[jj], MU], writes=[Z[jj]])
                fw.op(dve, lambda h: h.tensor_tensor(out=Z[jj].ap, in0=Z[jj].ap, in1=VR.ap, op=ALU.mult), reads=[Z[jj], VR], writes=[Z[jj]])
                gcol = cpcol("ln_g", e * 4 + jj)
                bcol = cpcol("ln_b", e * 4 + jj)
                fw.op(act, lambda h: h.activation(out=MIX.ap[:, 4 + jj, :], in_=Z[jj].ap, func=AF.Silu, bias=bcol, scale=gcol),
                      reads=[Z[jj], CP], writes=[MIX])
        yield from outproj_postnorm(cs, layer, 1, "a_w_out", e)

    def attn_sublayer(cs, layer):
        for c in cs:
            l = common_alloc(c)
            ar = c.ar
            l.Q = ar.alloc("Q", [KC, T], BF16)
            l.PT = [ar.alloc("PT%d" % i, [2, T], BF16) for i in range(2)]
            l.RC = [ar.alloc("RC%d" % i, [T], F32) for i in range(2)]
            l.KTb = ar.alloc("KTb%d" % c.s, [KC, NMEM], BF16)
            l.Vb = ar.alloc("Vb%d" % c.s, [2, D], BF16)
            fw.dma(sp, l.KTb.ap.rearrange("p a b -> p (a b)"), kvd[c.s, layer, :, 0:2048], reads=[c.KVD[layer]], writes=[l.KTb])
            fw.dma(sp, l.Vb.ap.rearrange("p a b -> p (a b)"), kvd[c.s, layer, :, 2048:4096], reads=[c.KVD[layer]], writes=[l.Vb])
            yield from prenorm(c, layer, 2)

        def evq(c, j, ps):
            fw.op(act, lambda h: h.activation(out=c.l.Q.ap[:, j, :], in_=ps.ap[:, 0:T], func=AF.Copy), reads=[ps], writes=[c.l.Q])
        yield from linear_fm(cs, lambda c: c.l.H, KC, "x_w_q", layer, 0, D, evq)
        for hd in range(4):
            for c in cs:
                l = c.l
                Q, MIX = l.Q, l.MIX
                pt = l.PT[hd % 2]
                rc = l.RC[hd % 2]
                for mc in range(2):
                    (ps,) = fw.psum(1)
                    for dj in range(2):
                        fw.op(pe, lambda h: h.matmul(ps.ap[:, 0:T], l.KTb.ap[:, 2 * hd + dj, mc * 128:(mc + 1) * 128], Q.ap[:, 2 * hd + dj, :],
                                                     start=(dj == 0), stop=(dj == 1)), reads=[l.KTb, Q], writes=[ps])
                    fw.op(act, lambda h: h.activation(out=pt.ap[:, mc, :], in_=ps.ap[:, 0:T], func=AF.Exp, scale=1.0 / 16.0),
                          reads=[ps], writes=[pt])
                yield None
                (pd,) = fw.psum(1)
                for mc in range(2):
                    fw.op(pe, lambda h: h.matmul(pd.ap[:, 0:T], ONESB, pt.ap[:, mc, :], start=(mc == 0), stop=(mc == 1)),
                          reads=[pt, CSB], writes=[pd])
                fw.op(dve, lambda h: h.reciprocal(out=rc.ap, in_=pd.ap[:, 0:T]), reads=[pd], writes=[rc])
                for dj in range(2):
                    (po,) = fw.psum(1)
                    for mc in range(2):
                        fw.op(pe, lambda h: h.matmul(po.ap[:, 0:T], l.Vb.ap[:, mc, (2 * hd + dj) * 128:(2 * hd + dj + 1) * 128], pt.ap[:, mc, :],
                                                     start=(mc == 0), stop=(mc == 1)), reads=[l.Vb, pt], writes=[po])
                    fw.op(dve, lambda h: h.tensor_tensor(out=MIX.ap[:, 2 * hd + dj, :], in0=po.ap[:, 0:T], in1=rc.ap, op=ALU.mult),
                          reads=[po, rc], writes=[MIX])
        yield from outproj_postnorm(cs, layer, 3, "x_w_o", layer)

    def ffn_sublayer(cs, layer):
        for c in cs:
            l = common_alloc(c)
            l.A = c.ar.alloc("A", [22, T], BF16)
            l.SG = [c.ar.alloc("SG%d" % i, [T], F32) for i in range(2)]
            yield from prenorm(c, layer, 4)
        for cc in range(11):
            slot, v = yield ("f_w_gu", layer, 0, KC, ((cc * 256, 256), (DFF + cc * 256, 256)))
            for jj in range(2):
                for c in cs:
                    l = c.l
                    H = l.H
                    (pg,) = fw.psum(1)
                    (pu,) = fw.psum(1)
                    for k in range(KC):
                        fw.op(pe, lambda h: h.matmul(pg.ap[:, 0:T], v[:, k, jj * 128:(jj + 1) * 128], H.ap[:, k, :],
                                                     start=(k == 0), stop=(k == KC - 1)), reads=[slot, H], writes=[pg])
                    for k in range(KC):
                        fw.op(pe, lambda h: h.matmul(pu.ap[:, 0:T], v[:, k, 256 + jj * 128:256 + (jj + 1) * 128], H.ap[:, k, :],
                                                     start=(k == 0), stop=(k == KC - 1)), reads=[slot, H], writes=[pu])
                    sg = l.SG[jj]
                    fw.op(act, lambda h: h.activation(out=sg.ap, in_=pg.ap[:, 0:T], func=AF.Silu), reads=[pg], writes=[sg])
                    fw.op(dve, lambda h: h.tensor_tensor(out=l.A.ap[:, 2 * cc + jj, :], in0=pu.ap[:, 0:T], in1=sg.ap, op=ALU.mult),
                          reads=[pu, sg], writes=[l.A])
        for m in range(8):
            slot, v = yield ("f_w_down", layer, 0, 22, ((m * 128, 128),))
            for c in cs:
                A = c.l.A
                (pb,) = fw.psum(1)
                for kc in range(22):
                    fw.op(pe, lambda h: h.matmul(pb.ap[:, 0:T], v[:, kc, :], A.ap[:, kc, :], start=(kc == 0), stop=(kc == 21)),
                          reads=[slot, A], writes=[pb])
                evac_y(c, m, pb, layer, 5)
        yield None
        for c in cs:
            postnorm(c, layer, 5)
        yield None

    def mlstm_sublayer(cs, layer):
        o = layer // 2
        W = "m_w_in"
        for c in cs:
            l = common_alloc(c)
            ar = c.ar
            l.Q = ar.alloc("Q", [KC, T], BF16)
            l.KT = ar.alloc("KT", [TC, D], BF16)
            l.KTT = ar.alloc("KTT", [KC, T], BF16)
            l.VA = ar.alloc("VA", [TC, 8, 129], BF16)
            l.SO = ar.alloc("SO", [TC, D], BF16)
            l.GT = ar.alloc("GT", [TC, 16], F32)
            l.LL = ar.alloc("LL", [TC, 8], F32)
            l.EQ = ar.alloc("EQ", [TC, 8], F32)
            l.EK = ar.alloc("EK", [TC, 8], F32)
            l.GG = ar.alloc("GG", [TC, 8], F32)
            l.WT = [ar.alloc("WT%d" % i, [8, 128], BF16) for i in range(2)]
            l.HH = ar.alloc("HH", [8, 128], F32)
            l.H2 = ar.alloc("H2", [8, 128], F32)
            l.YT = ar.alloc("YT", [8, 128], BF16)
            l.CB = ar.alloc("CB", [8, 129], BF16)
            l.ST = [ar.alloc("ST%d" % i, [8], F32) for i in range(6)]
            yield from prenorm(c, layer, 0)

        def tm_linear(col0, ncols, evac):
            slot, v = yield (W, o, 0, KC, ((col0, ncols),))
            for c in cs:
                H = c.l.H
                for tc in range(TC):
                    (ps,) = fw.psum(1)
                    for k in range(KC):
                        fw.op(pe, lambda h: h.matmul(ps.ap[:, 0:ncols], H.ap[:, k, tc * 128:(tc + 1) * 128], v[:, k, :],
                                                     start=(k == 0), stop=(k == KC - 1)), reads=[slot, H], writes=[ps])
                    evac(c, tc, ps)

        def ev_g(c, tc, ps):
            fw.op(dve, lambda h: h.tensor_tensor(out=c.l.GT.ap[:, tc, :], in0=ps.ap[:, 0:16], in1=RPt[:, o, :], op=ALU.add),
                  reads=[ps, RP], writes=[c.l.GT])
        yield from tm_linear(4 * D, 16, ev_g)
        for c in cs:
            l = c.l
            GT, LL, EQ, EK, GG = l.GT, l.LL, l.EQ, l.EK, l.GG
            fw.op(act, lambda h: h.activation(out=LL.ap, in_=GT.ap[:, :, 8:16], func=AF.Exp, scale=-1.0), reads=[GT], writes=[LL])
            fw.op(act, lambda h: h.activation(out=LL.ap, in_=LL.ap, func=AF.Ln, bias=1.0), reads=[LL], writes=[LL])
            (pc,) = fw.psum(1)
            (pg,) = fw.psum(1)
            for tc in range(TC):
                fw.op(pe, lambda h: h.matmul(pc.ap[:, tc * 8:(tc + 1) * 8], TRIF, LL.ap[:, tc, :], start=True, stop=True),
                      reads=[CST, LL], writes=[pc])
                fw.op(pe, lambda h: h.matmul(pg.ap[:, tc * 8:(tc + 1) * 8], ONESF, LL.ap[:, tc, :], start=True, stop=True),
                      reads=[CST, LL], writes=[pg])
            pcv = pc.ap[:, 0:TC * 8].rearrange("p (a b) -> p a b", b=8)
            pgv = pg.ap[:, 0:TC * 8].rearrange("p (a b) -> p a b", b=8)
            fw.op(act, lambda h: h.activation(out=EQ.ap, in_=pcv, func=AF.Exp, scale=-1.0), reads=[pc], writes=[EQ])
            fw.op(act, lambda h: h.activation(out=GG.ap, in_=pgv, func=AF.Exp, scale=-1.0), reads=[pg], writes=[GG])
            fw.op(dve, lambda h: h.tensor_tensor(out=EK.ap, in0=pcv, in1=GT.ap[:, :, 0:8], op=ALU.add), reads=[pc, GT], writes=[EK])
            fw.op(act, lambda h: h.activation(out=EK.ap, in_=EK.ap, func=AF.Exp), reads=[EK], writes=[EK])

        def evq(c, j, ps):
            fw.op(act, lambda h: h.activation(out=c.l.Q.ap[:, j, :], in_=ps.ap[:, 0:T], func=AF.Copy), reads=[ps], writes=[c.l.Q])
        yield from linear_fm(cs, lambda c: c.l.H, KC, W, o, 0, D, evq)
        for cc in range(2):
            def ev_k(c, tc, ps):
                l = c.l
                fw.op(dve, lambda h: h.scalar_tensor_tensor(
                    out=l.KT.ap[:, tc, cc * 512:(cc + 1) * 512].rearrange("p (a b) -> p a b", b=128),
                    in0=ps.ap.rearrange("p (a b) -> p a b", b=128), scalar=KSCALE,
                    in1=l.EK.ap[:, tc, cc * 4:(cc + 1) * 4].unsqueeze(2).broadcast_to([128, 4, 128]),
                    op0=ALU.mult, op1=ALU.mult), reads=[ps, l.EK], writes=[l.KT])
            yield from tm_linear(D + cc * 512, 512, ev_k)
        for c in cs:
            fw.op(dve, lambda h: h.memset(c.l.VA.ap[:, :, :, 128:129], 1.0), writes=[c.l.VA])
        for cc in range(2):
            def ev_v(c, tc, ps):
                fw.op(act, lambda h: h.activation(out=c.l.VA.ap[:, tc, cc * 4:(cc + 1) * 4, 0:128],
                                                  in_=ps.ap.rearrange("p (a b) -> p a b", b=128), func=AF.Copy),
                      reads=[ps], writes=[c.l.VA])
            yield from tm_linear(2 * D + cc * 512, 512, ev_v)
        for cc in range(2):
            def ev_o(c, tc, ps):
                fw.op(act, lambda h: h.activation(out=c.l.SO.ap[:, tc, cc * 512:(cc + 1) * 512], in_=ps.ap, func=AF.Sigmoid),
                      reads=[ps], writes=[c.l.SO])
            yield from tm_linear(3 * D + cc * 512, 512, ev_o)
        for c in cs:
            l = c.l
            for tc in range(TC):
                (pt,) = fw.psum(1)
                ptb = pt.ap.bitcast(BF16)
                for hd in range(8):
                    fw.op(pe, lambda h: h.transpose(ptb[:, hd * 128:(hd + 1) * 128], l.KT.ap[:, tc, hd * 128:(hd + 1) * 128], IDENTB),
                          reads=[l.KT, CSB], writes=[pt])
                fw.op(act, lambda h: h.activation(out=l.KTT.ap[:, :, tc * 128:(tc + 1) * 128],
                                                  in_=ptb.rearrange("p (a b) -> p a b", b=128), func=AF.Copy),
                      reads=[pt], writes=[l.KTT])
            fw.op(act, lambda h: h.activation(out=l.CB.ap, in_=c.C32t[:, o], func=AF.Copy), reads=[c.C32[o]], writes=[l.CB])
        for tc in range(TC):
            for c in cs:
                yield from chunk(c, o, tc)
        yield from outproj_postnorm(cs, layer, 1, "m_w_out", o)

    def chunk(c, o, tc):
        l = c.l
        Q, KT, KTT, VA, SO, EQ, GG, HH, H2, YT, CB, MIX = l.Q, l.KT, l.KTT, l.VA, l.SO, l.EQ, l.GG, l.HH, l.H2, l.YT, l.CB, l.MIX
        C32t, C32b = c.C32t, c.C32[o]
        sl = slice(tc * 128, (tc + 1) * 128)
        wt = l.WT[tc % 2]
        pss = fw.psum(2)
        for hd in range(8):
            b = pss[hd // 4]
            fw.op(pe, lambda h: h.matmul(b.ap[:, (hd % 4) * 128:(hd % 4 + 1) * 128], KTT.ap[:, hd, sl], Q.ap[:, hd, sl],
                                         start=True, stop=True), reads=[KTT, Q], writes=[b])
        for g2 in range(2):
            fw.op(dve, lambda h: h.tensor_tensor(out=wt.ap[:, g2 * 4:(g2 + 1) * 4, :],
                                                 in0=pss[g2].ap.rearrange("p (a b) -> p a b", b=128),
                                                 in1=CSTt[:, 1:2, :].broadcast_to([128, 4, 128]), op=ALU.mult),
                  reads=[pss[g2], CST], writes=[wt])
        psp = fw.psum(3)
        for hd in range(8):
            bp = psp[hd // 3]
            cs_ = slice((hd % 3) * 129, (hd % 3 + 1) * 129)
            fw.op(pe, lambda h: h.matmul(bp.ap[:, cs_], KT.ap[:, tc, hd * 128:(hd + 1) * 128], VA.ap[:, tc, hd, :],
                                         start=True, stop=True), reads=[KT, VA], writes=[bp])
        for g3 in range(3):
            nh = 3 if g3 < 2 else 2
            hs = slice(g3 * 3, g3 * 3 + nh)
            fw.op(dve, lambda h: h.tensor_tensor(out=C32t[:, o, hs, :], in0=psp[g3].ap[:, 0:nh * 129].rearrange("p (a b) -> p a b", b=129),
                                                 in1=C32t[:, o, hs, :], op=ALU.add), reads=[psp[g3], C32b], writes=[C32b])
        yield None
        psn = fw.psum(3)
        for hd in range(8):
            bn = psn[hd // 3]
            cs_ = slice((hd % 3) * 129, (hd % 3 + 1) * 129)
            fw.op(pe, lambda h: h.matmul(bn.ap[:, cs_], Q.ap[:, hd, sl], CB.ap[:, hd, :], start=True, stop=False),
                  reads=[Q, CB], writes=[bn])
            fw.op(pe, lambda h: h.matmul(bn.ap[:, cs_], wt.ap[:, hd, :], VA.ap[:, tc, hd, :], start=False, stop=True),
                  reads=[wt, VA], writes=[bn])
        fw.op(dve, lambda h: h.tensor_tensor(out=C32t[:, o], in0=C32t[:, o],
                                             in1=GG.ap[:, tc, :].unsqueeze(2).broadcast_to([128, 8, 129]), op=ALU.mult),
              reads=[C32b, GG], writes=[C32b])
        fw.op(act, lambda h: h.activation(out=CB.ap, in_=C32t[:, o], func=AF.Copy), reads=[C32b], writes=[CB])
        DN, RR, S1, S2, MUh, RS = l.ST
        for g3 in range(3):
            nh = 3 if g3 < 2 else 2
            hs = slice(g3 * 3, g3 * 3 + nh)
            v3 = psn[g3].ap[:, 0:nh * 129].rearrange("p (a b) -> p a b", b=129)
            fw.op(act, lambda h: h.activation(out=DN.ap[:, hs].unsqueeze(2), in_=v3[:, :, 128:129], func=AF.Abs),
                  reads=[psn[g3]], writes=[DN])
        fw.op(dve, lambda h: h.tensor_tensor(out=DN.ap, in0=DN.ap, in1=EQ.ap[:, tc, :], op=ALU.mult), reads=[DN, EQ], writes=[DN])
        fw.op(dve, lambda h: h.tensor_scalar(out=DN.ap, in0=DN.ap, scalar1=1.0, scalar2=None, op0=ALU.max), reads=[DN], writes=[DN])
        fw.op(dve, lambda h: h.reciprocal(out=DN.ap, in_=DN.ap), reads=[DN], writes=[DN])
        fw.op(dve, lambda h: h.tensor_tensor(out=RR.ap, in0=EQ.ap[:, tc, :], in1=DN.ap, op=ALU.mult), reads=[DN, EQ], writes=[RR])
        for g3 in range(3):
            nh = 3 if g3 < 2 else 2
            hs = slice(g3 * 3, g3 * 3 + nh)
            v3 = psn[g3].ap[:, 0:nh * 129].rearrange("p (a b) -> p a b", b=129)
            fw.op(dve, lambda h: h.tensor_tensor(out=HH.ap[:, hs, :], in0=v3[:, :, 0:128],
                                                 in1=RR.ap[:, hs].unsqueeze(2).broadcast_to([128, nh, 128]), op=ALU.mult),
                  reads=[psn[g3], RR], writes=[HH])
        fw.op(dve, lambda h: h.tensor_reduce(out=S1.ap, in_=HH.ap, axis=AX.X, op=ALU.add), reads=[HH], writes=[S1])
        fw.op(act, lambda h: h.activation(out=H2.ap, in_=HH.ap, func=AF.Square), reads=[HH], writes=[H2])
        fw.op(dve, lambda h: h.tensor_reduce(out=S2.ap, in_=H2.ap, axis=AX.X, op=ALU.add), reads=[H2], writes=[S2])
        fw.op(dve, lambda h: h.tensor_scalar(out=MUh.ap, in0=S1.ap, scalar1=1.0 / 128, scalar2=None, op0=ALU.mult), reads=[S1], writes=[MUh])
        fw.op(dve, lambda h: h.tensor_tensor(out=S1.ap, in0=MUh.ap, in1=MUh.ap, op=ALU.mult), reads=[MUh], writes=[S1])
        fw.op(dve, lambda h: h.scalar_tensor_tensor(out=RS.ap, in0=S2.ap, scalar=1.0 / 128, in1=S1.ap, op0=ALU.mult, op1=ALU.subtract),
              reads=[S2, S1], writes=[RS])
        fw.op(dve, lambda h: h.tensor_scalar(out=RS.ap, in0=RS.ap, scalar1=EPS, scalar2=None, op0=ALU.add), reads=[RS], writes=[RS])
        fw.op(act, lambda h: h.activation(out=RS.ap, in_=RS.ap, func=AF.Sqrt), reads=[RS], writes=[RS])
        fw.op(dve, lambda h: h.reciprocal(out=RS.ap, in_=RS.ap), reads=[RS], writes=[RS])
        fw.op(dve, lambda h: h.tensor_tensor(out=HH.ap, in0=HH.ap, in1=MUh.ap.unsqueeze(2).broadcast_to([128, 8, 128]), op=ALU.subtract),
              reads=[HH, MUh], writes=[HH])
        fw.op(dve, lambda h: h.tensor_tensor(out=HH.ap, in0=HH.ap, in1=RS.ap.unsqueeze(2).broadcast_to([128, 8, 128]), op=ALU.mult),
              reads=[HH, RS], writes=[HH])
        fw.op(dve, lambda h: h.tensor_tensor(out=YT.ap, in0=HH.ap, in1=SO.ap[:, tc, :].rearrange("p (a b) -> p a b", b=128), op=ALU.mult),
              reads=[HH, SO], writes=[YT])
        yield None
        (py,) = fw.psum(1)
        pyb = py.ap.bitcast(BF16)
        for hd in range(8):
            fw.op(pe, lambda h: h.transpose(pyb[:, hd * 128:(hd + 1) * 128], YT.ap[:, hd, :], IDENTB), reads=[YT, CSB], writes=[py])
        mg = CPt[:, CPO["m_g"] + o * 8: CPO["m_g"] + o * 8 + 8]
        fw.op(dve, lambda h: h.tensor_tensor(out=MIX.ap[:, :, sl], in0=pyb.rearrange("p (a b) -> p a b", b=128),
                                             in1=mg.unsqueeze(2).broadcast_to([128, 8, 128]), op=ALU.mult),
              reads=[py, CP], writes=[MIX])

    def ctx_program(c):
        cs = [c]
        for o in range(2):
            fw.op(dve, lambda h: h.memset(c.C32t[:, o], 0.0), writes=[c.C32[o]])
        for e in range(2):
            fw.op(dve, lambda h: h.memset(c.GLUt[:, e], 0.0), writes=[c.GLU[e]])
            fw.op(dve, lambda h: h.memset(c.PBt[:, e], 0.0), writes=[c.PB[e]])
        for layer in range(DEPTH):
            yield from kv_prep(cs, layer)
        for t in range(NT):
            fw.dma(sp, c.Xt[:], io["x"][c.s][:, t * T:(t + 1) * T].rearrange("(k p) t -> p k t", p=128), writes=[c.X])
            for layer in range(DEPTH):
                if layer % 2 == 0:
                    yield from conv_sublayer(cs, layer)
                else:
                    yield from mlstm_sublayer(cs, layer)
                yield from attn_sublayer(cs, layer)
                yield from ffn_sublayer(cs, layer)
            fw.dma(sp, io["y"][c.s][:, t * T:(t + 1) * T].rearrange("(k p) t -> p k t", p=128), c.Xt[:], reads=[c.X])

    run = ctxs[:1] if dry else ctxs
    gens = [ctx_program(c) for c in run]
    n = len(gens)
    nxt = [None] * n
    blk = [0] * n
    prog = [0] * n
    done = [False] * n
    for i, g in enumerate(gens):
        try:
            nxt[i] = next(g)
        except StopIteration:
            done[i] = True
    while not all(done):
        if n == 1 or done[1]:
            i = 0
        elif done[0]:
            i = 1
        else:
            i = 0 if (prog[0] - prog[1]) < SKEW else 1
        if nxt[i] is None:
            val = None
        else:
            bmin = min(blk[j] for j in range(n) if not done[j])
            val = wq.get(blk[i], nxt[i], bmin)
            blk[i] += 1
        prog[i] += 1
        try:
            nxt[i] = gens[i].send(val)
        except StopIteration:
            done[i] = True
    fw.wait_all(sp, [c.X for c in ctxs])
    return fw


def build_nc(NSEQ=2, NT=8, DEPTH=4):
    S = NT * T
    nc = bass.Bass("TRN2", target_bir_lowering=False)

    def din(name, shape):
        return nc.dram_tensor(name, shape, F32, kind="ExternalInput").ap()
    io = {}
    io["x"] = din("x", [NSEQ, D, S])
    io["mem"] = din("mem", [NSEQ, D, NMEM])
    io["consts"] = din("consts", [128, 3, 128])
    io["cp"] = din("cp", [128, NCP])
    io["rp"] = din("rp", [128, 2, 16])
    io["w"] = {
        "a_w_in": din("a_w_in", [2, D, 2560]), "a_w_out": din("a_w_out", [2, D, D]),
        "m_w_in": din("m_w_in", [2, D, 4112]), "m_w_out": din("m_w_out", [2, D, D]),
        "x_w_q": din("x_w_q", [4, D, D]), "x_w_kv": din("x_w_kv", [4, D, 2 * D]), "x_w_o": din("x_w_o", [4, D, D]),
        "f_w_gu": din("f_w_gu", [4, D, 2 * DFF]), "f_w_down": din("f_w_down", [4, DFF, D]),
    }
    io["y"] = nc.dram_tensor("y", [NSEQ, D, S], F32, kind="ExternalOutput").ap()
    io["kvd"] = nc.dram_tensor("kvd", [NSEQ, 4, 128, 4096], BF16, kind="Internal").ap()
    plan = []
    with contextlib.ExitStack() as es:
        _emit(nc, es, True, plan, NSEQ, NT, DEPTH, io)
    es2 = contextlib.ExitStack()
    with es2:
        fw = _emit(nc, es2, False, plan, NSEQ, NT, DEPTH, io)
    return nc, fw


def host_tables(inp):
    cp = np.zeros((128, NCP), np.float32)

    def put(name, idx, vec128):
        cp[:, CPO[name] + idx] = vec128
    ng = np.asarray(inp["norm_g"], np.float32)
    for l in range(4):
        for j in range(6):
            for k in range(8):
                put("norm_g", (l * 6 + j) * 8 + k, ng[l, j, k * 128:(k + 1) * 128])
    mg = np.asarray(inp["mem_norm_g"], np.float32)
    for l in range(4):
        for k in range(8):
            put("mem_g", l * 8 + k, mg[l, k * 128:(k + 1) * 128])
    ca = np.asarray(inp["a_conv_a"], np.float32)
    cb = np.asarray(inp["a_conv_b"], np.float32)
    cbb = np.asarray(inp["a_conv_b_bias"], np.float32)
    lg = np.asarray(inp["a_ln_g"], np.float32)
    lb = np.asarray(inp["a_ln_b"], np.float32)
    for e in range(2):
        for c in range(4):
            for k in range(3):
                put("conv_a", (e * 4 + c) * 3 + k, ca[e, k, c * 128:(c + 1) * 128])
            for k in range(31):
                put("conv_b", (e * 4 + c) * 31 + k, cb[e, k, c * 128:(c + 1) * 128])
            put("cb_bias", e * 4 + c, cbb[e, c * 128:(c + 1) * 128])
            put("ln_g", e * 4 + c, lg[e, c * 128:(c + 1) * 128])
            put("ln_b", e * 4 + c, lb[e, c * 128:(c + 1) * 128])
    mng = np.asarray(inp["m_norm_g"], np.float32)
    for o in range(2):
        for k in range(8):
            put("m_g", o * 8 + k, mng[o, k * 128:(k + 1) * 128])
    rp = np.zeros((128, 2, 16), np.float32)
    rp[:, :, 0:8] = np.asarray(inp["m_i_bias"], np.float32)[None]
    rp[:, :, 8:16] = np.asarray(inp["m_f_bias"], np.float32)[None]
    consts = np.zeros((128, 3, 128), np.float32)
    consts[:, 0, :] = np.eye(128, dtype=np.float32)
    consts[:, 1, :] = np.triu(np.ones((128, 128), np.float32))
    consts[:, 2, :] = 1.0
    return cp, rp, consts


WNAMES = ["a_w_in", "a_w_out", "m_w_in", "m_w_out", "x_w_q", "x_w_kv", "x_w_o", "f_w_gu", "f_w_down"]


def kernel(**inp):
    x = np.asarray(inp["x"], np.float32)
    mem = np.asarray(inp["mem"], np.float32)
    B = x.shape[0]
    nseq = B // NCORES
    cp, rp, consts = host_tables(inp)
    nc, _ = build_nc(NSEQ=nseq, NT=x.shape[1] // T, DEPTH=4)
    shared = {"consts": consts, "cp": cp, "rp": rp}
    for w in WNAMES:
        shared[w] = np.ascontiguousarray(np.asarray(inp[w], np.float32))
    in_maps = []
    for c in range(NCORES):
        m = dict(shared)
        m["x"] = np.ascontiguousarray(x[c * nseq:(c + 1) * nseq].transpose(0, 2, 1))
        m["mem"] = np.ascontiguousarray(mem[c * nseq:(c + 1) * nseq].transpose(0, 2, 1))
        in_maps.append(m)
    res = run_bass_kernel_spmd(nc, in_maps, core_ids=list(range(NCORES)))
    out = np.empty_like(x)
    for c in range(NCORES):
        y = res.results[c]["y"]
        out[c * nseq:(c + 1) * nseq] = y.transpose(0, 2, 1)
    return out
```

```python
import contextlib
import numpy as np
import concourse.bass as bass
import concourse.mybir as mybir
from concourse.bass_utils import run_bass_kernel_spmd

F32 = mybir.dt.float32
BF16 = mybir.dt.bfloat16
AF = mybir.ActivationFunctionType
ALU = mybir.AluOpType
AX = mybir.AxisListType

D = 1024
KC = 8
T = 256
NMEM = 256
DFF = 2816
SEQ = 4096
NCORES = 8
EPS = 1e-6
NRING = 5
SKEW = 2
SLOT = 4096
KSCALE = 128.0 ** -0.5

def _cp_layout():
    off = {}
    n = 0
    for name, cnt in (("norm_g", 4 * 6 * 8), ("mem_g", 4 * 8), ("conv_a", 2 * 4 * 3), ("conv_b", 2 * 4 * 31),
                      ("cb_bias", 2 * 4), ("ln_g", 2 * 4), ("ln_b", 2 * 4), ("m_g", 2 * 8)):
        off[name] = n
        n += cnt
    return off, n


CPO, NCP = _cp_layout()


class Ctr:
    LIMIT = 16000

    def __init__(self, fw, name, owner=None):
        self.fw, self.name, self.owner, self.k, self.val = fw, name, owner, 0, 0
        self.sem = fw.new_sem(name + "_0")

    def bump(self, inc):
        if self.val + inc > self.LIMIT:
            self.k += 1
            self.sem = self.fw.new_sem("%s_%d" % (self.name, self.k))
            self.val = 0
        self.val += inc
        return (self.sem, self.val, self.owner)


class Eng:
    def __init__(self, fw, name, h, selfsync=True):
        self.h, self.name, self.selfsync = h, name, selfsync
        self.ctr = Ctr(fw, name, self)
        self.waited = {}


class Buf:
    def __init__(self, name, ap=None):
        self.name, self.ap = name, ap
        self.w = None
        self.r = {}
        self.dctr = None

    def deps(self):
        d = list(self.r.values())
        if self.w is not None:
            d.append(self.w)
        return d


class FW:
    def __init__(self, nc, es, dry):
        self.nc, self.es, self.dry = nc, es, dry
        self.nsem = 0
        self.nops = 0
        self.dctrs = {}
        if not dry:
            self.pe = Eng(self, "pe", nc.tensor, selfsync=False)
            self.act = Eng(self, "act", nc.scalar)
            self.dve = Eng(self, "dve", nc.vector)
            self.pool = Eng(self, "pool", nc.gpsimd)
            self.sp = Eng(self, "sp", nc.sync)
        else:
            self.pe = self.act = self.dve = self.pool = self.sp = None
        self.psbanks = None
        self.psptr = 0

    def new_sem(self, name):
        if self.dry:
            return None
        self.nsem += 1
        return self.es.enter_context(self.nc.semaphore(name))

    def _wait(self, eng, deps):
        for (sem, val, owner) in deps:
            if owner is eng and not eng.selfsync:
                continue
            k = id(sem)
            if eng.waited.get(k, 0) < val:
                eng.h.wait_ge(sem, val)
                eng.waited[k] = val

    def _deps(self, reads, writes):
        deps = []
        for b in reads:
            if b.w is not None:
                deps.append(b.w)
        for b in writes:
            deps.extend(b.deps())
        return deps

    def _commit(self, d, reads, writes):
        k = id(d[0])
        for b in reads:
            old = b.r.get(k)
            if old is None or old[1] < d[1]:
                b.r[k] = d
        for b in writes:
            b.w = d
            b.r = {}

    def op(self, eng, fn, reads=(), writes=()):
        if self.dry:
            return
        self._wait(eng, self._deps(reads, writes))
        ins = fn(eng.h)
        d = eng.ctr.bump(1)
        ins.then_inc(d[0], 1)
        self._commit(d, reads, writes)
        self.nops += 1

    def dma(self, eng, out, in_, reads=(), writes=()):
        if self.dry:
            return
        self._wait(eng, self._deps(reads, writes))
        ins = eng.h.dma_start(out=out, in_=in_)
        b = writes[0] if writes else reads[0]
        if b.dctr is None:
            if b.name not in self.dctrs:
                self.dctrs[b.name] = Ctr(self, "d_" + b.name, None)
            b.dctr = self.dctrs[b.name]
        d = b.dctr.bump(16)
        ins.then_inc(d[0], 16)
        self._commit(d, reads, writes)
        self.nops += 1

    def wait_all(self, eng, bufs):
        if self.dry:
            return
        deps = []
        for b in bufs:
            deps.extend(b.deps())
        self._wait(eng, deps)

    def psum(self, n=1):
        if self.psptr + n > 8:
            self.psptr = 0
        bs = self.psbanks[self.psptr:self.psptr + n]
        self.psptr = (self.psptr + n) % 8
        return bs


class Arena:
    def __init__(self, fw, tensor, nelem):
        self.fw, self.t, self.n = fw, tensor, nelem
        self.hist = []
        self.cur = []
        self.off = 0

    def reset(self):
        newh = list(self.cur)
        for (a, b, buf) in self.hist:
            if not any(a < cb and ca < b for (ca, cb, _) in self.cur):
                newh.append((a, b, buf))
        self.hist = newh
        self.cur = []
        self.off = 0

    def alloc(self, name, shape, dtype):
        n = 1
        for s in shape:
            n *= s
        nb = n * (2 if dtype == F32 else 1)
        nb = (nb + 15) // 16 * 16
        a, b = self.off, self.off + nb
        assert b <= self.n, "arena overflow %s %d > %d" % (name, b, self.n)
        self.off = b
        ap = self.t[:, a:a + n * (2 if dtype == F32 else 1)]
        if dtype == F32:
            ap = ap.bitcast(F32)
        if len(shape) == 2:
            ap = ap.rearrange("p (a b) -> p a b", b=shape[1])
        elif len(shape) == 3:
            ap = ap.rearrange("p (a b c) -> p a b c", b=shape[1], c=shape[2])
        buf = Buf(name, ap)
        for (ha, hb, hbuf) in self.hist:
            if ha < b and a < hb:
                for d in hbuf.deps():
                    k = id(d[0])
                    old = buf.r.get(k)
                    if old is None or old[1] < d[1]:
                        buf.r[k] = d
        self.cur.append((a, b, buf))
        return buf


class WQ:
    def __init__(self, fw, slots, wdram, plan):
        self.fw, self.slots, self.wdram = fw, slots, wdram
        self.plan = plan
        self.i = 0
        self.issued = 0

    def _view(self, slot, nkc, ncols):
        return slot.ap[:, 0:nkc * ncols].rearrange("p (k m) -> p k m", m=ncols)

    def _issue(self, j):
        (wname, li, row0, nkc, segs) = self.plan[j]
        slot = self.slots[j % NRING]
        ncols = sum(n for (_, n) in segs)
        v = self._view(slot, nkc, ncols)
        W = self.wdram[wname][li]
        off = 0
        for (c0, n) in segs:
            src = W[row0:row0 + nkc * 128, c0:c0 + n].rearrange("(k p) m -> p k m", p=128)
            self.fw.dma(self.fw.pool, v[:, :, off:off + n], src, writes=[slot])
            off += n

    def get(self, k, spec, bmin):
        (wname, li, row0, nkc, segs) = spec
        ncols = sum(n for (_, n) in segs)
        assert nkc * ncols <= SLOT
        if self.fw.dry:
            assert k == len(self.plan)
            self.plan.append(spec)
            return self.slots[0], self._view(self.slots[0], nkc, ncols)
        assert self.plan[k] == spec, (k, self.plan[k], spec)
        while self.issued < min(len(self.plan), bmin + NRING):
            self._issue(self.issued)
            self.issued += 1
        assert k < self.issued
        slot = self.slots[k % NRING]
        return slot, self._view(slot, nkc, ncols)


class Ctx:
    pass


class L:
    pass


def _emit(nc, es, dry, plan, NSEQ, NT, DEPTH, io):
    fw = FW(nc, es, dry)
    E = es.enter_context
    sfx = "d" if dry else "r"
    TC = T // 128

    def sb(name, shape, dt):
        return E(nc.sbuf_tensor(name + sfx, shape, dt))

    ring = [Buf("ring%d" % i, sb("ring%d" % i, [128, SLOT], BF16)[:]) for i in range(NRING)]
    CSTt = sb("CST", [128, 3, 128], F32)
    CSBt = sb("CSB", [128, 3, 128], BF16)
    CST = Buf("CST", CSTt[:])
    CSB = Buf("CSB", CSBt[:])
    CPt = sb("CP", [128, NCP], F32)
    CP = Buf("CP", CPt[:])
    RPt = sb("RP", [128, 2, 16], F32)
    RP = Buf("RP", RPt[:])
    NA = 28544
    ctxs = []
    for s in range(NSEQ):
        c = Ctx()
        c.s = s
        c.Xt = sb("X%d" % s, [128, KC, T], F32)
        c.X = Buf("X%d" % s, c.Xt[:])
        c.C32t = sb("C32_%d" % s, [128, 2, 8, 129], F32)
        c.C32 = [Buf("C32_%d_%d" % (s, o), c.C32t[:, o]) for o in range(2)]
        c.GLUt = sb("GLU%d" % s, [128, 2, 4, 30 + T], BF16)
        c.PBt = sb("PB%d" % s, [128, 2, 4, 2 + T], BF16)
        c.GLU = [Buf("GLU%d_%d" % (s, e), c.GLUt[:, e]) for e in range(2)]
        c.PB = [Buf("PB%d_%d" % (s, e), c.PBt[:, e]) for e in range(2)]
        c.ARt = sb("AR%d" % s, [128, NA], BF16)
        c.ar = Arena(fw, c.ARt, NA)
        c.KVD = [Buf("kvd%d_%d" % (s, l)) for l in range(4)]
        ctxs.append(c)
    PSt = E(nc.psum_tensor("PS" + sfx, [128, 8, 512], F32))
    fw.psbanks = [Buf("ps%d" % i, PSt[:, i, :]) for i in range(8)]
    kvd = io["kvd"]

    wq = WQ(fw, ring, io["w"], plan)
    pe, act, dve, pool, sp = fw.pe, fw.act, fw.dve, fw.pool, fw.sp

    IDENTB = CSBt[:, 0, :]
    ONESB = CSBt[:, 2, :]
    TRIF = CSTt[:, 1, :]
    ONESF = CSTt[:, 2, :]

    def cpcol(name, idx):
        cc = CPO[name] + idx
        return CPt[:, cc:cc + 1]

    fw.dma(sp, CSTt[:], io["consts"], writes=[CST])
    fw.dma(sp, CPt[:], io["cp"], writes=[CP])
    fw.dma(sp, RPt[:], io["rp"], writes=[RP])
    fw.op(dve, lambda h: h.tensor_copy(CSBt[:], CSTt[:]), reads=[CST], writes=[CSB])

    def rms_rstd(srcs, srcbufs, nk, ncols, RB, SQ, dn):
        (ps,) = fw.psum(1)
        for k in range(nk):
            sq = SQ[k % 2]
            fw.op(act, lambda h: h.activation(out=sq.ap[:, 0:ncols], in_=srcs[k], func=AF.Square),
                  reads=[srcbufs[k]], writes=[sq])
            fw.op(pe, lambda h: h.matmul(ps.ap[:, 0:ncols], ONESB, sq.ap[:, 0:ncols], start=(k == 0), stop=(k == nk - 1)),
                  reads=[sq, CSB], writes=[ps])
        fw.op(dve, lambda h: h.tensor_scalar(out=RB.ap[:, 0:ncols], in0=ps.ap[:, 0:ncols], scalar1=1.0 / dn,
                                             scalar2=EPS, op0=ALU.mult, op1=ALU.add), reads=[ps], writes=[RB])
        fw.op(act, lambda h: h.activation(out=RB.ap[:, 0:ncols], in_=RB.ap[:, 0:ncols], func=AF.Sqrt),
              reads=[RB], writes=[RB])
        fw.op(dve, lambda h: h.reciprocal(out=RB.ap[:, 0:ncols], in_=RB.ap[:, 0:ncols]), reads=[RB], writes=[RB])

    def common_alloc(c):
        l = L()
        ar = c.ar
        ar.reset()
        l.H = ar.alloc("H", [KC, T], BF16)
        l.SQ = [ar.alloc("SQ%d" % i, [NMEM], BF16) for i in range(2)]
        l.RB = ar.alloc("RB", [NMEM], F32)
        l.RB2 = ar.alloc("RB2", [T], F32)
        l.YA = ar.alloc("YA", [KC, T], F32)
        l.SQA = ar.alloc("SQA", [KC, T], BF16)
        l.MIX = l.H
        c.l = l
        return l

    def rstd_from_ps(ps, RB):
        fw.op(act, lambda h: h.activation(out=RB.ap[:, 0:T], in_=ps.ap[:, 0:T], func=AF.Ln, scale=1.0 / D, bias=EPS),
              reads=[ps], writes=[RB])
        fw.op(act, lambda h: h.activation(out=RB.ap[:, 0:T], in_=RB.ap[:, 0:T], func=AF.Exp, scale=-0.5),
              reads=[RB], writes=[RB])

    def prenorm(c, layer, j):
        l = c.l
        fw.op(act, lambda h: h.activation(out=l.SQA.ap, in_=c.Xt[:], func=AF.Square), reads=[c.X], writes=[l.SQA])
        (ps,) = fw.psum(1)
        for k in range(KC):
            fw.op(pe, lambda h: h.matmul(ps.ap[:, 0:T], ONESB, l.SQA.ap[:, k, :], start=(k == 0), stop=(k == KC - 1)),
                  reads=[l.SQA, CSB], writes=[ps])
        rstd_from_ps(ps, l.RB)
        yield None
        for k in range(KC):
            g = cpcol("norm_g", (layer * 6 + j) * 8 + k)
            fw.op(dve, lambda h: h.scalar_tensor_tensor(out=l.H.ap[:, k, :], in0=c.Xt[:, k, :], scalar=g,
                                                        in1=l.RB.ap[:, 0:T], op0=ALU.mult, op1=ALU.mult),
                  reads=[c.X, l.RB, CP], writes=[l.H])

    def linear_fm(cs, getsrc, nk, wname, li, col0, ncols_total, evac, blk=512):
        j = 0
        for cc in range(0, ncols_total, blk):
            n = min(blk, ncols_total - cc)
            slot, v = yield (wname, li, 0, nk, ((col0 + cc, n),))
            for m in range(n // 128):
                for c in cs:
                    src = getsrc(c)
                    (ps,) = fw.psum(1)
                    for k in range(nk):
                        fw.op(pe, lambda h: h.matmul(ps.ap[:, 0:T], v[:, k, m * 128:(m + 1) * 128], src.ap[:, k, :],
                                                     start=(k == 0), stop=(k == nk - 1)),
                              reads=[slot, src], writes=[ps])
                    evac(c, j, ps)
                j += 1

    def evac_y(c, j, ps, layer, jg):
        l = c.l
        g = cpcol("norm_g", (layer * 6 + jg) * 8 + j)
        sq = l.SQ[j % 2]
        fw.op(act, lambda h: h.activation(out=l.YA.ap[:, j, :], in_=ps.ap[:, 0:T], func=AF.Copy, scale=g),
              reads=[ps, CP], writes=[l.YA])
        fw.op(act, lambda h: h.activation(out=sq.ap[:, 0:T], in_=ps.ap[:, 0:T], func=AF.Square), reads=[ps], writes=[sq])
        flush_stats(c)

        def deferred():
            (pst,) = fw.psum(1)
            fw.op(pe, lambda h: h.matmul(pst.ap[:, 0:T], ONESB, sq.ap[:, 0:T], start=True, stop=True), reads=[sq, CSB], writes=[pst])
            if j == 0:
                fw.op(dve, lambda h: h.tensor_copy(l.RB2.ap, pst.ap[:, 0:T]), reads=[pst], writes=[l.RB2])
            else:
                fw.op(dve, lambda h: h.tensor_tensor(out=l.RB2.ap, in0=pst.ap[:, 0:T], in1=l.RB2.ap, op=ALU.add),
                      reads=[pst, l.RB2], writes=[l.RB2])
        l.pending = deferred

    def flush_stats(c):
        f = getattr(c.l, "pending", None)
        if f is not None:
            c.l.pending = None
            f()

    def postnorm(c, layer, jg):
        l = c.l
        flush_stats(c)
        rstd_from_ps(l.RB2, l.RB2)
        fw.op(dve, lambda h: h.tensor_tensor(out=l.YA.ap, in0=l.YA.ap, in1=l.RB2.ap.unsqueeze(1).broadcast_to([128, KC, T]), op=ALU.mult),
              reads=[l.YA, l.RB2], writes=[l.YA])
        fw.op(dve, lambda h: h.tensor_tensor(out=c.Xt[:], in0=c.Xt[:], in1=l.YA.ap, op=ALU.add), reads=[c.X, l.YA], writes=[c.X])

    def outproj_postnorm(cs, layer, jg, wname, li):
        def ev(c, j, ps):
            evac_y(c, j, ps, layer, jg)
        yield from linear_fm(cs, lambda c: c.l.MIX, KC, wname, li, 0, D, ev)
        yield None
        for c in cs:
            postnorm(c, layer, jg)
        yield None

    def kv_prep(cs, layer):
        for c in cs:
            ar = c.ar
            ar.reset()
            l = L()
            c.l = l
            l.MT = ar.alloc("MT%d" % c.s, [KC, NMEM], F32)
            l.HM = ar.alloc("HM", [KC, NMEM], BF16)
            l.SQ = [ar.alloc("SQ%d" % i, [NMEM], BF16) for i in range(2)]
            l.RB = ar.alloc("RB", [NMEM], F32)
            l.KTb = ar.alloc("KTs%d" % c.s, [KC, NMEM], BF16)
            l.Vb = ar.alloc("Vs%d" % c.s, [2, D], BF16)
            fw.dma(sp, l.MT.ap, io["mem"][c.s].rearrange("(k p) m -> p k m", p=128), writes=[l.MT])
            rms_rstd([l.MT.ap[:, k, :] for k in range(KC)], [l.MT] * KC, KC, NMEM, l.RB, l.SQ, float(D))
            for k in range(KC):
                g = cpcol("mem_g", layer * 8 + k)
                fw.op(dve, lambda h: h.scalar_tensor_tensor(out=l.HM.ap[:, k, :], in0=l.MT.ap[:, k, :], scalar=g,
                                                            in1=l.RB.ap[:, 0:NMEM], op0=ALU.mult, op1=ALU.mult),
                      reads=[l.MT, l.RB, CP], writes=[l.HM])
        for cc in range(2):
            slot, v = yield ("x_w_kv", layer, 0, KC, ((cc * 512, 512),))
            for m in range(4):
                for c in cs:
                    l = c.l
                    (ps,) = fw.psum(1)
                    for k in range(KC):
                        fw.op(pe, lambda h: h.matmul(ps.ap[:, 0:NMEM], v[:, k, m * 128:(m + 1) * 128], l.HM.ap[:, k, :],
                                                     start=(k == 0), stop=(k == KC - 1)), reads=[slot, l.HM], writes=[ps])
                    fw.op(act, lambda h: h.activation(out=l.KTb.ap[:, cc * 4 + m, :], in_=ps.ap[:, 0:NMEM], func=AF.Copy),
                          reads=[ps], writes=[l.KTb])
        for cc in range(2):
            slot, v = yield ("x_w_kv", layer, 0, KC, ((D + cc * 512, 512),))
            for tc in range(2):
                for c in cs:
                    l = c.l
                    (ps,) = fw.psum(1)
                    for k in range(KC):
                        fw.op(pe, lambda h: h.matmul(ps.ap, l.HM.ap[:, k, tc * 128:(tc + 1) * 128], v[:, k, :],
                                                     start=(k == 0), stop=(k == KC - 1)), reads=[slot, l.HM], writes=[ps])
                    fw.op(act, lambda h: h.activation(out=l.Vb.ap[:, tc, cc * 512:(cc + 1) * 512], in_=ps.ap, func=AF.Copy),
                          reads=[ps], writes=[l.Vb])
        for c in cs:
            l = c.l
            fw.dma(sp, kvd[c.s, layer, :, 0:2048], l.KTb.ap.rearrange("p a b -> p (a b)"), reads=[l.KTb], writes=[c.KVD[layer]])
            fw.dma(sp, kvd[c.s, layer, :, 2048:4096], l.Vb.ap.rearrange("p a b -> p (a b)"), reads=[l.Vb], writes=[c.KVD[layer]])

    def conv_sublayer(cs, layer):
        e = layer // 2
        for c in cs:
            l = common_alloc(c)
            ar = c.ar
            l.GB = ar.alloc("GB", [4, T], BF16)
            l.GC = ar.alloc("GC", [4, T], F32)
            l.UA = ar.alloc("UA", [4, T], F32)
            l.Z = [ar.alloc("Z%d" % j, [T], F32) for j in range(4)]
            l.SG = [ar.alloc("SG%d" % i, [T], F32) for i in range(2)]
            l.DG = [ar.alloc("DG%d" % i, [34, 128], BF16) for i in range(2)]
            l.MU = ar.alloc("MU", [T], F32)
            l.VR = ar.alloc("VR", [T], F32)
            l.cnt = 0
            yield from prenorm(c, layer, 0)

        def ev(c, j, ps):
            l = c.l
            grp, jj = j // 4, j % 4
            pv = ps.ap[:, 0:T]
            if grp == 0:
                fw.op(act, lambda h: h.activation(out=l.GB.ap[:, jj, :], in_=pv, func=AF.Copy), reads=[ps], writes=[l.GB])
            elif grp == 1:
                fw.op(act, lambda h: h.activation(out=l.GC.ap[:, jj, :], in_=pv, func=AF.Copy), reads=[ps], writes=[l.GC])
            elif grp == 2:
                fw.op(dve, lambda h: h.tensor_tensor(out=c.PBt[:, e, jj, 2:2 + T], in0=pv, in1=l.GC.ap[:, jj, :], op=ALU.mult),
                      reads=[ps, l.GC], writes=[c.PB[e]])
            elif grp == 3:
                fw.op(act, lambda h: h.activation(out=l.UA.ap[:, jj, :], in_=pv, func=AF.Copy), reads=[ps], writes=[l.UA])
            else:
                sg = l.SG[l.cnt % 2]
                l.cnt += 1
                fw.op(act, lambda h: h.activation(out=sg.ap, in_=pv, func=AF.Sigmoid), reads=[ps], writes=[sg])
                fw.op(dve, lambda h: h.tensor_tensor(out=c.GLUt[:, e, jj, 30:30 + T], in0=sg.ap, in1=l.UA.ap[:, jj, :], op=ALU.mult),
                      reads=[sg, l.UA], writes=[c.GLU[e]])
        yield from linear_fm(cs, lambda c: c.l.H, KC, "a_w_in", e, 0, 2560, ev)
        for c in cs:
            l = c.l
            MIX = l.MIX
            Z = l.Z
            def build_dg(jj):
                dg = l.DG[jj % 2]
                wa = CPt[:, CPO["conv_a"] + (e * 4 + jj) * 3: CPO["conv_a"] + (e * 4 + jj) * 3 + 3]
                wb = CPt[:, CPO["conv_b"] + (e * 4 + jj) * 31: CPO["conv_b"] + (e * 4 + jj) * 31 + 31]
                fw.op(dve, lambda h: h.tensor_tensor(out=dg.ap[:, 0:3, :], in0=CSTt[:, 0:1, :].broadcast_to([128, 3, 128]),
                                                     in1=wa.unsqueeze(2).broadcast_to([128, 3, 128]), op=ALU.mult),
                      reads=[CST, CP], writes=[dg])
                fw.op(dve, lambda h: h.tensor_tensor(out=dg.ap[:, 3:34, :], in0=CSTt[:, 0:1, :].broadcast_to([128, 31, 128]),
                                                     in1=wb.unsqueeze(2).broadcast_to([128, 31, 128]), op=ALU.mult),
                      reads=[CST, CP], writes=[dg])
            build_dg(0)
            for jj in range(4):
                dg = l.DG[jj % 2]
                (ps,) = fw.psum(1)
                for k in range(3):
                    fw.op(pe, lambda h: h.matmul(ps.ap[:, 0:T], dg.ap[:, k, :], c.PBt[:, e, jj, k:k + T], start=(k == 0), stop=(k == 2)),
                          reads=[dg, c.PB[e]], writes=[ps])
                (ps2,) = fw.psum(1)
                for k in range(31):
                    fw.op(pe, lambda h: h.matmul(ps2.ap[:, 0:T], dg.ap[:, 3 + k, :], c.GLUt[:, e, jj, k:k + T], start=(k == 0), stop=(k == 30)),
                          reads=[dg, c.GLU[e]], writes=[ps2])
                if jj < 3:
                    build_dg(jj + 1)
                fw.op(dve, lambda h: h.tensor_tensor(out=MIX.ap[:, jj, :], in0=ps.ap[:, 0:T], in1=l.GB.ap[:, jj, :], op=ALU.mult),
                      reads=[ps, l.GB], writes=[MIX])
                bcol = cpcol("cb_bias", e * 4 + jj)
                fw.op(dve, lambda h: h.tensor_scalar(out=Z[jj].ap, in0=ps2.ap[:, 0:T], scalar1=bcol, scalar2=None, op0=ALU.add),
                      reads=[ps2, CP], writes=[Z[jj]])
                yield None
            fw.op(dve, lambda h: h.tensor_copy(c.PBt[:, e, :, 0:2], c.PBt[:, e, :, T:T + 2]), reads=[c.PB[e]], writes=[c.PB[e]])
            fw.op(dve, lambda h: h.tensor_copy(c.GLUt[:, e, :, 0:30], c.GLUt[:, e, :, T:T + 30]), reads=[c.GLU[e]], writes=[c.GLU[e]])
            (pm,) = fw.psum(1)
            (pq,) = fw.psum(1)
            MU, VR = l.MU, l.VR
            for jj in range(4):
                zb = l.SQ[0]
                fw.op(act, lambda h: h.activation(out=zb.ap[:, 0:T], in_=Z[jj].ap, func=AF.Copy), reads=[Z[jj]], writes=[zb])
                fw.op(pe, lambda h: h.matmul(pm.ap[:, 0:T], ONESB, zb.ap[:, 0:T], start=(jj == 0), stop=(jj == 3)), reads=[zb, CSB], writes=[pm])
                zq = l.SQ[1]
                fw.op(act, lambda h: h.activation(out=zq.ap[:, 0:T], in_=Z[jj].ap, func=AF.Square), reads=[Z[jj]], writes=[zq])
                fw.op(pe, lambda h: h.matmul(pq.ap[:, 0:T], ONESB, zq.ap[:, 0:T], start=(jj == 0), stop=(jj == 3)), reads=[zq, CSB], writes=[pq])
            fw.op(act, lambda h: h.activation(out=MU.ap, in_=pm.ap[:, 0:T], func=AF.Copy, scale=1.0 / 512), reads=[pm], writes=[MU])
            fw.op(dve, lambda h: h.tensor_tensor(out=VR.ap, in0=MU.ap, in1=MU.ap, op=ALU.mult), reads=[MU], writes=[VR])
            fw.op(dve, lambda h: h.scalar_tensor_tensor(out=VR.ap, in0=pq.ap[:, 0:T], scalar=1.0 / 512, in1=VR.ap, op0=ALU.mult,
                                                        op1=ALU.subtract), reads=[pq, VR], writes=[VR])
            fw.op(dve, lambda h: h.tensor_scalar(out=VR.ap, in0=VR.ap, scalar1=EPS, scalar2=None, op0=ALU.add), reads=[VR], writes=[VR])
            fw.op(act, lambda h: h.activation(out=VR.ap, in_=VR.ap, func=AF.Sqrt), reads=[VR], writes=[VR])
            fw.op(dve, lambda h: h.reciprocal(out=VR.ap, in_=VR.ap), reads=[VR], writes=[VR])
            yield None
            for jj in range(4):
                fw.op(dve, lambda h: h.tensor_tensor(out=Z[jj].ap, in0=Z[jj].ap, in1=MU.ap, op=ALU.subtract), reads=[Z[jj], MU], writes=[Z[jj]])
                fw.op(dve, lambda h: h.tensor_tensor(out=Z[jj].ap, in0=Z[jj].ap, in1=VR.ap, op=ALU.mult), reads=[Z[jj], VR], writes=[Z[jj]])
                gcol = cpcol("ln_g", e * 4 + jj)
                bcol = cpcol("ln_b", e * 4 + jj)
                fw.op(act, lambda h: h.activation(out=MIX.ap[:, 4 + jj, :], in_=Z[jj].ap, func=AF.Silu, bias=bcol, scale=gcol),
                      reads=[Z[jj], CP], writes=[MIX])
        yield from outproj_postnorm(cs, layer, 1, "a_w_out", e)

    def attn_sublayer(cs, layer):
        for c in cs:
            l = common_alloc(c)
            ar = c.ar
            l.Q = ar.alloc("Q", [KC, T], BF16)
            l.PT = [ar.alloc("PT%d" % i, [2, T], BF16) for i in range(2)]
            l.RC = [ar.alloc("RC%d" % i, [T], F32) for i in range(2)]
            l.KTb = ar.alloc("KTb%d" % c.s, [KC, NMEM], BF16)
            l.Vb = ar.alloc("Vb%d" % c.s, [2, D], BF16)
            fw.dma(sp, l.KTb.ap.rearrange("p a b -> p (a b)"), kvd[c.s, layer, :, 0:2048], reads=[c.KVD[layer]], writes=[l.KTb])
            fw.dma(sp, l.Vb.ap.rearrange("p a b -> p (a b)"), kvd[c.s, layer, :, 2048:4096], reads=[c.KVD[layer]], writes=[l.Vb])
            yield from prenorm(c, layer, 2)

        def evq(c, j, ps):
            fw.op(act, lambda h: h.activation(out=c.l.Q.ap[:, j, :], in_=ps.ap[:, 0:T], func=AF.Copy), reads=[ps], writes=[c.l.Q])
        yield from linear_fm(cs, lambda c: c.l.H, KC, "x_w_q", layer, 0, D, evq)
        for hd in range(4):
            for c in cs:
                l = c.l
                Q, MIX = l.Q, l.MIX
                pt = l.PT[hd % 2]
                rc = l.RC[hd % 2]
                for mc in range(2):
                    (ps,) = fw.psum(1)
                    for dj in range(2):
                        fw.op(pe, lambda h: h.matmul(ps.ap[:, 0:T], l.KTb.ap[:, 2 * hd + dj, mc * 128:(mc + 1) * 128], Q.ap[:, 2 * hd + dj, :],
                                                     start=(dj == 0), stop=(dj == 1)), reads=[l.KTb, Q], writes=[ps])
                    fw.op(act, lambda h: h.activation(out=pt.ap[:, mc, :], in_=ps.ap[:, 0:T], func=AF.Exp, scale=1.0 / 16.0),
                          reads=[ps], writes=[pt])
                yield None
                (pd,) = fw.psum(1)
                for mc in range(2):
                    fw.op(pe, lambda h: h.matmul(pd.ap[:, 0:T], ONESB, pt.ap[:, mc, :], start=(mc == 0), stop=(mc == 1)),
                          reads=[pt, CSB], writes=[pd])
                fw.op(dve, lambda h: h.reciprocal(out=rc.ap, in_=pd.ap[:, 0:T]), reads=[pd], writes=[rc])
                for dj in range(2):
                    (po,) = fw.psum(1)
                    for mc in range(2):
                        fw.op(pe, lambda h: h.matmul(po.ap[:, 0:T], l.Vb.ap[:, mc, (2 * hd + dj) * 128:(2 * hd + dj + 1) * 128], pt.ap[:, mc, :],
                                                     start=(mc == 0), stop=(mc == 1)), reads=[l.Vb, pt], writes=[po])
                    fw.op(dve, lambda h: h.tensor_tensor(out=MIX.ap[:, 2 * hd + dj, :], in0=po.ap[:, 0:T], in1=rc.ap, op=ALU.mult),
                          reads=[po, rc], writes=[MIX])
        yield from outproj_postnorm(cs, layer, 3, "x_w_o", layer)

    def ffn_sublayer(cs, layer):
        for c in cs:
            l = common_alloc(c)
            l.A = c.ar.alloc("A", [22, T], BF16)
            l.SG = [c.ar.alloc("SG%d" % i, [T], F32) for i in range(2)]
            yield from prenorm(c, layer, 4)
        for cc in range(11):
            slot, v = yield ("f_w_gu", layer, 0, KC, ((cc * 256, 256), (DFF + cc * 256, 256)))
            for jj in range(2):
                for c in cs:
                    l = c.l
                    H = l.H
                    (pg,) = fw.psum(1)
                    (pu,) = fw.psum(1)
                    for k in range(KC):
                        fw.op(pe, lambda h: h.matmul(pg.ap[:, 0:T], v[:, k, jj * 128:(jj + 1) * 128], H.ap[:, k, :],
                                                     start=(k == 0), stop=(k == KC - 1)), reads=[slot, H], writes=[pg])
                    for k in range(KC):
                        fw.op(pe, lambda h: h.matmul(pu.ap[:, 0:T], v[:, k, 256 + jj * 128:256 + (jj + 1) * 128], H.ap[:, k, :],
                                                     start=(k == 0), stop=(k == KC - 1)), reads=[slot, H], writes=[pu])
                    sg = l.SG[jj]
                    fw.op(act, lambda h: h.activation(out=sg.ap, in_=pg.ap[:, 0:T], func=AF.Silu), reads=[pg], writes=[sg])
                    fw.op(dve, lambda h: h.tensor_tensor(out=l.A.ap[:, 2 * cc + jj, :], in0=pu.ap[:, 0:T], in1=sg.ap, op=ALU.mult),
                          reads=[pu, sg], writes=[l.A])
        for m in range(8):
            slot, v = yield ("f_w_down", layer, 0, 22, ((m * 128, 128),))
            for c in cs:
                A = c.l.A
                (pb,) = fw.psum(1)
                for kc in range(22):
                    fw.op(pe, lambda h: h.matmul(pb.ap[:, 0:T], v[:, kc, :], A.ap[:, kc, :], start=(kc == 0), stop=(kc == 21)),
                          reads=[slot, A], writes=[pb])
                evac_y(c, m, pb, layer, 5)
        yield None
        for c in cs:
            postnorm(c, layer, 5)
        yield None

    def mlstm_sublayer(cs, layer):
        o = layer // 2
        W = "m_w_in"
        for c in cs:
            l = common_alloc(c)
            ar = c.ar
            l.Q = ar.alloc("Q", [KC, T], BF16)
            l.KT = ar.alloc("KT", [TC, D], BF16)
            l.KTT = ar.alloc("KTT", [KC, T], BF16)
            l.VA = ar.alloc("VA", [TC, 8, 129], BF16)
            l.SO = ar.alloc("SO", [TC, D], BF16)
            l.GT = ar.alloc("GT", [TC, 16], F32)
            l.LL = ar.alloc("LL", [TC, 8], F32)
            l.EQ = ar.alloc("EQ", [TC, 8], F32)
            l.EK = ar.alloc("EK", [TC, 8], F32)
            l.GG = ar.alloc("GG", [TC, 8], F32)
            l.WT = [ar.alloc("WT%d" % i, [8, 128], BF16) for i in range(2)]
            l.HH = ar.alloc("HH", [8, 128], F32)
            l.H2 = ar.alloc("H2", [8, 128], F32)
            l.YT = ar.alloc("YT", [8, 128], BF16)
            l.CB = ar.alloc("CB", [8, 129], BF16)
            l.ST = [ar.alloc("ST%d" % i, [8], F32) for i in range(6)]
            yield from prenorm(c, layer, 0)

        def tm_linear(col0, ncols, evac):
            slot, v = yield (W, o, 0, KC, ((col0, ncols),))
            for c in cs:
                H = c.l.H
                for tc in range(TC):
                    (ps,) = fw.psum(1)
                    for k in range(KC):
                        fw.op(pe, lambda h: h.matmul(ps.ap[:, 0:ncols], H.ap[:, k, tc * 128:(tc + 1) * 128], v[:, k, :],
                                                     start=(k == 0), stop=(k == KC - 1)), reads=[slot, H], writes=[ps])
                    evac(c, tc, ps)

        def ev_g(c, tc, ps):
            fw.op(dve, lambda h: h.tensor_tensor(out=c.l.GT.ap[:, tc, :], in0=ps.ap[:, 0:16], in1=RPt[:, o, :], op=ALU.add),
                  reads=[ps, RP], writes=[c.l.GT])
        yield from tm_linear(4 * D, 16, ev_g)
        for c in cs:
            l = c.l
            GT, LL, EQ, EK, GG = l.GT, l.LL, l.EQ, l.EK, l.GG
            fw.op(act, lambda h: h.activation(out=LL.ap, in_=GT.ap[:, :, 8:16], func=AF.Exp, scale=-1.0), reads=[GT], writes=[LL])
            fw.op(act, lambda h: h.activation(out=LL.ap, in_=LL.ap, func=AF.Ln, bias=1.0), reads=[LL], writes=[LL])
            yield None
            (pc,) = fw.psum(1)
            (pg,) = fw.psum(1)
            for tc in range(TC):
                fw.op(pe, lambda h: h.matmul(pc.ap[:, tc * 8:(tc + 1) * 8], TRIF, LL.ap[:, tc, :], start=True, stop=True),
                      reads=[CST, LL], writes=[pc])
                fw.op(pe, lambda h: h.matmul(pg.ap[:, tc * 8:(tc + 1) * 8], ONESF, LL.ap[:, tc, :], start=True, stop=True),
                      reads=[CST, LL], writes=[pg])
            pcv = pc.ap[:, 0:TC * 8].rearrange("p (a b) -> p a b", b=8)
            pgv = pg.ap[:, 0:TC * 8].rearrange("p (a b) -> p a b", b=8)
            fw.op(act, lambda h: h.activation(out=EQ.ap, in_=pcv, func=AF.Exp, scale=-1.0), reads=[pc], writes=[EQ])
            fw.op(act, lambda h: h.activation(out=GG.ap, in_=pgv, func=AF.Exp, scale=-1.0), reads=[pg], writes=[GG])
            fw.op(dve, lambda h: h.tensor_tensor(out=EK.ap, in0=pcv, in1=GT.ap[:, :, 0:8], op=ALU.add), reads=[pc, GT], writes=[EK])
            fw.op(act, lambda h: h.activation(out=EK.ap, in_=EK.ap, func=AF.Exp), reads=[EK], writes=[EK])

        def evq(c, j, ps):
            fw.op(act, lambda h: h.activation(out=c.l.Q.ap[:, j, :], in_=ps.ap[:, 0:T], func=AF.Copy), reads=[ps], writes=[c.l.Q])
        yield from linear_fm(cs, lambda c: c.l.H, KC, W, o, 0, D, evq)
        for cc in range(2):
            def ev_k(c, tc, ps):
                l = c.l
                fw.op(dve, lambda h: h.scalar_tensor_tensor(
                    out=l.KT.ap[:, tc, cc * 512:(cc + 1) * 512].rearrange("p (a b) -> p a b", b=128),
                    in0=ps.ap.rearrange("p (a b) -> p a b", b=128), scalar=KSCALE,
                    in1=l.EK.ap[:, tc, cc * 4:(cc + 1) * 4].unsqueeze(2).broadcast_to([128, 4, 128]),
                    op0=ALU.mult, op1=ALU.mult), reads=[ps, l.EK], writes=[l.KT])
            yield from tm_linear(D + cc * 512, 512, ev_k)
        for c in cs:
            fw.op(dve, lambda h: h.memset(c.l.VA.ap[:, :, :, 128:129], 1.0), writes=[c.l.VA])
        for cc in range(2):
            def ev_v(c, tc, ps):
                fw.op(act, lambda h: h.activation(out=c.l.VA.ap[:, tc, cc * 4:(cc + 1) * 4, 0:128],
                                                  in_=ps.ap.rearrange("p (a b) -> p a b", b=128), func=AF.Copy),
                      reads=[ps], writes=[c.l.VA])
            yield from tm_linear(2 * D + cc * 512, 512, ev_v)
        for cc in range(2):
            def ev_o(c, tc, ps):
                fw.op(act, lambda h: h.activation(out=c.l.SO.ap[:, tc, cc * 512:(cc + 1) * 512], in_=ps.ap, func=AF.Sigmoid),
                      reads=[ps], writes=[c.l.SO])
            yield from tm_linear(3 * D + cc * 512, 512, ev_o)
        for c in cs:
            l = c.l
            for tc in range(TC):
                (pt,) = fw.psum(1)
                ptb = pt.ap.bitcast(BF16)
                for hd in range(8):
                    fw.op(pe, lambda h: h.transpose(ptb[:, hd * 128:(hd + 1) * 128], l.KT.ap[:, tc, hd * 128:(hd + 1) * 128], IDENTB),
                          reads=[l.KT, CSB], writes=[pt])
                fw.op(act, lambda h: h.activation(out=l.KTT.ap[:, :, tc * 128:(tc + 1) * 128],
                                                  in_=ptb.rearrange("p (a b) -> p a b", b=128), func=AF.Copy),
                      reads=[pt], writes=[l.KTT])
            fw.op(act, lambda h: h.activation(out=l.CB.ap, in_=c.C32t[:, o], func=AF.Copy), reads=[c.C32[o]], writes=[l.CB])
        for tc in range(TC):
            for c in cs:
                yield from chunk(c, o, tc)
        yield from outproj_postnorm(cs, layer, 1, "m_w_out", o)

    def chunk(c, o, tc):
        l = c.l
        Q, KT, KTT, VA, SO, EQ, GG, HH, H2, YT, CB, MIX = l.Q, l.KT, l.KTT, l.VA, l.SO, l.EQ, l.GG, l.HH, l.H2, l.YT, l.CB, l.MIX
        C32t, C32b = c.C32t, c.C32[o]
        sl = slice(tc * 128, (tc + 1) * 128)
        wt = l.WT[tc % 2]
        pss = fw.psum(2)
        for hd in range(8):
            b = pss[hd // 4]
            fw.op(pe, lambda h: h.matmul(b.ap[:, (hd % 4) * 128:(hd % 4 + 1) * 128], KTT.ap[:, hd, sl], Q.ap[:, hd, sl],
                                         start=True, stop=True), reads=[KTT, Q], writes=[b])
        for g2 in range(2):
            fw.op(dve, lambda h: h.tensor_tensor(out=wt.ap[:, g2 * 4:(g2 + 1) * 4, :],
                                                 in0=pss[g2].ap.rearrange("p (a b) -> p a b", b=128),
                                                 in1=CSTt[:, 1:2, :].broadcast_to([128, 4, 128]), op=ALU.mult),
                  reads=[pss[g2], CST], writes=[wt])
        psp = fw.psum(3)
        for hd in range(8):
            bp = psp[hd // 3]
            cs_ = slice((hd % 3) * 129, (hd % 3 + 1) * 129)
            fw.op(pe, lambda h: h.matmul(bp.ap[:, cs_], KT.ap[:, tc, hd * 128:(hd + 1) * 128], VA.ap[:, tc, hd, :],
                                         start=True, stop=True), reads=[KT, VA], writes=[bp])
        for g3 in range(3):
            nh = 3 if g3 < 2 else 2
            hs = slice(g3 * 3, g3 * 3 + nh)
            fw.op(dve, lambda h: h.tensor_tensor(out=C32t[:, o, hs, :], in0=psp[g3].ap[:, 0:nh * 129].rearrange("p (a b) -> p a b", b=129),
                                                 in1=C32t[:, o, hs, :], op=ALU.add), reads=[psp[g3], C32b], writes=[C32b])
        yield None
        psn = fw.psum(3)
        for hd in range(8):
            bn = psn[hd // 3]
            cs_ = slice((hd % 3) * 129, (hd % 3 + 1) * 129)
            fw.op(pe, lambda h: h.matmul(bn.ap[:, cs_], Q.ap[:, hd, sl], CB.ap[:, hd, :], start=True, stop=False),
                  reads=[Q, CB], writes=[bn])
            fw.op(pe, lambda h: h.matmul(bn.ap[:, cs_], wt.ap[:, hd, :], VA.ap[:, tc, hd, :], start=False, stop=True),
                  reads=[wt, VA], writes=[bn])
        fw.op(dve, lambda h: h.tensor_tensor(out=C32t[:, o], in0=C32t[:, o],
                                             in1=GG.ap[:, tc, :].unsqueeze(2).broadcast_to([128, 8, 129]), op=ALU.mult),
              reads=[C32b, GG], writes=[C32b])
        fw.op(act, lambda h: h.activation(out=CB.ap, in_=C32t[:, o], func=AF.Copy), reads=[C32b], writes=[CB])
        DN, RR, S1, S2, MUh, RS = l.ST
        for g3 in range(3):
            nh = 3 if g3 < 2 else 2
            hs = slice(g3 * 3, g3 * 3 + nh)
            v3 = psn[g3].ap[:, 0:nh * 129].rearrange("p (a b) -> p a b", b=129)
            fw.op(act, lambda h: h.activation(out=DN.ap[:, hs].unsqueeze(2), in_=v3[:, :, 128:129], func=AF.Abs),
                  reads=[psn[g3]], writes=[DN])
        fw.op(dve, lambda h: h.tensor_tensor(out=DN.ap, in0=DN.ap, in1=EQ.ap[:, tc, :], op=ALU.mult), reads=[DN, EQ], writes=[DN])
        fw.op(dve, lambda h: h.tensor_scalar(out=DN.ap, in0=DN.ap, scalar1=1.0, scalar2=None, op0=ALU.max), reads=[DN], writes=[DN])
        fw.op(dve, lambda h: h.reciprocal(out=DN.ap, in_=DN.ap), reads=[DN], writes=[DN])
        fw.op(dve, lambda h: h.tensor_tensor(out=RR.ap, in0=EQ.ap[:, tc, :], in1=DN.ap, op=ALU.mult), reads=[DN, EQ], writes=[RR])
        for g3 in range(3):
            nh = 3 if g3 < 2 else 2
            hs = slice(g3 * 3, g3 * 3 + nh)
            v3 = psn[g3].ap[:, 0:nh * 129].rearrange("p (a b) -> p a b", b=129)
            fw.op(dve, lambda h: h.tensor_tensor(out=HH.ap[:, hs, :], in0=v3[:, :, 0:128],
                                                 in1=RR.ap[:, hs].unsqueeze(2).broadcast_to([128, nh, 128]), op=ALU.mult),
                  reads=[psn[g3], RR], writes=[HH])
        fw.op(dve, lambda h: h.tensor_reduce(out=S1.ap, in_=HH.ap, axis=AX.X, op=ALU.add), reads=[HH], writes=[S1])
        fw.op(act, lambda h: h.activation(out=H2.ap, in_=HH.ap, func=AF.Square), reads=[HH], writes=[H2])
        fw.op(dve, lambda h: h.tensor_reduce(out=S2.ap, in_=H2.ap, axis=AX.X, op=ALU.add), reads=[H2], writes=[S2])
        fw.op(dve, lambda h: h.tensor_scalar(out=MUh.ap, in0=S1.ap, scalar1=1.0 / 128, scalar2=None, op0=ALU.mult), reads=[S1], writes=[MUh])
        fw.op(dve, lambda h: h.tensor_tensor(out=S1.ap, in0=MUh.ap, in1=MUh.ap, op=ALU.mult), reads=[MUh], writes=[S1])
        fw.op(dve, lambda h: h.scalar_tensor_tensor(out=RS.ap, in0=S2.ap, scalar=1.0 / 128, in1=S1.ap, op0=ALU.mult, op1=ALU.subtract),
              reads=[S2, S1], writes=[RS])
        fw.op(dve, lambda h: h.tensor_scalar(out=RS.ap, in0=RS.ap, scalar1=EPS, scalar2=None, op0=ALU.add), reads=[RS], writes=[RS])
        fw.op(act, lambda h: h.activation(out=RS.ap, in_=RS.ap, func=AF.Sqrt), reads=[RS], writes=[RS])
        fw.op(dve, lambda h: h.reciprocal(out=RS.ap, in_=RS.ap), reads=[RS], writes=[RS])
        fw.op(dve, lambda h: h.tensor_tensor(out=HH.ap, in0=HH.ap, in1=MUh.ap.unsqueeze(2).broadcast_to([128, 8, 128]), op=ALU.subtract),
              reads=[HH, MUh], writes=[HH])
        fw.op(dve, lambda h: h.tensor_tensor(out=HH.ap, in0=HH.ap, in1=RS.ap.unsqueeze(2).broadcast_to([128, 8, 128]), op=ALU.mult),
              reads=[HH, RS], writes=[HH])
        fw.op(dve, lambda h: h.tensor_tensor(out=YT.ap, in0=HH.ap, in1=SO.ap[:, tc, :].rearrange("p (a b) -> p a b", b=128), op=ALU.mult),
              reads=[HH, SO], writes=[YT])
        yield None
        (py,) = fw.psum(1)
        pyb = py.ap.bitcast(BF16)
        for hd in range(8):
            fw.op(pe, lambda h: h.transpose(pyb[:, hd * 128:(hd + 1) * 128], YT.ap[:, hd, :], IDENTB), reads=[YT, CSB], writes=[py])
        mg = CPt[:, CPO["m_g"] + o * 8: CPO["m_g"] + o * 8 + 8]
        fw.op(dve, lambda h: h.tensor_tensor(out=MIX.ap[:, :, sl], in0=pyb.rearrange("p (a b) -> p a b", b=128),
                                             in1=mg.unsqueeze(2).broadcast_to([128, 8, 128]), op=ALU.mult),
              reads=[py, CP], writes=[MIX])

    def ctx_program(c):
        cs = [c]
        for o in range(2):
            fw.op(dve, lambda h: h.memset(c.C32t[:, o], 0.0), writes=[c.C32[o]])
        for e in range(2):
            fw.op(dve, lambda h: h.memset(c.GLUt[:, e], 0.0), writes=[c.GLU[e]])
            fw.op(dve, lambda h: h.memset(c.PBt[:, e], 0.0), writes=[c.PB[e]])
        for layer in range(DEPTH):
            yield from kv_prep(cs, layer)
        for t in range(NT):
            fw.dma(sp, c.Xt[:], io["x"][c.s][:, t * T:(t + 1) * T].rearrange("(k p) t -> p k t", p=128), writes=[c.X])
            for layer in range(DEPTH):
                if layer % 2 == 0:
                    yield from conv_sublayer(cs, layer)
                else:
                    yield from mlstm_sublayer(cs, layer)
                yield from attn_sublayer(cs, layer)
                yield from ffn_sublayer(cs, layer)
            fw.dma(sp, io["y"][c.s][:, t * T:(t + 1) * T].rearrange("(k p) t -> p k t", p=128), c.Xt[:], reads=[c.X])

    run = ctxs[:1] if dry else ctxs
    gens = [ctx_program(c) for c in run]
    n = len(gens)
    nxt = [None] * n
    blk = [0] * n
    prog = [0] * n
    done = [False] * n
    for i, g in enumerate(gens):
        try:
            nxt[i] = next(g)
        except StopIteration:
            done[i] = True
    while not all(done):
        if n == 1 or done[1]:
            i = 0
        elif done[0]:
            i = 1
        else:
            i = 0 if (prog[0] - prog[1]) < SKEW else 1
        if nxt[i] is None:
            val = None
        else:
            bmin = min(blk[j] for j in range(n) if not done[j])
            val = wq.get(blk[i], nxt[i], bmin)
            blk[i] += 1
        prog[i] += 1
        try:
            nxt[i] = gens[i].send(val)
        except StopIteration:
            done[i] = True
    fw.wait_all(sp, [c.X for c in ctxs])
    return fw


def build_nc(NSEQ=2, NT=8, DEPTH=4):
    S = NT * T
    nc = bass.Bass("TRN2", target_bir_lowering=False)

    def din(name, shape):
        return nc.dram_tensor(name, shape, F32, kind="ExternalInput").ap()
    io = {}
    io["x"] = din("x", [NSEQ, D, S])
    io["mem"] = din("mem", [NSEQ, D, NMEM])
    io["consts"] = din("consts", [128, 3, 128])
    io["cp"] = din("cp", [128, NCP])
    io["rp"] = din("rp", [128, 2, 16])
    io["w"] = {
        "a_w_in": din("a_w_in", [2, D, 2560]), "a_w_out": din("a_w_out", [2, D, D]),
        "m_w_in": din("m_w_in", [2, D, 4112]), "m_w_out": din("m_w_out", [2, D, D]),
        "x_w_q": din("x_w_q", [4, D, D]), "x_w_kv": din("x_w_kv", [4, D, 2 * D]), "x_w_o": din("x_w_o", [4, D, D]),
        "f_w_gu": din("f_w_gu", [4, D, 2 * DFF]), "f_w_down": din("f_w_down", [4, DFF, D]),
    }
    io["y"] = nc.dram_tensor("y", [NSEQ, D, S], F32, kind="ExternalOutput").ap()
    io["kvd"] = nc.dram_tensor("kvd", [NSEQ, 4, 128, 4096], BF16, kind="Internal").ap()
    plan = []
    with contextlib.ExitStack() as es:
        _emit(nc, es, True, plan, NSEQ, NT, DEPTH, io)
    es2 = contextlib.ExitStack()
    with es2:
        fw = _emit(nc, es2, False, plan, NSEQ, NT, DEPTH, io)
    return nc, fw


def host_tables(inp):
    cp = np.zeros((128, NCP), np.float32)

    def put(name, idx, vec128):
        cp[:, CPO[name] + idx] = vec128
    ng = np.asarray(inp["norm_g"], np.float32)
    for l in range(4):
        for j in range(6):
            for k in range(8):
                put("norm_g", (l * 6 + j) * 8 + k, ng[l, j, k * 128:(k + 1) * 128])
    mg = np.asarray(inp["mem_norm_g"], np.float32)
    for l in range(4):
        for k in range(8):
            put("mem_g", l * 8 + k, mg[l, k * 128:(k + 1) * 128])
    ca = np.asarray(inp["a_conv_a"], np.float32)
    cb = np.asarray(inp["a_conv_b"], np.float32)
    cbb = np.asarray(inp["a_conv_b_bias"], np.float32)
    lg = np.asarray(inp["a_ln_g"], np.float32)
    lb = np.asarray(inp["a_ln_b"], np.float32)
    for e in range(2):
        for c in range(4):
            for k in range(3):
                put("conv_a", (e * 4 + c) * 3 + k, ca[e, k, c * 128:(c + 1) * 128])
            for k in range(31):
                put("conv_b", (e * 4 + c) * 31 + k, cb[e, k, c * 128:(c + 1) * 128])
            put("cb_bias", e * 4 + c, cbb[e, c * 128:(c + 1) * 128])
            put("ln_g", e * 4 + c, lg[e, c * 128:(c + 1) * 128])
            put("ln_b", e * 4 + c, lb[e, c * 128:(c + 1) * 128])
    mng = np.asarray(inp["m_norm_g"], np.float32)
    for o in range(2):
        for k in range(8):
            put("m_g", o * 8 + k, mng[o, k * 128:(k + 1) * 128])
    rp = np.zeros((128, 2, 16), np.float32)
    rp[:, :, 0:8] = np.asarray(inp["m_i_bias"], np.float32)[None]
    rp[:, :, 8:16] = np.asarray(inp["m_f_bias"], np.float32)[None]
    consts = np.zeros((128, 3, 128), np.float32)
    consts[:, 0, :] = np.eye(128, dtype=np.float32)
    consts[:, 1, :] = np.triu(np.ones((128, 128), np.float32))
    consts[:, 2, :] = 1.0
    return cp, rp, consts


WNAMES = ["a_w_in", "a_w_out", "m_w_in", "m_w_out", "x_w_q", "x_w_kv", "x_w_o", "f_w_gu", "f_w_down"]


def kernel(**inp):
    x = np.asarray(inp["x"], np.float32)
    mem = np.asarray(inp["mem"], np.float32)
    B = x.shape[0]
    nseq = B // NCORES
    cp, rp, consts = host_tables(inp)
    nc, _ = build_nc(NSEQ=nseq, NT=x.shape[1] // T, DEPTH=4)
    shared = {"consts": consts, "cp": cp, "rp": rp}
    for w in WNAMES:
        shared[w] = np.ascontiguousarray(np.asarray(inp[w], np.float32))
    in_maps = []
    for c in range(NCORES):
        m = dict(shared)
        m["x"] = np.ascontiguousarray(x[c * nseq:(c + 1) * nseq].transpose(0, 2, 1))
        m["mem"] = np.ascontiguousarray(mem[c * nseq:(c + 1) * nseq].transpose(0, 2, 1))
        in_maps.append(m)
    res = run_bass_kernel_spmd(nc, in_maps, core_ids=list(range(NCORES)))
    out = np.empty_like(x)
    for c in range(NCORES):
        y = res.results[c]["y"]
        out[c * nseq:(c + 1) * nseq] = y.transpose(0, 2, 1)
    return out
```
